# Optimizing a Trainium2 kernel written in Bass

```python
import jax
import jax.numpy as jnp
from jax import lax
import numpy as np

D_MODEL = 1024
BATCH = 16
SEQ = 2048
DEPTH = 1

N_MEM = 256
EPS = 1e-6

ML_HEADS = 4
ML_QK_DIM = 128
ML_V_DIM = D_MODEL // ML_HEADS
ML_CHUNK = 64
ML_QK_W = ML_HEADS * ML_QK_DIM
ML_V_W = ML_HEADS * ML_V_DIM

FX_HEADS = 8
FX_HEAD_DIM = D_MODEL // FX_HEADS
FX_BLOCK = 128
FX_W = FX_HEADS * FX_HEAD_DIM

XA_HEADS = 4
XA_HEAD_DIM = D_MODEL // XA_HEADS

N_GROUPS = 4
EXPERTS_PER_GROUP = 8
N_EXPERTS = N_GROUPS * EXPERTS_PER_GROUP
TOP_K = 2
D_EXPERT = D_MODEL // 2

IN_SPLITS = (ML_QK_W, ML_QK_W, ML_V_W, ML_V_W, ML_HEADS, ML_HEADS,
             FX_W, FX_W, FX_W, FX_HEADS, D_MODEL, D_MODEL)
D_IN = 2 * ML_QK_W + 2 * ML_V_W + 2 * ML_HEADS + 3 * FX_W + FX_HEADS + 2 * D_MODEL

kernel_name = 'hybrid_mlstm_fox_hmoe_block'


def _rmsnorm(x, g):
    xf = x.astype(jnp.float32)
    y = xf * lax.rsqrt(jnp.mean(xf * xf, axis=-1, keepdims=True) + EPS)
    return (y * g.astype(jnp.float32)).astype(x.dtype)


def _head_rmsnorm(y, g):
    yf = y.astype(jnp.float32)
    return yf * lax.rsqrt(jnp.mean(yf * yf, axis=-1, keepdims=True) + EPS) * g.astype(jnp.float32)


def _split_cols(z, sizes):
    parts, off = [], 0
    for s in sizes:
        parts.append(z[..., off:off + s])
        off += s
    return parts


def _to_chunks(a, n_chunks, chunk):
    b, _, h = a.shape[:3]
    a = a.reshape((b, n_chunks, chunk, h) + a.shape[3:])
    return jnp.moveaxis(a, (1, 3), (0, 2))


def _mlstm(q, k, v, i_pre, log_f):
    B, S, H, dk = q.shape
    dv = v.shape[-1]
    L = ML_CHUNK
    nc = S // L
    f32 = jnp.float32
    q = q.astype(f32)
    k = k.astype(f32) * (dk ** -0.5)
    v = v.astype(f32)
    xs = tuple(_to_chunks(a, nc, L) for a in (q, k, v, i_pre, log_f))
    causal = jnp.tril(jnp.ones((L, L), dtype=bool))

    def step(carry, inp):
        C, n, m = carry
        qc, kc, vc, ic, lfc = inp
        b = jnp.cumsum(lfc, axis=-1)
        log_d = jnp.where(causal, b[..., :, None] - b[..., None, :] + ic[..., None, :], -jnp.inf)
        log_inter = b + m[..., None]
        m_t = jnp.maximum(log_inter, jnp.max(log_d, axis=-1))
        s = jnp.einsum('bhtd,bhsd->bhts', qc, kc) * jnp.exp(log_d - m_t[..., None])
        w_inter = jnp.exp(log_inter - m_t)
        num = (jnp.einsum('bhts,bhsv->bhtv', s, vc)
               + w_inter[..., None] * jnp.einsum('bhvd,bhtd->bhtv', C, qc))
        den = jnp.sum(s, axis=-1) + w_inter * jnp.einsum('bhd,bhtd->bht', n, qc)
        h = num / jnp.maximum(jnp.abs(den), jnp.exp(-m_t))[..., None]
        b_end = b[..., -1]
        log_w = b_end[..., None] - b + ic
        m_new = jnp.maximum(b_end + m, jnp.max(log_w, axis=-1))
        w_s = jnp.exp(log_w - m_new[..., None])
        decay = jnp.exp(b_end + m - m_new)
        C_new = decay[..., None, None] * C + jnp.einsum('bhs,bhsv,bhsd->bhvd', w_s, vc, kc)
        n_new = decay[..., None] * n + jnp.einsum('bhs,bhsd->bhd', w_s, kc)
        return (C_new, n_new, m_new), h

    init = (jnp.zeros((B, H, dv, dk), f32), jnp.zeros((B, H, dk), f32), jnp.zeros((B, H), f32))
    _, hs = lax.scan(step, init, xs)
    return jnp.moveaxis(hs, (0, 2), (1, 3)).reshape(B, S, H, dv)


def _forgetting_attention(q, k, v, log_f):
    B, S, H, d = q.shape
    c = jnp.cumsum(log_f.astype(jnp.float32), axis=1).transpose(0, 2, 1)
    qh = q.transpose(0, 2, 1, 3)
    kh = k.transpose(0, 2, 1, 3)
    vh = v.transpose(0, 2, 1, 3)
    scale = d ** -0.5
    outs = []
    for blk in range(S // FX_BLOCK):
        q0 = blk * FX_BLOCK
        q1 = q0 + FX_BLOCK
        logits = jnp.einsum('bhqd,bhkd->bhqk', qh[:, :, q0:q1], kh[:, :, :q1]).astype(jnp.float32) * scale
        logits = logits + c[:, :, q0:q1, None] - c[:, :, None, :q1]
        mask = (q0 + jnp.arange(FX_BLOCK))[:, None] >= jnp.arange(q1)[None, :]
        p = jax.nn.softmax(jnp.where(mask, logits, -jnp.inf), axis=-1)
        outs.append(jnp.einsum('bhqk,bhkd->bhqd', p.astype(v.dtype), vh[:, :, :q1]))
    o = jnp.concatenate(outs, axis=2)
    return o.transpose(0, 2, 1, 3).reshape(B, S, H * d)


def _cross_attention(h, m, w_xq, w_xkv, w_xo):
    B, S, D = h.shape
    N = m.shape[1]
    q = (h @ w_xq).reshape(B, S, XA_HEADS, XA_HEAD_DIM)
    kv = m @ w_xkv
    k = kv[..., :D].reshape(B, N, XA_HEADS, XA_HEAD_DIM)
    v = kv[..., D:].reshape(B, N, XA_HEADS, XA_HEAD_DIM)
    logits = jnp.einsum('bshd,bnhd->bhsn', q, k).astype(jnp.float32) * (XA_HEAD_DIM ** -0.5)
    p = jax.nn.softmax(logits, axis=-1)
    o = jnp.einsum('bhsn,bnhd->bshd', p.astype(v.dtype), v)
    return o.reshape(B, S, D) @ w_xo


def _hier_moe(h, w_rg, b_rg, w_re, b_re, w_gate, w_up, w_down):
    B, S, D = h.shape
    f32 = jnp.float32
    t = h.reshape(B * S, D)
    g_logits = (t @ w_rg).astype(f32) + b_rg.astype(f32)
    g_prob = jax.nn.softmax(g_logits, axis=-1)
    g_idx = jnp.argmax(g_logits, axis=-1)
    g_onehot = jax.nn.one_hot(g_idx, N_GROUPS, dtype=f32)
    g_p = jnp.sum(g_prob * g_onehot, axis=-1, keepdims=True)
    e_logits = ((t @ w_re).astype(f32) + b_re.astype(f32)).reshape(-1, N_GROUPS, EXPERTS_PER_GROUP)
    e_sel = jnp.einsum('tge,tg->te', e_logits, g_onehot)
    top_v, top_i = lax.top_k(e_sel, TOP_K)
    top_w = jax.nn.softmax(top_v, axis=-1) * g_p
    expert_id = g_idx[:, None] * EXPERTS_PER_GROUP + top_i
    combine = jnp.sum(jax.nn.one_hot(expert_id, N_EXPERTS, dtype=f32) * top_w[..., None], axis=1)
    y = jnp.zeros((B * S, D), f32)
    for e in range(N_EXPERTS):
        he = jax.nn.silu(t @ w_gate[e]) * (t @ w_up[e])
        y = y + combine[:, e:e + 1] * (he @ w_down[e]).astype(f32)
    return y.astype(h.dtype).reshape(B, S, D)


def setup_inputs(seed: int = 0) -> dict:
    key = jax.random.key(seed)
    ks = jax.random.split(key, 32)
    f32 = jnp.float32
    L = DEPTH

    def nrm(k, shape, scale):
        return jax.random.normal(k, shape, f32) * scale

    def gain(k, shape):
        return 1.0 + 0.05 * jax.random.normal(k, shape, f32)

    return {
        'x': nrm(ks[0], (BATCH, SEQ, D_MODEL), 1.0),
        'mem': nrm(ks[1], (BATCH, N_MEM, D_MODEL), 1.0),
        'g_mix': gain(ks[2], (L, D_MODEL)),
        'w_in': nrm(ks[3], (L, D_MODEL, D_IN), D_MODEL ** -0.5),
        'b_ml_i': nrm(ks[4], (L, ML_HEADS), 0.1),
        'b_ml_f': jnp.linspace(3.0, 6.0, ML_HEADS, dtype=f32) + nrm(ks[5], (L, ML_HEADS), 0.1),
        'b_fx_f': jnp.linspace(1.0, 5.0, FX_HEADS, dtype=f32) + nrm(ks[6], (L, FX_HEADS), 0.1),
        'b_gate_ml': nrm(ks[7], (L, D_MODEL), 0.02),
        'b_gate_fx': nrm(ks[8], (L, D_MODEL), 0.02),
        'g_ml_head': gain(ks[9], (L, ML_V_W)),
        'w_proj_ml': nrm(ks[10], (L, ML_V_W, D_MODEL), ML_V_W ** -0.5),
        'w_proj_fx': nrm(ks[11], (L, FX_W, D_MODEL), FX_W ** -0.5),
        'w_out': nrm(ks[12], (L, D_MODEL, D_MODEL), D_MODEL ** -0.5),
        'g_xq': gain(ks[13], (L, D_MODEL)),
        'g_xmem': gain(ks[14], (L, D_MODEL)),
        'w_xq': nrm(ks[15], (L, D_MODEL, D_MODEL), D_MODEL ** -0.5),
        'w_xkv': nrm(ks[16], (L, D_MODEL, 2 * D_MODEL), D_MODEL ** -0.5),
        'w_xo': nrm(ks[17], (L, D_MODEL, D_MODEL), D_MODEL ** -0.5),
        'g_moe': gain(ks[18], (L, D_MODEL)),
        'w_rg': nrm(ks[19], (L, D_MODEL, N_GROUPS), D_MODEL ** -0.5),
        'b_rg': nrm(ks[20], (L, N_GROUPS), 0.01),
        'w_re': nrm(ks[21], (L, D_MODEL, N_EXPERTS), D_MODEL ** -0.5),
        'b_re': nrm(ks[22], (L, N_EXPERTS), 0.01),
        'w_gate': nrm(ks[23], (L, N_EXPERTS, D_MODEL, D_EXPERT), D_MODEL ** -0.5),
        'w_up': nrm(ks[24], (L, N_EXPERTS, D_MODEL, D_EXPERT), D_MODEL ** -0.5),
        'w_down': nrm(ks[25], (L, N_EXPERTS, D_EXPERT, D_MODEL), D_EXPERT ** -0.5),
        'g_final': gain(ks[26], (D_MODEL,)),
    }


def reference(x, mem, g_mix, w_in, b_ml_i, b_ml_f, b_fx_f, b_gate_ml, b_gate_fx, g_ml_head,
              w_proj_ml, w_proj_fx, w_out, g_xq, g_xmem, w_xq, w_xkv, w_xo, g_moe,
              w_rg, b_rg, w_re, b_re, w_gate, w_up, w_down, g_final):
    B, S, _ = x.shape
    f32 = jnp.float32
    for l in range(DEPTH):
        h = _rmsnorm(x, g_mix[l])
        (ml_q, ml_k, ml_v, ml_o, ml_i, ml_f,
         fx_q, fx_k, fx_v, fx_f, gt_ml, gt_fx) = _split_cols(h @ w_in[l], IN_SPLITS)
        y_ml = _mlstm(ml_q.reshape(B, S, ML_HEADS, ML_QK_DIM),
                      ml_k.reshape(B, S, ML_HEADS, ML_QK_DIM),
                      ml_v.reshape(B, S, ML_HEADS, ML_V_DIM),
                      ml_i.astype(f32) + b_ml_i[l].astype(f32),
                      jax.nn.log_sigmoid(ml_f.astype(f32) + b_ml_f[l].astype(f32)))
        y_ml = _head_rmsnorm(y_ml, g_ml_head[l].reshape(ML_HEADS, ML_V_DIM))
        y_ml = y_ml.reshape(B, S, ML_V_W).astype(x.dtype) * jax.nn.sigmoid(ml_o)
        y_fx = _forgetting_attention(fx_q.reshape(B, S, FX_HEADS, FX_HEAD_DIM),
                                     fx_k.reshape(B, S, FX_HEADS, FX_HEAD_DIM),
                                     fx_v.reshape(B, S, FX_HEADS, FX_HEAD_DIM),
                                     jax.nn.log_sigmoid(fx_f.astype(f32) + b_fx_f[l].astype(f32)))
        merged = (jax.nn.sigmoid(gt_ml + b_gate_ml[l]) * (y_ml @ w_proj_ml[l])
                  + jax.nn.sigmoid(gt_fx + b_gate_fx[l]) * (y_fx @ w_proj_fx[l]))
        x = x + merged @ w_out[l]
        x = x + _cross_attention(_rmsnorm(x, g_xq[l]), _rmsnorm(mem, g_xmem[l]),
                                 w_xq[l], w_xkv[l], w_xo[l])
        x = x + _hier_moe(_rmsnorm(x, g_moe[l]), w_rg[l], b_rg[l], w_re[l], b_re[l],
                          w_gate[l], w_up[l], w_down[l])
    return _rmsnorm(x, g_final)
```

```python
import contextlib
import numpy as np
import concourse.bass as bass
import concourse.mybir as mybir
from concourse.bass_utils import run_bass_kernel_spmd

F32 = mybir.dt.float32
BF16 = mybir.dt.bfloat16
I32 = mybir.dt.int32
AF = mybir.ActivationFunctionType
ALU = mybir.AluOpType
AX = mybir.AxisListType

ENGS = ("pe", "dve", "act", "pool", "sp")
SEM_EPOCH = 20000
NDMA_SEMS = 12
STRICT_SAME_ENGINE = True


class Buf:
    __slots__ = ("name", "last_w", "readers", "wgroup")

    def __init__(self, name):
        self.name = name
        self.last_w = []
        self.readers = []
        self.wgroup = None


class Op:
    __slots__ = ("eng", "fn", "deps", "is_dma", "idx", "signal", "count", "dsem", "dval", "dprev", "seg", "kind", "aux")

    def __init__(self, eng, fn, is_dma):
        self.eng = eng
        self.fn = fn
        self.deps = []
        self.is_dma = is_dma
        self.idx = -1
        self.signal = False
        self.count = 0
        self.dsem = -1
        self.dval = 0
        self.dprev = 0
        self.seg = 0
        self.kind = "op"
        self.aux = None


class Prog:
    def __init__(self, nc):
        self.nc = nc
        self.streams = {e: [] for e in ENGS}
        self.ndma = {}
        self.dma_uses = {}
        self.out_dmas = []
        self.seg = 0
        self.cond_segs = set()
        self.markers = {}
        self.block_dmas = None

    def _redirect(self, d):
        if d.seg in self.cond_segs and d.seg != self.seg:
            return self.markers[d.seg][d.eng]
        return d

    def _add(self, eng, fn, reads, writes, is_dma, kind="op", group=None):
        op = Op(eng, fn, is_dma)
        op.seg = self.seg
        op.kind = kind
        st = self.streams[eng]
        op.idx = len(st)
        deps = []
        for b in reads:
            if b is not None:
                for w_ in b.last_w:
                    deps.append((w_, "raw"))
        for b in writes:
            if b is None:
                continue
            if not (group is not None and b.wgroup == group):
                for w_ in b.last_w:
                    deps.append((w_, "waw"))
            for r in b.readers:
                deps.append((r, "war"))
        latest = {}
        dma_deps = {}
        for (d, kind_) in deps:
            d = self._redirect(d)
            if d is op:
                continue
            if d.is_dma:
                dma_deps[id(d)] = d
                continue
            if d.eng == eng and not is_dma:
                if eng == "pe":
                    continue
                if not STRICT_SAME_ENGINE and (kind_ != "raw" or (op.idx - d.idx) > 2):
                    continue
            cur = latest.get(d.eng)
            if cur is None or d.idx > cur.idx:
                latest[d.eng] = d
        for d in list(latest.values()) + list(dma_deps.values()):
            op.deps.append(d)
            d.signal = True
        for b in reads:
            if b is not None:
                b.readers.append(op)
        for b in writes:
            if b is not None:
                if group is not None and b.wgroup == group:
                    b.last_w.append(op)
                else:
                    b.last_w = [op]
                    b.wgroup = group
                b.readers = []
        if is_dma:
            n = self.ndma.get((eng, self.seg), 0)
            k = n % NDMA_SEMS
            self.ndma[(eng, self.seg)] = n + 1
            key = (eng, self.seg, k)
            u = self.dma_uses.get(key, 0)
            op.dsem = key
            op.dprev = 16 * u
            op.dval = 16 * (u + 1)
            self.dma_uses[key] = u + 1
            if self.block_dmas is not None:
                self.block_dmas[eng].append(op)
        st.append(op)
        return op

    def op(self, eng, fn, reads=(), writes=()):
        return self._add(eng, fn, reads, writes, False)

    def dma(self, eng, fn, reads=(), writes=(), is_out=False, group=None):
        o = self._add(eng, fn, reads, writes, True, group=group)
        if is_out:
            self.out_dmas.append(o)
        return o

    def inherit(self, new_bufs, old_bufs):
        latest = {}
        dmas = {}
        for b in old_bufs:
            cand = list(b.readers) + list(b.last_w)
            for d in cand:
                d = self._redirect(d)
                if d.is_dma:
                    dmas[id(d)] = d
                else:
                    cur = latest.get(d.eng)
                    if cur is None or d.idx > cur.idx:
                        latest[d.eng] = d
        S = list(latest.values()) + list(dmas.values())
        for nb in new_bufs:
            nb.last_w = []
            nb.wgroup = None
            nb.readers = list(S)

    def if_begin(self, flag_ap, flag_buf):
        assert self.block_dmas is None
        for e in ENGS:
            o = self._add(e, None, [flag_buf], (), False, kind="ifb")
            o.aux = flag_ap
        self.seg += 1
        self.cond_segs.add(self.seg)
        self.block_dmas = {e: [] for e in ENGS}

    def if_end(self):
        cseg = self.seg
        for e in ENGS:
            o = self._add(e, None, (), (), False, kind="ife")
            o.aux = list(self.block_dmas[e])
        self.block_dmas = None
        self.seg += 1
        self.markers[cseg] = {}
        for e in ENGS:
            m = self._add(e, lambda h: h.drain(), (), (), False, kind="marker")
            self.markers[cseg][e] = m

    def emit(self):
        nc = self.nc
        fin = Op("sp", None, False)
        fin.seg = self.seg
        fin.idx = len(self.streams["sp"])
        for o in self.out_dmas:
            fin.deps.append(self._redirect(o))
            fin.deps[-1].signal = True
        self.streams["sp"].append(fin)
        nsem = {}
        for e in ENGS:
            c = {}
            for o in self.streams[e]:
                if o.is_dma:
                    continue
                if o.signal:
                    c[o.seg] = c.get(o.seg, 0) + 1
                    o.count = c[o.seg]
            for sg, v in c.items():
                nsem[(e, sg)] = (v - 1) // SEM_EPOCH + 1
        with contextlib.ExitStack() as es:
            sems = {}
            for (e, sg), n in nsem.items():
                for k in range(n):
                    sems[(e, sg, k)] = es.enter_context(nc.semaphore(f"s_{e}_{sg}_{k}"))
            dsems = {}
            for key in self.dma_uses:
                dsems[key] = es.enter_context(nc.semaphore(f"d_{key[0]}_{key[1]}_{key[2]}"))
            block = es.enter_context(nc.Block())
            handles = {"pe": nc.tensor, "dve": nc.vector, "act": nc.scalar, "pool": nc.gpsimd, "sp": nc.sync}

            def run(e):
                h = handles[e]
                state = {"waited": {}}
                stack = []

                def wait(semkey, sem, val):
                    w = state["waited"]
                    if w.get(semkey, 0) >= val:
                        return
                    w[semkey] = val
                    h.wait_ge(sem, val)

                for o in self.streams[e]:
                    for d in o.deps:
                        if d.is_dma:
                            wait(("d",) + d.dsem, dsems[d.dsem], d.dval)
                        else:
                            ep = (d.count - 1) // SEM_EPOCH
                            wait((d.eng, d.seg, ep), sems[(d.eng, d.seg, ep)], d.count - ep * SEM_EPOCH)
                    if o.kind == "ifb":
                        v = h.value_load(o.aux, min_val=0, max_val=1)
                        g = h.If(v)
                        g.__enter__()
                        stack.append((g, dict(state["waited"])))
                    elif o.kind == "ife":
                        for d in o.aux:
                            wait(("d",) + d.dsem, dsems[d.dsem], d.dval)
                        g, snap = stack.pop()
                        g.__exit__(None, None, None)
                        state["waited"] = snap
                    elif o.is_dma:
                        if o.dprev > 0:
                            wait(("d",) + o.dsem, dsems[o.dsem], o.dprev)
                        ins = o.fn(h)
                        ins.then_inc(dsems[o.dsem], 16)
                    elif o.fn is not None:
                        ins = o.fn(h)
                        if o.signal:
                            ep = (o.count - 1) // SEM_EPOCH
                            ins.then_inc(sems[(e, o.seg, ep)], 1)

            @block.tensor
            def _(eng):
                run("pe")

            @block.vector
            def _(eng):
                run("dve")

            @block.scalar
            def _(eng):
                run("act")

            @block.gpsimd
            def _(eng):
                run("pool")

            @block.sync
            def _(eng):
                run("sp")


D = 1024
S = 2048
NB = 2
NT = S // 128
NMEM = 256
EPS = 1e-6
NE = 32
DE = 512
C_MLQ, C_MLK, C_MLV, C_MLO, C_MLI, C_MLF = 0, 512, 1024, 2048, 3072, 3076
C_FXQ, C_FXK, C_FXV, C_FXF, C_GML, C_GFX = 3080, 4104, 5128, 6152, 6160, 7184
D_IN = 8208
CG_MIX, CG_XQ, CG_XMEM, CG_MOE, CG_MLH, CB_GML, CB_GFX = 0, 8, 16, 24, 32, 40, 48
R_MLI, R_MLF, R_FXF, R_RG, R_RE = 0, 4, 8, 16, 20
NROWS = 52
DENSE_MOE = False
NTT = NB * NT
XW = 1048
NGT = 11
DEBUG = False


def build_program(n_experts_run=NE):
    nc = bass.Bass("TRN2", target_bir_lowering=False)
    dt = lambda name, shape, dtype=F32, kind="ExternalInput": nc.dram_tensor(name, shape, dtype, kind=kind).ap()
    x_d = dt("x", [NB, S, D])
    mem_d = dt("mem", [NB, NMEM, D])
    w_in_d = dt("w_in", [D, D_IN])
    w_pm_d = dt("w_proj_ml", [D, D])
    w_pf_d = dt("w_proj_fx", [D, D])
    w_out_d = dt("w_out", [D, D])
    w_xq_d = dt("w_xq", [D, D])
    w_xkv_d = dt("w_xkv", [D, 2 * D])
    w_xo_d = dt("w_xo", [D, D])
    w_r_d = dt("w_r", [D, 36])
    w_gate_d = dt("w_gate", [NE * 128, 8 * DE])
    w_up_d = dt("w_up", [NE * 128, 8 * DE])
    w_down_d = dt("w_down", [NE * 128, 4 * D])
    cols_d = dt("cols", [128, 56])
    rows_d = dt("rows", [1, NROWS])
    gfin_d = dt("g_final", [1, D])
    ident_d = dt("ident", [128, 128])
    tri_d = dt("tri", [128, 128])
    out_d = dt("out", [NB, S, D], F32, "ExternalOutput")
    ymlT_d = dt("ymlT_scr", [NB, NT, 128, 1024], BF16, "Internal")
    yfxT_d = dt("yfxT_scr", [NB, 8, 128, S], BF16, "Internal")
    x1_d = dt("x1_scr", [NB, NT, 128, 1024], F32, "Internal")
    x2_d = dt("x2_scr", [NB, NT, 128, 1024], F32, "Internal")
    gmoe_d = dt("g_moe_row", [1, D])
    cgu_d = dt("cgu", [128, 64])
    cd_d = dt("cd", [128, 32])
    NSL = NGT * 512
    dbg_d = dt("dbg", [NB, 128, 64], F32, "ExternalOutput") if DEBUG else None
    xn3tm_d = dt("xn3tm_scr", [NB, NT, 128, 1024], BF16, "Internal")
    Xs_d = dt("xs_scr", [NSL, XW], BF16, "Internal")
    C8s_d = dt("c8s_scr", [NSL, 128], F32, "Internal")
    Ys_d = dt("ys_scr", [NSL, D], F32, "Internal")
    wg_rows, wu_rows, wd_rows = w_gate_d, w_up_d, w_down_d

    P = Prog(nc)
    with contextlib.ExitStack() as es:
        sb = lambda n, s, d: es.enter_context(nc.sbuf_tensor(n, s, d))
        ps = lambda n, s, d: es.enter_context(nc.psum_tensor(n, s, d))
        ASZ = 35840
        ARENA = sb("ARENA", [128, ASZ], F32)
        ARB = ARENA[:, :].bitcast(BF16)
        LG_OFF = ASZ - 576 * NB
        A32 = sb("A32", [128, 8, S], BF16)
        idf = sb("idf", [128, 128], F32)
        idb = sb("idb", [128, 128], BF16)
        trif = sb("trif", [128, 128], F32)
        onesf = sb("onesf", [128, 128], F32)
        selb = sb("selb", [128, 128], BF16)
        strib = sb("strib", [128, 128], BF16)
        onesb = sb("onesb", [128, 128], BF16)
        gmoeB = sb("gmoeB", [128, D], F32)
        cguS = sb("cguS", [128, 64], F32)
        cdS = sb("cdS", [128, 32], F32)
        ARI = ARENA[:, :].bitcast(I32)
        colsS = sb("colsS", [128, 56], F32)
        rowsS = sb("rowsS", [128, NROWS], F32)
        gfinS = sb("gfin", [128, D], F32)
        epsc = sb("epsc", [128, 1], F32)
        onec = sb("onec", [128, 1], F32)
        wrS = sb("wr", [128, 8, 36], F32)
        xt = [sb(f"xt{i}", [128, D], F32) for i in range(2)]
        junk = sb("junk", [128, D], BF16)
        xs_b = [sb(f"xsb{i}", [128, D], BF16) for i in range(2)]
        xs_f = sb("xsf", [128, D], F32)
        st_ssq = [sb(f"ssq{i}", [128, 1], F32) for i in range(2)]
        st_ln = [sb(f"lnv{i}", [128, 1], F32) for i in range(2)]
        st_rs = [sb(f"rstd{i}", [128, 1], F32) for i in range(2)]
        PSF = ps("PSF", [128, 6, 512], F32)
        PSB = ps("PSB", [128, 2, 1024], BF16)

        bA32t = [Buf(f"A32_{i}") for i in range(NT)]
        bconst = Buf("const")
        bxt = [Buf("xt0"), Buf("xt1")]
        bjunk = Buf("junk")
        bxsb = [Buf("xsb0"), Buf("xsb1")]
        bxsf = Buf("xsf")
        bst = [Buf("st0"), Buf("st1")]
        bF = [Buf(f"PSF{i}") for i in range(6)]
        bB = [Buf(f"PSB{i}") for i in range(2)]
        bymlT_d = [[Buf(f"ymlTd{b}_{i}") for i in range(NT)] for b in range(NB)]
        byfxT_d = [[Buf(f"yfxTd{b}_{i}") for i in range(8)] for b in range(NB)]
        bx1_d = [[Buf(f"x1d{b}_{i}") for i in range(NT)] for b in range(NB)]
        bx2_d = [[Buf(f"x2d{b}_{i}") for i in range(NT)] for b in range(NB)]
        bxn3_d = [[Buf(f"xn3d{b}_{i}") for i in range(NT)] for b in range(NB)]
        bXs = [Buf(f"Xs{i}") for i in range(NGT)]
        bC8s = [Buf(f"C8s{i}") for i in range(7)]
        bYs = [Buf(f"Ys{i}") for i in range(NGT)]
        blg = Buf("lg")
        lg = ARENA[:, LG_OFF:LG_OFF + 576 * NB].rearrange("p (t n) -> p t n", n=36)

        def DMA(q, out, in_, r=(), w=(), is_out=False, group=None):
            return P.dma(q, lambda e: e.dma_start(out=out, in_=in_), r, w, is_out, group=group)

        def MM(out, lhsT, rhs, start=True, stop=True, r=(), w=()):
            return P.op("pe", lambda e: e.matmul(out, lhsT=lhsT, rhs=rhs, start=start, stop=stop), r, w)

        def TR(out, in_, r=(), w=()):
            return P.op("pe", lambda e: e.transpose(out=out, in_=in_, identity=idb[:]), list(r) + [bconst], w)

        def ACT(out, in_, func, bias=None, scale=1.0, accum=None, r=(), w=()):
            def f(e):
                kw = {}
                if bias is not None:
                    kw["bias"] = bias
                if accum is not None:
                    kw["accum_out"] = accum
                return e.activation(out=out, in_=in_, func=func, scale=scale, **kw)
            return P.op("act", f, r, w)

        def TS(eng, out, in0, s1, s2, op0, op1=None, r=(), w=()):
            def f(e):
                if op1 is None:
                    return e.tensor_scalar(out=out, in0=in0, scalar1=s1, scalar2=None, op0=op0)
                return e.tensor_scalar(out=out, in0=in0, scalar1=s1, scalar2=s2, op0=op0, op1=op1)
            return P.op(eng, f, r, w)

        def STT(eng, out, in0, scalar, in1, op0, op1, r=(), w=()):
            return P.op(eng, lambda e: e.scalar_tensor_tensor(out=out, in0=in0, scalar=scalar, in1=in1, op0=op0, op1=op1), r, w)

        def TT(eng, out, in0, in1, op, r=(), w=()):
            return P.op(eng, lambda e: e.tensor_tensor(out=out, in0=in0, in1=in1, op=op), r, w)

        def CP(eng, out, in_, r=(), w=()):
            if eng == "act":
                return P.op("act", lambda e: e.copy(out=out, in_=in_), r, w)
            return P.op(eng, lambda e: e.tensor_copy(out=out, in_=in_), r, w)

        def MSET(eng, ap, val, w=()):
            return P.op(eng, lambda e: e.memset(ap, val), (), w)

        def RED(eng, out, in_, op, r=(), w=()):
            return P.op(eng, lambda e: e.tensor_reduce(out=out, in_=in_, axis=AX.X, op=op), r, w)

        def RECIP(out, in_, r=(), w=()):
            return P.op("dve", lambda e: e.reciprocal(out=out, in_=in_), r, w)

        scr_off = [0]
        scr_lim = [LG_OFF]
        phase_bufs = [[]]

        def carve_f(n):
            o = scr_off[0]
            scr_off[0] += n
            assert scr_off[0] <= scr_lim[0], (scr_off[0], scr_lim[0])
            return ARENA[:, o:o + n]

        def carve_b(n):
            assert n % 2 == 0
            o = scr_off[0]
            scr_off[0] += n // 2
            assert scr_off[0] <= scr_lim[0], (scr_off[0], scr_lim[0])
            return ARB[:, 2 * o:2 * o + n]

        def new_phase(names, base, manual=None):
            manual = manual or {}
            bs = {n: Buf(n) for n in names}
            P.inherit([v for n, v in bs.items() if n not in manual], phase_bufs[0])
            for n, olds in manual.items():
                P.inherit([bs[n]], olds)
            phase_bufs[0] = list(bs.values())
            scr_off[0] = base
            return bs

        WBv = lambda off, nk, ncols: ARB[:, off:off + nk * ncols].rearrange("p (k n) -> p k n", n=ncols)
        SCR0 = 24640
        SCR5 = 16384
        last_p6 = [[]]
        keep = {}

        DMA("sp", idf[:], ident_d[:, :], w=[bconst])
        DMA("sp", trif[:], tri_d[:, :], w=[bconst])
        DMA("sp", colsS[:], cols_d[:, :], w=[bconst])
        DMA("sp", rowsS[:], rows_d.partition_broadcast(128), w=[bconst])
        DMA("sp", gfinS[:], gfin_d.partition_broadcast(128), w=[bconst])
        DMA("sp", wrS[:], w_r_d.rearrange("(k p) n -> p k n", p=128), w=[bconst])
        MSET("pool", onesf[:], 1.0, w=[bconst])
        MSET("pool", epsc[:], EPS, w=[bconst])
        MSET("pool", onec[:], 1.0, w=[bconst])
        CP("dve", idb[:], idf[:], r=[bconst], w=[bconst])
        DMA("sp", gmoeB[:], gmoe_d.partition_broadcast(128), w=[bconst])
        DMA("sp", cguS[:], cgu_d[:, :], w=[bconst])
        DMA("sp", cdS[:], cd_d[:, :], w=[bconst])
        TT("dve", strib[:], trif[:], idf[:], ALU.subtract, r=[bconst], w=[bconst])
        CP("dve", onesb[:], onesf[:], r=[bconst], w=[bconst])
        MSET("dve", selb[:], 0.0, w=[bconst])
        for p0 in (0, 32, 64):
            MSET("dve", selb[p0:p0 + 1, :], 1.0, w=[bconst])

        zt = xs_b[0]
        MSET("dve", zt[:], 0.0, w=[bxsb[0]])
        for r0 in range(0, NSL, 128):
            DMA("sp", Xs_d[r0:r0 + 128, 0:1024], zt[:], r=[bxsb[0]], w=bXs, group=("zero",))
            DMA("sp", Xs_d[r0:r0 + 128, 1024:XW], zt[:, 0:XW - 1024], r=[bxsb[0]], w=bXs, group=("zero",))
        nt_ctr = [0]

        def norm_transpose(src, src_bufs, gcol, dstT, dst_bufs, fp32=False, dstT_b=None, dstb_bufs=(), tm_store=None):
            i = nt_ctr[0] % 2
            nt_ctr[0] += 1
            ssq, lnv, rstd = st_ssq[i], st_ln[i], st_rs[i]
            ACT(junk[:], src, AF.Square, accum=ssq[:], r=list(src_bufs), w=[bjunk, bst[i]])
            ACT(lnv[:], ssq[:], AF.Ln, bias=epsc[:, 0:1], scale=1.0 / D, r=[bst[i], bconst], w=[bst[i]])
            ACT(rstd[:], lnv[:], AF.Exp, scale=-0.5, r=[bst[i]], w=[bst[i]])
            gb = gcol.unsqueeze(2).to_broadcast([128, 8, 128])
            if not fp32:
                xs = xs_b[i]
                TS("dve", xs[:], src, rstd[:, 0:1], None, ALU.mult, r=list(src_bufs) + [bst[i]], w=[bxsb[i]])
                for k in range(8):
                    TR(PSB[:, i, k * 128:(k + 1) * 128], xs[:, k * 128:(k + 1) * 128], r=[bxsb[i]], w=[bB[i]])
                pv = PSB[:, i, :].rearrange("p (k t) -> p k t", t=128)
                TT("dve", dstT, pv, gb, ALU.mult, r=[bB[i], bconst], w=list(dst_bufs))
            else:
                TS("dve", xs_f[:], src, rstd[:, 0:1], None, ALU.mult, r=list(src_bufs) + [bst[i]], w=[bxsf])
                for k in range(8):
                    MM(PSF[:, 4 + k // 4, (k % 4) * 128:(k % 4 + 1) * 128], xs_f[:, k * 128:(k + 1) * 128], idf[:],
                       r=[bxsf, bconst], w=[bF[4 + k // 4]])
                pv = PSF[:, 4:6, :].rearrange("p a (c t) -> p (a c) t", t=128)
                TT("dve", dstT, pv, gb, ALU.mult, r=[bF[4], bF[5], bconst], w=list(dst_bufs))
                if dstT_b is not None:
                    CP("act", dstT_b, dstT, r=list(dst_bufs), w=list(dstb_bufs))
                if tm_store is not None:
                    TT("pool", xs_b[i][:], xs_f[:], gmoeB[:], ALU.mult, r=[bxsf, bconst], w=[bxsb[i]])
                    DMA("sp", tm_store[0], xs_b[i][:], r=[bxsb[i]], w=[tm_store[1]])

        grp_ctr = [0]

        def load_w(dst_view, src, wbufs, nk=8):
            grp_ctr[0] += 1
            gid_ = ("lw", grp_ctr[0])
            for k in range(nk):
                P.dma("pool", (lambda o_, i_: (lambda e: e.dma_start(out=o_, in_=i_)))(dst_view[:, k, :], src[k * 128:(k + 1) * 128, :]),
                      (), list(wbufs), group=gid_)

        for b in range(NB):
            def p1_tile(bb, i):
                DMA("sp", xt[i % 2][:], x_d[bb, i * 128:(i + 1) * 128, :], w=[bxt[i % 2]])
                norm_transpose(xt[i % 2][:], [bxt[i % 2]], colsS[:, CG_MIX:CG_MIX + 8],
                               A32[:, :, i * 128:(i + 1) * 128], [bA32t[i]])

            if b == 0:
                for i in range(NT):
                    p1_tile(0, i)

            bS = new_phase("W CnT Cnb vext0 vext1 qT0 qT1 kT0 kT1 kw0 kw1 PT sig0 sig1 yb0 yb1 ymlT gsm gA".split(), SCR0)
            bW = [bS["W"]]
            keep["Wml"] = bS["W"]
            wml = WBv(0, 8, 3080)
            load_w(wml, w_in_d[:, 0:3080], bW)
            bWfx = Buf("Wfx")
            P.inherit([bWfx], last_p6[0])
            wfx = WBv(24640, 8, 3080)
            load_w(wfx, w_in_d[:, C_FXQ:C_FXQ + 3080], [bWfx])
            CnT = carve_f(4 * 258).rearrange("p (h n) -> p h n", n=258)
            Cnb = carve_b(4 * 258).rearrange("p (h n) -> p h n", n=258)
            vext2 = [carve_b(4 * 258).rearrange("p (h n) -> p h n", n=258) for _ in range(2)]
            qT2 = [carve_b(512).rearrange("p (h n) -> p h n", n=128) for _ in range(2)]
            kT2 = [carve_b(512).rearrange("p (h n) -> p h n", n=128) for _ in range(2)]
            kw2 = [carve_b(512).rearrange("p (h n) -> p h n", n=128) for _ in range(2)]
            PT = carve_b(128)
            sig2 = [carve_f(1024) for _ in range(2)]
            yb2 = [carve_b(1024) for _ in range(2)]
            ymlT_t = carve_b(1024).rearrange("p (k t) -> p k t", t=128)
            gsm = carve_f(64)
            d1, sc, sa, t2, tot = gsm[:, 28:32], gsm[:, 32:36], gsm[:, 36:40], gsm[:, 40:44], gsm[:, 44:48]
            gA = carve_f(64 * 7)
            gi_a, l1_a, tmp_a, eb_a, dec_a, wv_a, wd_a = [gA[:, 64 * q:64 * (q + 1)] for q in range(7)]
            G_ = [bS["gsm"]]
            bGA = [bS["gA"]]
            MSET("dve", CnT, 0.0, w=[bS["CnT"]])
            MSET("dve", Cnb, 0.0, w=[bS["Cnb"]])
            for q in range(2):
                MSET("dve", vext2[q], 1.0, w=[bS[f"vext{q}"]])
            for c in range(NT):
                for k in range(8):
                    MM(PSF[:, 5, c * 8:(c + 1) * 8], A32[:, k, c * 128:(c + 1) * 128], wml[:, k, C_MLI:C_MLI + 8], k == 0, k == 7,
                       r=[bA32t[c]] + bW, w=[bF[5]])
            pg3 = PSF[:, 5, 0:128].rearrange("p (t g) -> p t g", g=8)
            v3 = lambda ap: ap.rearrange("p (t h) -> p t h", h=4)
            TT("dve", v3(gi_a), pg3[:, :, 0:4], rowsS[:, R_MLI:R_MLI + 4].unsqueeze(1).to_broadcast([128, 16, 4]), ALU.add,
               r=[bF[5], bconst], w=bGA)
            TT("dve", v3(l1_a), pg3[:, :, 4:8], rowsS[:, R_MLF:R_MLF + 4].unsqueeze(1).to_broadcast([128, 16, 4]), ALU.add,
               r=[bF[5], bconst], w=bGA)
            ACT(l1_a, l1_a, AF.Exp, scale=-1.0, r=bGA, w=bGA)
            ACT(l1_a, l1_a, AF.Ln, bias=onec[:, 0:1], r=bGA + [bconst], w=bGA)
            MM(PSF[:, 4, 0:64], trif[:], l1_a, r=bGA + [bconst], w=[bF[4]])
            MM(PSF[:, 4, 64:128], onesf[:], l1_a, r=bGA + [bconst], w=[bF[4]])
            ACT(eb_a, PSF[:, 4, 0:64], AF.Exp, scale=-1.0, r=[bF[4]], w=bGA)
            ACT(dec_a, PSF[:, 4, 64:128], AF.Exp, scale=-1.0, r=[bF[4]], w=bGA)
            TT("dve", tmp_a, gi_a, PSF[:, 4, 0:64], ALU.add, r=bGA + [bF[4]], w=bGA)
            ACT(wv_a, tmp_a, AF.Exp, r=bGA, w=bGA)
            TT("dve", tmp_a, tmp_a, PSF[:, 4, 64:128], ALU.subtract, r=bGA + [bF[4]], w=bGA)
            ACT(wd_a, tmp_a, AF.Exp, r=bGA, w=bGA)

            def p2_proj(c):
                q = c % 2
                tsl = slice(c * 128, (c + 1) * 128)
                qT, kT, kw, vext, sig = qT2[q], kT2[q], kw2[q], vext2[q], sig2[q]
                wd = wd_a[:, 4 * c:4 * c + 4]
                rA = [bA32t[c]] + bW
                for h in range(4):
                    for k in range(8):
                        MM(PSF[:, 4, h * 128:(h + 1) * 128], wml[:, k, C_MLQ + h * 128:C_MLQ + (h + 1) * 128], A32[:, k, tsl],
                           k == 0, k == 7, r=rA, w=[bF[4]])
                ACT(qT, PSF[:, 4, :].rearrange("p (h n) -> p h n", n=128), AF.Identity, scale=128.0 ** -0.5, r=[bF[4]], w=[bS[f"qT{q}"]])
                for h in range(4):
                    for k in range(8):
                        MM(PSF[:, 5, h * 128:(h + 1) * 128], wml[:, k, C_MLK + h * 128:C_MLK + (h + 1) * 128], A32[:, k, tsl],
                           k == 0, k == 7, r=rA, w=[bF[5]])
                CP("dve", kT, PSF[:, 5, :].rearrange("p (h n) -> p h n", n=128), r=[bF[5]], w=[bS[f"kT{q}"]])
                for k in range(8):
                    MM(PSF[:, 4, :], A32[:, k, tsl], wml[:, k, C_MLK:C_MLK + 512], k == 0, k == 7, r=rA, w=[bF[4]])
                TT("dve", kw, PSF[:, 4, :].rearrange("p (h n) -> p h n", n=128), wd.unsqueeze(2).to_broadcast([128, 4, 128]),
                   ALU.mult, r=[bF[4]] + bGA, w=[bS[f"kw{q}"]])
                for hf in range(2):
                    pb_ = 5 - hf
                    for k in range(8):
                        MM(PSF[:, pb_, :], A32[:, k, tsl], wml[:, k, C_MLV + hf * 512:C_MLV + (hf + 1) * 512], k == 0, k == 7,
                           r=rA, w=[bF[pb_]])
                    CP("act", vext[:, 2 * hf:2 * hf + 2, 0:256], PSF[:, pb_, :].rearrange("p (h n) -> p h n", n=256),
                       r=[bF[pb_]], w=[bS[f"vext{q}"]])
                for hf in range(2):
                    pb_ = 5 - hf
                    for k in range(8):
                        MM(PSF[:, pb_, :], A32[:, k, tsl], wml[:, k, C_MLO + hf * 512:C_MLO + (hf + 1) * 512], k == 0, k == 7,
                           r=rA, w=[bF[pb_]])
                    ACT(sig[:, hf * 512:(hf + 1) * 512], PSF[:, pb_, :], AF.Sigmoid, r=[bF[pb_]], w=[bS[f"sig{q}"]])

            def p2_mix(c):
                q = c % 2
                qT, kT, kw, vext, sig, yb = qT2[q], kT2[q], kw2[q], vext2[q], sig2[q], yb2[q]
                eb, dec, wv_ = eb_a[:, 4 * c:4 * c + 4], dec_a[:, 4 * c:4 * c + 4], wv_a[:, 4 * c:4 * c + 4]
                for h in range(4):
                    MM(PSF[:, 4, 0:128], kT[:, h, :], qT[:, h, :], r=[bS[f"kT{q}"], bS[f"qT{q}"]], w=[bF[4]])
                    STT("dve", PT, PSF[:, 4, 0:128], wv_[:, h:h + 1], trif[:], ALU.mult, ALU.mult,
                        r=[bF[4], bconst] + bGA, w=[bS["PT"]])
                    MM(PSF[:, h, 0:257], PT, vext[:, h, 0:257], True, False, r=[bS["PT"], bS[f"vext{q}"]], w=[bF[h]])
                    MM(PSF[:, h, 0:257], qT[:, h, :], Cnb[:, h, 0:257], False, True, r=[bS[f"qT{q}"], bS["Cnb"]], w=[bF[h]])
                for h in range(4):
                    pb_ = 5 - (h % 2)
                    MM(PSF[:, pb_, 0:257], kw[:, h, :], vext[:, h, 0:257], r=[bS[f"kw{q}"], bS[f"vext{q}"]], w=[bF[pb_]])
                    STT("dve", CnT[:, h, 0:257], CnT[:, h, 0:257], dec[:, h:h + 1], PSF[:, pb_, 0:257], ALU.mult, ALU.add,
                        r=[bS["CnT"], bF[pb_]] + bGA, w=[bS["CnT"]])
                    CP("act", Cnb[:, h, 0:257], CnT[:, h, 0:257], r=[bS["CnT"]], w=[bS["Cnb"]])
                den = PSF[:, 0:4, 256]
                CP("dve", d1, den, r=bF[0:4], w=G_)
                STT("dve", d1, d1, -1.0, d1, ALU.mult, ALU.max, r=G_, w=G_)
                TT("dve", d1, d1, eb, ALU.mult, r=G_ + bGA, w=G_)
                TS("dve", d1, d1, 1.0, None, ALU.max, r=G_, w=G_)
                RECIP(d1, d1, r=G_, w=G_)
                TT("dve", sc, d1, eb, ALU.mult, r=G_ + bGA, w=G_)
                for h in range(4):
                    ACT(junk[:, 0:256], PSF[:, h, 0:256], AF.Square, accum=sa[:, h:h + 1], r=[bF[h]], w=[bjunk] + G_)
                TT("dve", t2, sc, sc, ALU.mult, r=G_, w=G_)
                TT("dve", t2, t2, sa, ALU.mult, r=G_, w=G_)
                ACT(t2, t2, AF.Ln, bias=epsc[:, 0:1], scale=1.0 / 256, r=G_ + [bconst], w=G_)
                ACT(t2, t2, AF.Exp, scale=-0.5, r=G_, w=G_)
                TT("dve", tot, t2, sc, ALU.mult, r=G_, w=G_)
                for h in range(4):
                    STT("dve", yb[:, h * 256:(h + 1) * 256], PSF[:, h, 0:256], tot[:, h:h + 1], sig[:, h * 256:(h + 1) * 256],
                        ALU.mult, ALU.mult, r=[bF[h], bS[f"sig{q}"]] + G_, w=[bS[f"yb{q}"]])

            def p2_store(c):
                q = c % 2
                yb = yb2[q]
                for k in range(8):
                    TR(PSB[:, 0, k * 128:(k + 1) * 128], yb[:, k * 128:(k + 1) * 128], r=[bS[f"yb{q}"]], w=[bB[0]])
                TT("dve", ymlT_t, PSB[:, 0, :].rearrange("p (k t) -> p k t", t=128),
                   colsS[:, CG_MLH:CG_MLH + 8].unsqueeze(2).to_broadcast([128, 8, 128]), ALU.mult,
                   r=[bB[0], bconst], w=[bS["ymlT"]])
                DMA("sp", ymlT_d[b, c, :, :], ymlT_t.rearrange("p k t -> p (k t)"), r=[bS["ymlT"]], w=[bymlT_d[b][c]])

            for c in range(NT + 1):
                if c < NT:
                    p2_proj(c)
                if c >= 1:
                    p2_store(c - 1)
                if c < NT:
                    p2_mix(c)

            bS = new_phase("kTh qTh vxh l1f af cend biasall PT0 PT1 rcol yft0 yft1 yo0 yo1 Rr0 Rr1".split(), SCR0)
            bS["W"] = bWfx
            phase_bufs[0] = phase_bufs[0] + [bWfx]
            bW = [bWfx]
            bW40, bW41 = Buf("W40"), Buf("W41")
            P.inherit([bW40, bW41], [keep["Wml"]])
            wg = WBv(0, 8, 2048)
            wpm = WBv(16384, 8, 1024)
            load_w(wg, w_in_d[:, C_GML:C_GML + 2048], [bW40])
            load_w(wpm, w_pm_d, [bW41])
            kTh = carve_b(2048)
            qTh = carve_b(2048)
            vxh = carve_b(16 * 130).rearrange("p (t n) -> p t n", n=130)
            l1f = carve_f(128)
            af = carve_f(128).rearrange("p (t h) -> p t h", h=8)
            cend = carve_f(128).rearrange("p (t h) -> p t h", h=8)
            ncf = carve_f(128)
            tmpf = carve_f(128)
            Rall = carve_b(128)
            tmpb = carve_b(128)
            Rrow = [carve_b(512).rearrange("p (j t) -> p j t", t=128) for _ in range(2)]
            PTf = [carve_b(512).rearrange("p (j t) -> p j t", t=128) for _ in range(2)]
            rcol = carve_f(4)
            yft = [carve_b(128) for _ in range(2)]
            yfo = [carve_b(2048) for _ in range(2)]
            for i in range(NT):
                for k in range(8):
                    MM(PSF[:, 0, i * 8:(i + 1) * 8], A32[:, k, i * 128:(i + 1) * 128], wfx[:, k, 3072:3080], k == 0, k == 7,
                       r=[bA32t[i]] + bW, w=[bF[0]])
            TT("dve", l1f.rearrange("p (t h) -> p t h", h=8), PSF[:, 0, 0:128].rearrange("p (t h) -> p t h", h=8),
               rowsS[:, R_FXF:R_FXF + 8].unsqueeze(1).to_broadcast([128, 16, 8]), ALU.add, r=[bF[0], bconst], w=[bS["l1f"]])
            ACT(l1f, l1f, AF.Exp, scale=-1.0, r=[bS["l1f"]], w=[bS["l1f"]])
            ACT(l1f, l1f, AF.Ln, bias=onec[:, 0:1], r=[bS["l1f"], bconst], w=[bS["l1f"]])
            MM(PSF[:, 1, 0:128], trif[:], l1f, r=[bS["l1f"], bconst], w=[bF[1]])
            MM(PSF[:, 2, 0:128], onesf[:], l1f, r=[bS["l1f"], bconst], w=[bF[2]])
            pTt = PSF[:, 2, 0:128].rearrange("p (t h) -> p t h", h=8)
            CP("dve", cend[:, 0, :], pTt[:, 0, :], r=[bF[2]], w=[bS["cend"]])
            for m in range(1, NT):
                TT("dve", cend[:, m, :], cend[:, m - 1, :], pTt[:, m, :], ALU.add, r=[bF[2], bS["cend"]], w=[bS["cend"]])
            TT("dve", af, cend, pTt, ALU.subtract, r=[bS["cend"], bF[2]], w=[bS["af"]])
            TT("dve", af, af, PSF[:, 1, 0:128].rearrange("p (t h) -> p t h", h=8), ALU.add, r=[bS["af"], bF[1]], w=[bS["af"]])
            cflat = cend.rearrange("p t h -> p (t h)")
            TS("dve", ncf, cflat, -1.0, None, ALU.mult, r=[bS["cend"]], w=[bS["biasall"]])
            CP("dve", Rall, ncf, r=[bS["biasall"]], w=[bS["biasall"]])
            CP("dve", tmpf, Rall, r=[bS["biasall"]], w=[bS["biasall"]])
            TT("dve", ncf, ncf, tmpf, ALU.subtract, r=[bS["biasall"]], w=[bS["biasall"]])
            CP("dve", Rall[32:64, :], ncf[32:64, :], r=[bS["biasall"]], w=[bS["biasall"]])
            CP("dve", tmpb, ncf, r=[bS["biasall"]], w=[bS["biasall"]])
            CP("dve", tmpf, tmpb, r=[bS["biasall"]], w=[bS["biasall"]])
            TT("dve", ncf, ncf, tmpf, ALU.subtract, r=[bS["biasall"]], w=[bS["biasall"]])
            CP("dve", Rall[64:96, :], ncf[64:96, :], r=[bS["biasall"]], w=[bS["biasall"]])
            Rall3 = Rall.rearrange("p (t h) -> p t h", h=8)
            MSET("dve", vxh, 1.0, w=[bS["vxh"]])
            pti = 0
            for h in range(8):
                yo = yfo[h % 2]
                byo = bS[f"yo{h % 2}"]
                for G in range(4):
                    gsl = slice(G * 512, (G + 1) * 512)
                    for k in range(8):
                        MM(PSF[:, 0, :], wfx[:, k, 1024 + h * 128:1024 + (h + 1) * 128], A32[:, k, gsl], k == 0, k == 7,
                           r=bA32t[4 * G:4 * G + 4] + bW, w=[bF[0]])
                    CP("dve", kTh[:, gsl], PSF[:, 0, :], r=[bF[0]], w=[bS["kTh"]])
                    for k in range(8):
                        MM(PSF[:, 1, :], wfx[:, k, h * 128:(h + 1) * 128], A32[:, k, gsl], k == 0, k == 7,
                           r=bA32t[4 * G:4 * G + 4] + bW, w=[bF[1]])
                    ACT(qTh[:, gsl], PSF[:, 1, :], AF.Identity, scale=128.0 ** -0.5, r=[bF[1]], w=[bS["qTh"]])
                    for ti in range(4):
                        i = G * 4 + ti
                        for k in range(8):
                            MM(PSF[:, 2, ti * 128:(ti + 1) * 128], A32[:, k, i * 128:(i + 1) * 128],
                               wfx[:, k, 2048 + h * 128:2048 + (h + 1) * 128], k == 0, k == 7, r=[bA32t[i]] + bW, w=[bF[2]])
                    CP("dve", vxh[:, G * 4:(G + 1) * 4, 0:128], PSF[:, 2, :].rearrange("p (t n) -> p t n", n=128),
                       r=[bF[2]], w=[bS["vxh"]])
                steps = [(J, kb) for J in range(4) for kb in range(4 * J + 4)]

                def emit_qk(si):
                    J, kb = steps[si]
                    jmin = max(0, kb - 4 * J)
                    nq = 4 - jmin
                    if kb == 0:
                        CP("dve", Rrow[J % 2], Rall3[:, 4 * J:4 * J + 4, h:h + 1].to_broadcast([128, 4, 128]),
                           r=[bS["biasall"]], w=[bS[f"Rr{J % 2}"]])
                    MM(PSF[:, si % 2, 0:nq * 128], kTh[:, kb * 128:(kb + 1) * 128],
                       qTh[:, (4 * J + jmin) * 128:(4 * J + 4) * 128], True, False, r=[bS["kTh"], bS["qTh"]], w=[bF[si % 2]])
                    MM(PSF[:, si % 2, 0:nq * 128], selb[:], Rrow[J % 2].rearrange("p j t -> p (j t)")[:, jmin * 128:512], False, True,
                       r=[bconst, bS[f"Rr{J % 2}"]], w=[bF[si % 2]])

                def emit_rest(si):
                    J, kb = steps[si]
                    jmin = max(0, kb - 4 * J)
                    sb_i = si % 2
                    ptb = PTf[si % 2]
                    ptflat = ptb.rearrange("p j t -> p (j t)")
                    bpt = bS[f"PT{si % 2}"]
                    ACT(ptflat[:, jmin * 128:512], PSF[:, sb_i, 0:(4 - jmin) * 128], AF.Exp,
                        bias=af[:, kb, h:h + 1], r=[bF[sb_i], bS["af"]], w=[bpt])
                    for jj in range(jmin, 4):
                        if kb == 4 * J + jj:
                            TT("pool", ptb[:, jj, :], ptb[:, jj, :], trif[:], ALU.mult, r=[bpt, bconst], w=[bpt])
                    for jj in range(jmin, 4):
                        j = 4 * J + jj
                        MM(PSF[:, 2 + jj, 0:129], ptb[:, jj, :], vxh[:, kb, 0:129], kb == 0, kb == j,
                           r=[bpt, bS["vxh"]], w=[bF[2 + jj]])
                    if kb == 4 * J + 3:
                        for jj in range(4):
                            j = 4 * J + jj
                            RECIP(rcol[:, jj:jj + 1], PSF[:, 2 + jj, 128:129], r=[bF[2 + jj]], w=[bS["rcol"]])
                            TS("dve", yft[jj % 2], PSF[:, 2 + jj, 0:128], rcol[:, jj:jj + 1], None, ALU.mult,
                               r=[bF[2 + jj], bS["rcol"]], w=[bS[f"yft{jj % 2}"]])
                            TR(PSB[:, 1, (jj % 2) * 128:(jj % 2 + 1) * 128], yft[jj % 2], r=[bS[f"yft{jj % 2}"]], w=[bB[1]])
                            CP("act", yo[:, j * 128:(j + 1) * 128], PSB[:, 1, (jj % 2) * 128:(jj % 2 + 1) * 128], r=[bB[1]], w=[byo])

                emit_qk(0)
                for si in range(len(steps)):
                    if si + 1 < len(steps):
                        emit_qk(si + 1)
                    emit_rest(si)
                DMA("sp", yfxT_d[b, h, :, :], yo, r=[byo], w=[byfxT_d[b][h]])

            bS = new_phase("W2 W3 yg0 yg1 yf0 yf1 mT sgA sgB tA x1t0 x1t1".split(), SCR0,
                           manual={"W2": [keep["Wml"], bWfx], "W3": [bWfx], "yg1": [bWfx], "yf1": [bWfx]})
            bS["W0"], bS["W1"] = bW40, bW41
            phase_bufs[0] = phase_bufs[0] + [bW40, bW41]
            wpf = WBv(24576, 8, 1024)
            wo = WBv(32768, 8, 1024)
            load_w(wpf, w_pf_d, [bS["W2"]])
            load_w(wo, w_out_d, [bS["W3"]])
            ymlTg = [carve_b(4096).rearrange("p (i k t) -> p i k t", k=8, t=128),
                     ARB[:, 40960:45056].rearrange("p (i k t) -> p i k t", k=8, t=128)]
            yfxTg = [carve_b(4096).rearrange("p (k t) -> p k t", t=512),
                     ARB[:, 45056:49152].rearrange("p (k t) -> p k t", t=512)]
            mT = carve_b(4096).rearrange("p (k t) -> p k t", t=512)
            sgA = carve_f(512)
            sgB = carve_f(512)
            tA = carve_f(512)
            x1tt = [carve_f(1024) for _ in range(2)]
            for G in range(4):
                gsl = slice(G * 512, (G + 1) * 512)
                yg = ymlTg[G % 2]
                byg = bS[f"yg{G % 2}"]
                yf = yfxTg[G % 2]
                byf = bS[f"yf{G % 2}"]
                for ti in range(4):
                    DMA("sp", yg[:, ti, :, :].rearrange("p k t -> p (k t)"), ymlT_d[b, G * 4 + ti, :, :],
                        r=[bymlT_d[b][G * 4 + ti]], w=[byg])
                DMA("sp", yf, yfxT_d[b, :, :, G * 512:(G + 1) * 512].rearrange("h p t -> p h t"), r=byfxT_d[b], w=[byf])
                for m in range(8):
                    msl = slice(m * 128, (m + 1) * 128)
                    for k in range(8):
                        MM(PSF[:, 0, :], wpm[:, k, msl], yg[:, :, k, :], k == 0, k == 7, r=[bS["W1"], byg], w=[bF[0]])
                    for k in range(8):
                        MM(PSF[:, 1, :], wg[:, k, msl], A32[:, k, gsl], k == 0, k == 7, r=[bS["W0"]] + bA32t[4 * G:4 * G + 4], w=[bF[1]])
                    for k in range(8):
                        MM(PSF[:, 2, :], wpf[:, k, msl], yf[:, k, :], k == 0, k == 7, r=[bS["W2"], byf], w=[bF[2]])
                    for k in range(8):
                        MM(PSF[:, 3, :], wg[:, k, 1024 + m * 128:1024 + (m + 1) * 128], A32[:, k, gsl], k == 0, k == 7,
                           r=[bS["W0"]] + bA32t[4 * G:4 * G + 4], w=[bF[3]])
                    ACT(sgA, PSF[:, 1, :], AF.Sigmoid, bias=colsS[:, CB_GML + m:CB_GML + m + 1], r=[bF[1], bconst], w=[bS["sgA"]])
                    ACT(sgB, PSF[:, 3, :], AF.Sigmoid, bias=colsS[:, CB_GFX + m:CB_GFX + m + 1], r=[bF[3], bconst], w=[bS["sgB"]])
                    TT("dve", tA, PSF[:, 0, :], sgA, ALU.mult, r=[bF[0], bS["sgA"]], w=[bS["tA"]])
                    TT("dve", sgB, PSF[:, 2, :], sgB, ALU.mult, r=[bF[2], bS["sgB"]], w=[bS["sgB"]])
                    TT("pool", mT[:, m, :], tA, sgB, ALU.add, r=[bS["tA"], bS["sgB"]], w=[bS["mT"]])
                for ti in range(4):
                    i = G * 4 + ti
                    DMA("sp", xt[i % 2][:], x_d[b, i * 128:(i + 1) * 128, :], w=[bxt[i % 2]])
                    for hf in range(2):
                        for k in range(8):
                            MM(PSF[:, 4 + hf, :], mT[:, k, ti * 128:(ti + 1) * 128], wo[:, k, hf * 512:(hf + 1) * 512], k == 0, k == 7,
                               r=[bS["mT"], bS["W3"]], w=[bF[4 + hf]])
                        TT("dve", x1tt[i % 2][:, hf * 512:(hf + 1) * 512], xt[i % 2][:, hf * 512:(hf + 1) * 512], PSF[:, 4 + hf, :], ALU.add,
                           r=[bxt[i % 2], bF[4 + hf]], w=[bS[f"x1t{i % 2}"]])
                    DMA("sp", x1_d[b, i, :, :], x1tt[i % 2], r=[bS[f"x1t{i % 2}"]], w=[bx1_d[b][i]])

            bS = new_phase("W0 W1 W3 mnT KT Vx xn2T xqT PT0 PT1 otm x1a0 x1a1 x1b0 x1b1 x2t0 x2t1 xn3f0 xn3f1 rc5".split(), SCR5)
            wxq = WBv(0, 8, 1024)
            wxkv = WBv(8192, 8, 2048)
            wxo = WBv(24576, 8, 1024)
            load_w(wxkv, w_xkv_d, [bS["W1"]])
            load_w(wxq, w_xq_d, [bS["W0"]])
            load_w(wxo, w_xo_d, [bS["W3"]])
            mn_o = scr_off[0]
            mnT = carve_b(2048).rearrange("p (k n) -> p k n", n=256)
            KT = carve_b(2048).rearrange("p (c n) -> p c n", n=256)
            Vx = carve_b(2 * 4 * 258).rearrange("p (t h n) -> p t h n", h=4, n=258)
            xn2T = carve_b(4096).rearrange("p (k t) -> p k t", t=512)
            oT = xn2T
            xqT = carve_b(4096).rearrange("p (k t) -> p k t", t=512)
            PTx = [carve_b(1024).rearrange("p (n t) -> p n t", t=512) for _ in range(2)]
            otm = carve_b(4096).rearrange("p (t f) -> p t f", f=1024)
            x1a_ = [carve_f(1024), ARENA[:, mn_o:mn_o + 1024]]
            bS["x1a1"] = bS["mnT"]
            x1b_ = [carve_f(1024) for _ in range(2)]
            x2t_ = [carve_f(1024) for _ in range(2)]
            xn3f_ = [carve_f(1024).rearrange("p (k t) -> p k t", t=128) for _ in range(2)]
            rc5 = carve_f(4)
            if b == 0:
                P.inherit([blg], phase_bufs[0])
            MSET("dve", Vx, 1.0, w=[bS["Vx"]])
            for mt in range(2):
                DMA("sp", xt[mt][:], mem_d[b, mt * 128:(mt + 1) * 128, :], w=[bxt[mt]])
                norm_transpose(xt[mt][:], [bxt[mt]], colsS[:, CG_XMEM:CG_XMEM + 8], mnT[:, :, mt * 128:(mt + 1) * 128], [bS["mnT"]])
            for c8 in range(8):
                for k in range(8):
                    MM(PSF[:, c8 % 2, 0:256], wxkv[:, k, c8 * 128:(c8 + 1) * 128], mnT[:, k, :], k == 0, k == 7,
                       r=[bS["W1"], bS["mnT"]], w=[bF[c8 % 2]])
                CP("dve", KT[:, c8, :], PSF[:, c8 % 2, 0:256], r=[bF[c8 % 2]], w=[bS["KT"]])
            for mt in range(2):
                for hf in range(2):
                    for k in range(8):
                        MM(PSF[:, 2 + hf, :], mnT[:, k, mt * 128:(mt + 1) * 128], wxkv[:, k, 1024 + hf * 512:1024 + (hf + 1) * 512],
                           k == 0, k == 7, r=[bS["W1"], bS["mnT"]], w=[bF[2 + hf]])
                    CP("act", Vx[:, mt, 2 * hf:2 * hf + 2, 0:256], PSF[:, 2 + hf, :].rearrange("p (h n) -> p h n", n=256),
                       r=[bF[2 + hf]], w=[bS["Vx"]])
            pti = 0
            for G in range(4):
                for ti in range(4):
                    i = G * 4 + ti
                    DMA("sp", x1a_[ti % 2], x1_d[b, i, :, :], r=[bx1_d[b][i]], w=[bS[f"x1a{ti % 2}"]])
                    norm_transpose(x1a_[ti % 2], [bS[f"x1a{ti % 2}"]], colsS[:, CG_XQ:CG_XQ + 8], xn2T[:, :, ti * 128:(ti + 1) * 128], [bS["xn2T"]])
                for c8 in range(8):
                    for k in range(8):
                        MM(PSF[:, c8 % 2, :], wxq[:, k, c8 * 128:(c8 + 1) * 128], xn2T[:, k, :], k == 0, k == 7,
                           r=[bS["W0"], bS["xn2T"]], w=[bF[c8 % 2]])
                    ACT(xqT[:, c8, :], PSF[:, c8 % 2, :], AF.Identity, scale=1.0 / 16.0, r=[bF[c8 % 2]], w=[bS["xqT"]])
                for h in range(4):
                    ptb = PTx[pti % 2]
                    bpt = bS[f"PT{pti % 2}"]
                    pti += 1
                    for nt_ in range(2):
                        for c2 in range(2):
                            MM(PSF[:, nt_, :], KT[:, 2 * h + c2, nt_ * 128:(nt_ + 1) * 128], xqT[:, 2 * h + c2, :], c2 == 0, c2 == 1,
                               r=[bS["KT"], bS["xqT"]], w=[bF[nt_]])
                        ACT(ptb[:, nt_, :], PSF[:, nt_, :], AF.Exp, r=[bF[nt_]], w=[bpt])
                    for ti in range(4):
                        pb = 2 + (ti % 2)
                        for nt_ in range(2):
                            MM(PSF[:, pb, 0:257], ptb[:, nt_, ti * 128:(ti + 1) * 128], Vx[:, nt_, h, 0:257], nt_ == 0, nt_ == 1,
                               r=[bpt, bS["Vx"]], w=[bF[pb]])
                        RECIP(rc5[:, 0:1], PSF[:, pb, 256:257], r=[bF[pb]], w=[bS["rc5"]])
                        TS("dve", otm[:, ti, h * 256:(h + 1) * 256], PSF[:, pb, 0:256], rc5[:, 0:1], None, ALU.mult,
                           r=[bF[pb], bS["rc5"]], w=[bS["otm"]])
                if b + 1 < NB:
                    for ti in range(4):
                        p1_tile(b + 1, G * 4 + ti)
                for ti in range(4):
                    for k in range(8):
                        TR(PSB[:, ti % 2, k * 128:(k + 1) * 128], otm[:, ti, k * 128:(k + 1) * 128], r=[bS["otm"]], w=[bB[ti % 2]])
                    CP("act", oT[:, :, ti * 128:(ti + 1) * 128], PSB[:, ti % 2, :].rearrange("p (k t) -> p k t", t=128),
                       r=[bB[ti % 2]], w=[bS["xn2T"]])
                def e_mm(ti):
                    i = G * 4 + ti
                    x1b, x2t = x1b_[ti % 2], x2t_[ti % 2]
                    bx1b, bx2t = bS[f"x1b{ti % 2}"], bS[f"x2t{ti % 2}"]
                    DMA("sp", x1b, x1_d[b, i, :, :], r=[bx1_d[b][i]], w=[bx1b])
                    for hf in range(2):
                        for k in range(8):
                            MM(PSF[:, 2 + hf, :], oT[:, k, ti * 128:(ti + 1) * 128], wxo[:, k, hf * 512:(hf + 1) * 512], k == 0, k == 7,
                               r=[bS["xn2T"], bS["W3"]], w=[bF[2 + hf]])
                        TT("dve", x2t[:, hf * 512:(hf + 1) * 512], x1b[:, hf * 512:(hf + 1) * 512], PSF[:, 2 + hf, :], ALU.add,
                           r=[bx1b, bF[2 + hf]], w=[bx2t])
                    DMA("sp", x2_d[b, i, :, :], x2t, r=[bx2t], w=[bx2_d[b][i]])

                def e_tail(ti):
                    i = G * 4 + ti
                    x2t, xn3f = x2t_[ti % 2], xn3f_[ti % 2]
                    bx2t, bxn3f = bS[f"x2t{ti % 2}"], bS[f"xn3f{ti % 2}"]
                    norm_transpose(x2t, [bx2t], colsS[:, CG_MOE:CG_MOE + 8], xn3f, [bxn3f], fp32=True,
                                   tm_store=(xn3tm_d[b, i, :, :], bxn3_d[b][i]))
                    for k in range(8):
                        MM(PSF[:, 0, 0:36], xn3f[:, k, :], wrS[:, k, :], k == 0, k == 7, r=[bxn3f, bconst], w=[bF[0]])
                    TT("dve", lg[:, b * NT + i, :], PSF[:, 0, 0:36], rowsS[:, R_RG:R_RG + 36], ALU.add, r=[bF[0], bconst], w=[blg])

                e_mm(0)
                for ti in range(4):
                    if ti + 1 < 4:
                        e_mm(ti + 1)
                    e_tail(ti)

            phase_bufs[0] = phase_bufs[0] + [blg]
            last_p6[0] = list(phase_bufs[0])
        b = None
        names6 = ("GU0 GU1 GU2 DN0 DN1 route srt idx xst0 xst1 xst2 xg0 xg1 c8d0 c8d1 xT he0 he1 sg0 sg1 ys").split()
        bS = new_phase(names6, 16384)
        gmax = carve_f(NTT)
        goh = carve_f(NTT * 4).rearrange("p (t g) -> p t g", g=4)
        gex = carve_f(NTT * 4).rearrange("p (t g) -> p t g", g=4)
        gp = carve_f(NTT)
        tmp32_o = scr_off[0]
        tmp32 = carve_f(NTT * 32).rearrange("p (t g e) -> p t g e", g=4, e=8)
        esel = carve_f(NTT * 8).rearrange("p (t e) -> p t e", e=8)
        m1 = carve_f(NTT)
        m2 = carve_f(NTT)
        oh1 = carve_f(NTT * 8).rearrange("p (t e) -> p t e", e=8)
        oh2 = carve_f(NTT * 8).rearrange("p (t e) -> p t e", e=8)
        msk = carve_f(NTT * 8).rearrange("p (t e) -> p t e", e=8)
        w1 = carve_f(NTT)
        w2 = carve_f(NTT)
        c8t = carve_f(NTT * 8).rearrange("p (t e) -> p t e", e=8)
        bR = [bS["route"]]
        bc16 = lambda ap, n: ap.unsqueeze(2).to_broadcast([128, NTT, n])
        gl = lg[:, :, 0:4]
        RED("dve", gmax, gl, ALU.max, r=[blg], w=bR)
        TT("dve", goh, gl, bc16(gmax, 4), ALU.is_equal, r=[blg] + bR, w=bR)
        TT("dve", gex, gl, bc16(gmax, 4), ALU.subtract, r=[blg] + bR, w=bR)
        ACT(gex, gex, AF.Exp, r=bR, w=bR)
        RED("dve", gp, gex, ALU.add, r=bR, w=bR)
        RECIP(gp, gp, r=bR, w=bR)
        el = lg[:, :, 4:36].rearrange("p t (g e) -> p t g e", e=8)
        TT("dve", tmp32, el, goh.unsqueeze(3).to_broadcast([128, NTT, 4, 8]), ALU.mult, r=[blg] + bR, w=bR)
        RED("dve", esel, tmp32.rearrange("p t g e -> p t e g"), ALU.add, r=bR, w=bR)
        RED("dve", m1, esel, ALU.max, r=bR, w=bR)
        TT("dve", oh1, esel, bc16(m1, 8), ALU.is_equal, r=bR, w=bR)
        STT("dve", msk, oh1, -1e30, esel, ALU.mult, ALU.add, r=bR, w=bR)
        RED("dve", m2, msk, ALU.max, r=bR, w=bR)
        TT("dve", oh2, msk, bc16(m2, 8), ALU.is_equal, r=bR, w=bR)
        TT("dve", w2, m2, m1, ALU.subtract, r=bR, w=bR)
        ACT(w2, w2, AF.Exp, r=bR, w=bR)
        TS("dve", w2, w2, 1.0, None, ALU.add, r=bR, w=bR)
        RECIP(w1, w2, r=bR, w=bR)
        TT("dve", w1, w1, gp, ALU.mult, r=bR, w=bR)
        TT("dve", w2, gp, w1, ALU.subtract, r=bR, w=bR)
        TT("dve", c8t, oh1, bc16(w1, 8), ALU.mult, r=bR, w=bR)
        TT("dve", oh2, oh2, bc16(w2, 8), ALU.mult, r=bR, w=bR)
        TT("dve", c8t, c8t, oh2, ALU.add, r=bR, w=bR)
        bT = [bS["srt"]]
        gohb = carve_b(NTT * 4)
        pre = carve_f(NTT * 4).rearrange("p (t g) -> p t g", g=4)
        posall = carve_f(NTT * 4).rearrange("p (t g) -> p t g", g=4)
        ntot = carve_f(4)
        cnt = carve_f(4)
        tmp4 = carve_f(4)
        endg = carve_f(4)
        baseg = carve_f(4)
        pos_f = carve_f(NTT)
        pos_o = scr_off[0]
        pos_i = ARI[:, pos_o:pos_o + NTT]
        carve_f(NTT)
        gid = carve_f(NGT)
        g8k = carve_f(NGT)
        g4k = carve_f(NGT)
        idxf = carve_f(8)
        io = scr_off[0]
        idx_e = ARI[:, io:io + NGT * 8].rearrange("p (s n) -> p s n", n=8)
        carve_f(NGT * 8)
        CP("dve", gohb, goh.rearrange("p t g -> p (t g)"), r=bR, w=bT)
        MM(PSF[:, 0, 0:NTT * 4], strib[:], gohb, r=bT + [bconst], w=[bF[0]])
        MM(PSF[:, 1, 0:NTT * 4], onesb[:], gohb, r=bT + [bconst], w=[bF[1]])
        Lp = PSF[:, 0, 0:NTT * 4].rearrange("p (t g) -> p t g", g=4)
        Tp = PSF[:, 1, 0:NTT * 4].rearrange("p (t g) -> p t g", g=4)
        MSET("dve", pre[:, 0, :], 0.0, w=bT)
        for i in range(1, NTT):
            TT("dve", pre[:, i, :], pre[:, i - 1, :], Tp[:, i - 1, :], ALU.add, r=bT + [bF[1]], w=bT)
        TT("dve", ntot, pre[:, NTT - 1, :], Tp[:, NTT - 1, :], ALU.add, r=bT + [bF[1]], w=bT)
        TS("dve", cnt, ntot, 0.0, None, ALU.is_gt, r=bT, w=bT)
        for q in range(1, 8):
            TS("dve", tmp4, ntot, 512.0 * q, None, ALU.is_gt, r=bT, w=bT)
            TT("dve", cnt, cnt, tmp4, ALU.add, r=bT, w=bT)
        TS("dve", cnt, cnt, 512.0, None, ALU.mult, r=bT, w=bT)
        CP("dve", endg[:, 0:1], cnt[:, 0:1], r=bT, w=bT)
        for g in (1, 2, 3):
            TT("dve", endg[:, g:g + 1], endg[:, g - 1:g], cnt[:, g:g + 1], ALU.add, r=bT, w=bT)
        TT("dve", baseg, endg, cnt, ALU.subtract, r=bT, w=bT)
        TT("dve", posall, pre, Lp, ALU.add, r=bT + [bF[0]], w=bT)
        TT("dve", posall, posall, baseg.unsqueeze(1).to_broadcast([128, NTT, 4]), ALU.add, r=bT, w=bT)
        TT("dve", posall, posall, goh, ALU.mult, r=bT + bR, w=bT)
        RED("dve", pos_f, posall, ALU.add, r=bT, w=bT)
        CP("dve", pos_i, pos_f, r=bT, w=bT)
        for sidx in range(NGT):
            TS("dve", tmp4, endg, 512.0 * sidx, None, ALU.is_le, r=bT, w=bT)
            RED("dve", gid[:, sidx:sidx + 1], tmp4, ALU.add, r=bT, w=bT)
        TS("dve", gid, gid, 3.0, None, ALU.min, r=bT, w=bT)
        TS("dve", g8k, gid, 1024.0, None, ALU.mult, r=bT, w=bT)
        bI = [bS["idx"]]
        for sidx in range(NGT):
            TS("dve", idxf, cguS[:, 0:8], g8k[:, sidx:sidx + 1], None, ALU.add, r=bT + [bconst], w=bT)
            CP("dve", idx_e[:, sidx, :], idxf, r=bT, w=bI)
        if DEBUG:
            dbt = carve_f(64)
            MSET("dve", dbt, 0.0, w=bT)
            CP("dve", dbt[:, 0:16], pos_f, r=bT, w=bT)
            CP("dve", dbt[:, 16:24], gid, r=bT, w=bT)
            CP("dve", dbt[:, 24:28], endg, r=bT, w=bT)
            CP("dve", dbt[:, 28:32], ntot, r=bT, w=bT)
            CP("dve", dbt[:, 32:48], pos_i, r=bT, w=bT)
            DMA("sp", dbg_d[0, :, :], dbt, r=bT, is_out=True)
        c8h = ARENA[:, tmp32_o:tmp32_o + NTT * 8]
        c8r = ARENA[:, tmp32_o + NTT * 8:tmp32_o + NTT * 16]
        c8p = ARB[:, 2 * (tmp32_o + NTT * 16):2 * (tmp32_o + NTT * 16) + NTT * 24].rearrange("p (t q e) -> p t q e", q=3, e=8)
        c8flat = c8t.rearrange("p t e -> p (t e)")
        c8rv = c8r.rearrange("p (t e) -> p t e", e=8)
        c8hv = c8h.rearrange("p (t e) -> p t e", e=8)
        CP("dve", c8p[:, :, 0, :], c8t, r=bR, w=bT)
        CP("dve", c8hv, c8p[:, :, 0, :], r=bT, w=bT)
        TT("dve", c8r, c8flat, c8h, ALU.subtract, r=bR + bT, w=bT)
        CP("dve", c8p[:, :, 1, :], c8rv, r=bT, w=bT)
        CP("dve", c8hv, c8p[:, :, 1, :], r=bT, w=bT)
        TT("dve", c8r, c8r, c8h, ALU.subtract, r=bT, w=bT)
        CP("dve", c8p[:, :, 2, :], c8rv, r=bT, w=bT)
        xst = [carve_b(XW) for _ in range(2)]
        for i in range(NTT):
            bx = bS[f"xst{i % 2}"]
            DMA("sp", xst[i % 2][:, 0:1024], xn3tm_d[i // NT, i % NT, :, :], r=[bxn3_d[i // NT][i % NT]], w=[bx])
            CP("dve", xst[i % 2][:, 1024:1048], c8p[:, i, :, :].rearrange("p q e -> p (q e)"), r=bT, w=[bx])
            P.dma("pool", (lambda xs_, col: (lambda e: e.indirect_dma_start(
                out=Xs_d[:, :], out_offset=bass.IndirectOffsetOnAxis(ap=pos_i[:, col:col + 1], axis=0),
                in_=xs_, in_offset=None, bounds_check=NSL - 1, oob_is_err=False)))(xst[i % 2], i),
                [bx] + bT, bXs, group=("scatter",))
        fin_base = scr_off[0]
        xg2 = [carve_b(4 * XW).rearrange("p (c f) -> p c f", f=XW) for _ in range(2)]
        c8s2 = [carve_f(32).rearrange("p (c e) -> p c e", e=8) for _ in range(2)]
        xTs = carve_b(4096).rearrange("p (k t) -> p k t", t=512)
        heT = [carve_b(2048).rearrange("p (f t) -> p f t", t=512) for _ in range(2)]
        sgm = [carve_f(512) for _ in range(2)]
        ys = carve_f(4096).rearrange("p (c f) -> p c f", f=D)
        bhe = [bS["he0"], bS["he1"]]
        bsg = [bS["sg0"], bS["sg1"]]
        bGU = [bS["GU0"], bS["GU1"], bS["GU2"]]
        bDN = [bS["DN0"], bS["DN1"]]
        bxg = [bS["xg0"], bS["xg1"]]
        units = [(sidx, j) for sidx in range(NGT) for j in range(8)]
        NU = len(units)

        def gather_gu(u):
            sidx, j = units[u]
            g_ = u % 3
            grp_ctr[0] += 1
            for (dst_, rows_) in ((WBv(g_ * 8192, 8, 512), wg_rows), (WBv(g_ * 8192 + 4096, 8, 512), wu_rows)):
                P.dma("pool", (lambda o_, r_, ix_: (lambda e: e.indirect_dma_start(
                    out=o_, out_offset=None, in_=r_, in_offset=bass.IndirectOffsetOnAxis(ap=ix_, axis=0))))(
                        dst_.rearrange("p k n -> p (k n)"), rows_[:, :], idx_e[:, sidx, j:j + 1]),
                    bI, [bGU[g_]], group=("gu", grp_ctr[0]))

        def gather_dn(u):
            sidx, j = units[u]
            d_ = u % 2
            grp_ctr[0] += 1
            P.dma("pool", (lambda o_, r_, ix_: (lambda e: e.indirect_dma_start(
                out=o_, out_offset=None, in_=r_, in_offset=bass.IndirectOffsetOnAxis(ap=ix_, axis=0))))(
                    WBv(24576 + d_ * 4096, 4, 1024).rearrange("p k n -> p (k n)"), wd_rows[:, :], idx_e[:, sidx, j:j + 1]),
                bI, [bDN[d_]], group=("dn", grp_ctr[0]))

        def prologue(sidx):
            xg = xg2[sidx % 2]
            DMA("sp", xg, Xs_d[sidx * 512:(sidx + 1) * 512, :].rearrange("(c p) f -> p c f", p=128), r=[bXs[sidx]], w=[bxg[sidx % 2]])
            for c in range(4):
                for k in range(8):
                    TR(PSB[:, c % 2, k * 128:(k + 1) * 128], xg[:, c, k * 128:(k + 1) * 128], r=[bxg[sidx % 2]], w=[bB[c % 2]])
                CP("act" if c % 2 == 0 else "dve", xTs[:, :, c * 128:(c + 1) * 128],
                   PSB[:, c % 2, :].rearrange("p (k t) -> p k t", t=128), r=[bB[c % 2]], w=[bS["xT"]])
            pcs = xg[:, :, 1024:1048].rearrange("p c (q e) -> p c q e", e=8)
            c8d = c8s2[sidx % 2]
            TT("dve", c8d, pcs[:, :, 0, :], pcs[:, :, 1, :], ALU.add, r=[bxg[sidx % 2]], w=[bS[f"c8d{sidx % 2}"]])
            TT("dve", c8d, c8d, pcs[:, :, 2, :], ALU.add, r=[bxg[sidx % 2], bS[f"c8d{sidx % 2}"]], w=[bS[f"c8d{sidx % 2}"]])

        def emit_gu(u):
            g_ = u % 3
            wge = WBv(g_ * 8192, 8, 512)
            wue = WBv(g_ * 8192 + 4096, 8, 512)
            he, bh = heT[u % 2], bhe[u % 2]
            for fc in range(4):
                fsl = slice(fc * 128, (fc + 1) * 128)
                pg, pu = (fc % 2) * 2, (fc % 2) * 2 + 1
                for k in range(8):
                    MM(PSF[:, pg, :], wge[:, k, fsl], xTs[:, k, :], k == 0, k == 7, r=[bGU[g_], bS["xT"]], w=[bF[pg]])
                for k in range(8):
                    MM(PSF[:, pu, :], wue[:, k, fsl], xTs[:, k, :], k == 0, k == 7, r=[bGU[g_], bS["xT"]], w=[bF[pu]])
                ACT(sgm[fc % 2], PSF[:, pg, :], AF.Silu, r=[bF[pg]], w=[bsg[fc % 2]])
                TT("dve", he[:, fc, :], sgm[fc % 2], PSF[:, pu, :], ALU.mult, r=[bsg[fc % 2], bF[pu]], w=[bh])

        def emit_down(u):
            sidx, j = units[u]
            d_ = u % 2
            wde = WBv(24576 + d_ * 4096, 4, 1024)
            he, bh = heT[u % 2], bhe[u % 2]
            c8s = c8s2[sidx % 2]
            for c in range(4):
                for hf in range(2):
                    for fc in range(4):
                        MM(PSF[:, 4 + hf, :], he[:, fc, c * 128:(c + 1) * 128], wde[:, fc, hf * 512:(hf + 1) * 512],
                           fc == 0, fc == 3, r=[bh, bDN[d_]], w=[bF[4 + hf]])
                    ysl = ys[:, c, hf * 512:(hf + 1) * 512]
                    if j == 0:
                        TS("dve", ysl, PSF[:, 4 + hf, :], c8s[:, c, 0:1], None, ALU.mult, r=[bF[4 + hf], bS[f"c8d{sidx % 2}"]], w=[bS["ys"]])
                    else:
                        STT("dve", ysl, PSF[:, 4 + hf, :], c8s[:, c, j:j + 1], ysl, ALU.mult, ALU.add,
                            r=[bF[4 + hf], bS[f"c8d{sidx % 2}"], bS["ys"]], w=[bS["ys"]])
            if j == 7:
                DMA("sp", Ys_d[sidx * 512:(sidx + 1) * 512, :].rearrange("(c p) f -> p c f", p=128), ys, r=[bS["ys"]], w=[bYs[sidx]])

        gather_gu(0)
        gather_dn(0)
        gather_gu(1)
        gather_dn(1)
        prologue(0)
        emit_gu(0)
        for u in range(NU):
            if u + 2 < NU:
                gather_gu(u + 2)
            if u + 1 < NU:
                if units[u + 1][0] != units[u][0]:
                    prologue(units[u + 1][0])
                emit_gu(u + 1)
            emit_down(u)
            if u + 2 < NU:
                gather_dn(u + 2)
        fin_b = [Buf(f"yg{q}") for q in range(4)] + [Buf(f"x2f{q}") for q in range(4)]
        P.inherit(fin_b, [bxg[0], bxg[1], bS["xT"], bhe[0], bhe[1], bsg[0], bsg[1]])
        phase_bufs[0] = phase_bufs[0] + fin_b
        scr_off[0] = fin_base
        ygt = [carve_f(1024) for _ in range(4)]
        x2f = [carve_f(1024) for _ in range(4)]
        fst = [carve_f(4) for _ in range(4)]
        def fin_load(i):
            q = i % 4
            byg, bx2 = fin_b[q], fin_b[4 + q]
            P.dma("pool", (lambda o_, col: (lambda e: e.indirect_dma_start(
                out=o_, out_offset=None, in_=Ys_d[:, :],
                in_offset=bass.IndirectOffsetOnAxis(ap=pos_i[:, col:col + 1], axis=0))))(ygt[q], i),
                bYs + bT, [byg])
            DMA("sp", x2f[q], x2_d[i // NT, i % NT, :, :], r=[bx2_d[i // NT][i % NT]], w=[bx2])

        def fin_compute(i):
            q = i % 4
            byg, bx2 = fin_b[q], fin_b[4 + q]
            TT("dve", x2f[q], x2f[q], ygt[q], ALU.add, r=[bx2, byg], w=[bx2])
            ACT(junk[:], x2f[q], AF.Square, accum=fst[q][:, 0:1], r=[bx2], w=[bjunk, bfst[q]])
            ACT(fst[q][:, 1:2], fst[q][:, 0:1], AF.Ln, bias=epsc[:, 0:1], scale=1.0 / D, r=[bfst[q], bconst], w=[bfst[q]])
            ACT(fst[q][:, 2:3], fst[q][:, 1:2], AF.Exp, scale=-0.5, r=[bfst[q]], w=[bfst[q]])
            STT("dve", x2f[q], x2f[q], fst[q][:, 2:3], gfinS[:], ALU.mult, ALU.mult, r=[bx2, bfst[q], bconst], w=[bx2])
            DMA("sp", out_d[i // NT, (i % NT) * 128:(i % NT + 1) * 128, :], x2f[q], r=[bx2], is_out=True)

        bfst = [Buf(f"fst{q}") for q in range(4)]
        P.inherit(bfst, [bxg[0], bxg[1], bS["xT"], bhe[0], bhe[1], bsg[0], bsg[1]])
        phase_bufs[0] = phase_bufs[0] + bfst
        for i in range(NTT + 3):
            if i < NTT:
                fin_load(i)
            if i >= 3:
                fin_compute(i - 3)
        P.emit()
    return nc


_CACHE = {}
_p = np.arange(128, dtype=np.float32)[:, None]
_CGU = np.zeros((128, 64), np.float32)
_CGU[:, 0:8] = np.arange(8, dtype=np.float32)[None, :] * 128 + _p
_CD = np.ascontiguousarray((np.arange(8, dtype=np.float32)[None, :, None] * 512 + np.arange(4, dtype=np.float32)[None, None, :] * 128
                            + _p[:, :, None]).reshape(128, 32).astype(np.float32))


def _host_consts():
    ident = np.eye(128, dtype=np.float32)
    tri = np.triu(np.ones((128, 128), np.float32))
    return ident, tri


def kernel(x, mem, g_mix, w_in, b_ml_i, b_ml_f, b_fx_f, b_gate_ml, b_gate_fx, g_ml_head,
           w_proj_ml, w_proj_fx, w_out, g_xq, g_xmem, w_xq, w_xkv, w_xo, g_moe,
           w_rg, b_rg, w_re, b_re, w_gate, w_up, w_down, g_final):
    f = lambda a: np.ascontiguousarray(np.asarray(a, dtype=np.float32))
    x = f(x)
    mem = f(mem)
    col = lambda v: f(v).reshape(8, 128).T
    cols = np.ascontiguousarray(np.concatenate(
        [col(g_mix[0]), col(g_xq[0]), col(g_xmem[0]), col(g_moe[0]), col(g_ml_head[0]),
         col(b_gate_ml[0]), col(b_gate_fx[0])], axis=1))
    rows = np.ascontiguousarray(np.concatenate(
        [f(b_ml_i[0]), f(b_ml_f[0]), f(b_fx_f[0]), f(b_rg[0]), f(b_re[0])])[None, :])
    w_r = np.ascontiguousarray(np.concatenate([f(w_rg[0]), f(w_re[0])], axis=1))
    ident, tri = _host_consts()
    shared = dict(
        w_in=f(w_in[0]), w_proj_ml=f(w_proj_ml[0]), w_proj_fx=f(w_proj_fx[0]), w_out=f(w_out[0]),
        w_xq=f(w_xq[0]), w_xkv=f(w_xkv[0]), w_xo=f(w_xo[0]), w_r=w_r,
        w_gate=np.ascontiguousarray(f(w_gate[0]).reshape(NE, 8, 128, DE).transpose(0, 2, 1, 3)).reshape(NE * 128, 8 * DE),
        w_up=np.ascontiguousarray(f(w_up[0]).reshape(NE, 8, 128, DE).transpose(0, 2, 1, 3)).reshape(NE * 128, 8 * DE),
        w_down=np.ascontiguousarray(f(w_down[0]).reshape(NE, 4, 128, D).transpose(0, 2, 1, 3)).reshape(NE * 128, 4 * D),
        cols=cols, rows=rows, g_final=f(g_final)[None, :], ident=ident, tri=tri,
        g_moe_row=f(g_moe[0])[None, :], cgu=_CGU, cd=_CD)
    if "nc" not in _CACHE:
        _CACHE["nc"] = build_program()
    nc = _CACHE["nc"]
    in_maps = []
    for c in range(8):
        m = dict(shared)
        m["x"] = np.ascontiguousarray(x[2 * c:2 * c + 2])
        m["mem"] = np.ascontiguousarray(mem[2 * c:2 * c + 2])
        in_maps.append(m)
    res = run_bass_kernel_spmd(nc, in_maps, core_ids=list(range(8)))
    out = np.concatenate([np.asarray(r["out"]) for r in res.results], axis=0)
    return out.astype(np.float32)
```

```python
import contextlib
import numpy as np
import concourse.bass as bass
import concourse.mybir as mybir
from concourse.bass_utils import run_bass_kernel_spmd

F32 = mybir.dt.float32
BF16 = mybir.dt.bfloat16
I32 = mybir.dt.int32
AF = mybir.ActivationFunctionType
ALU = mybir.AluOpType
AX = mybir.AxisListType

ENGS = ("pe", "dve", "act", "pool", "sp")
SEM_EPOCH = 20000
NDMA_SEMS = 12
STRICT_SAME_ENGINE = True


class Buf:
    __slots__ = ("name", "last_w", "readers", "wgroup")

    def __init__(self, name):
        self.name = name
        self.last_w = []
        self.readers = []
        self.wgroup = None


class Op:
    __slots__ = ("eng", "fn", "deps", "is_dma", "idx", "signal", "count", "dsem", "dval", "dprev", "seg", "kind", "aux")

    def __init__(self, eng, fn, is_dma):
        self.eng = eng
        self.fn = fn
        self.deps = []
        self.is_dma = is_dma
        self.idx = -1
        self.signal = False
        self.count = 0
        self.dsem = -1
        self.dval = 0
        self.dprev = 0
        self.seg = 0
        self.kind = "op"
        self.aux = None


class Prog:
    def __init__(self, nc):
        self.nc = nc
        self.streams = {e: [] for e in ENGS}
        self.ndma = {}
        self.dma_uses = {}
        self.out_dmas = []
        self.seg = 0
        self.cond_segs = set()
        self.markers = {}
        self.block_dmas = None

    def _redirect(self, d):
        if d.seg in self.cond_segs and d.seg != self.seg:
            return self.markers[d.seg][d.eng]
        return d

    def _add(self, eng, fn, reads, writes, is_dma, kind="op", group=None):
        op = Op(eng, fn, is_dma)
        op.seg = self.seg
        op.kind = kind
        st = self.streams[eng]
        op.idx = len(st)
        deps = []
        for b in reads:
            if b is not None:
                for w_ in b.last_w:
                    deps.append((w_, "raw"))
        for b in writes:
            if b is None:
                continue
            if not (group is not None and b.wgroup == group):
                for w_ in b.last_w:
                    deps.append((w_, "waw"))
            for r in b.readers:
                deps.append((r, "war"))
        latest = {}
        dma_deps = {}
        for (d, kind_) in deps:
            d = self._redirect(d)
            if d is op:
                continue
            if d.is_dma:
                dma_deps[id(d)] = d
                continue
            if d.eng == eng and not is_dma:
                if eng == "pe":
                    continue
                if not STRICT_SAME_ENGINE and (kind_ != "raw" or (op.idx - d.idx) > 2):
                    continue
            cur = latest.get(d.eng)
            if cur is None or d.idx > cur.idx:
                latest[d.eng] = d
        for d in list(latest.values()) + list(dma_deps.values()):
            op.deps.append(d)
            d.signal = True
        for b in reads:
            if b is not None:
                b.readers.append(op)
        for b in writes:
            if b is not None:
                if group is not None and b.wgroup == group:
                    b.last_w.append(op)
                else:
                    b.last_w = [op]
                    b.wgroup = group
                b.readers = []
        if is_dma:
            n = self.ndma.get((eng, self.seg), 0)
            k = n % NDMA_SEMS
            self.ndma[(eng, self.seg)] = n + 1
            key = (eng, self.seg, k)
            u = self.dma_uses.get(key, 0)
            op.dsem = key
            op.dprev = 16 * u
            op.dval = 16 * (u + 1)
            self.dma_uses[key] = u + 1
            if self.block_dmas is not None:
                self.block_dmas[eng].append(op)
        st.append(op)
        return op

    def op(self, eng, fn, reads=(), writes=()):
        return self._add(eng, fn, reads, writes, False)

    def dma(self, eng, fn, reads=(), writes=(), is_out=False, group=None):
        o = self._add(eng, fn, reads, writes, True, group=group)
        if is_out:
            self.out_dmas.append(o)
        return o

    def inherit(self, new_bufs, old_bufs):
        latest = {}
        dmas = {}
        for b in old_bufs:
            cand = list(b.readers) + list(b.last_w)
            for d in cand:
                d = self._redirect(d)
                if d.is_dma:
                    dmas[id(d)] = d
                else:
                    cur = latest.get(d.eng)
                    if cur is None or d.idx > cur.idx:
                        latest[d.eng] = d
        S = list(latest.values()) + list(dmas.values())
        for nb in new_bufs:
            nb.last_w = []
            nb.wgroup = None
            nb.readers = list(S)

    def if_begin(self, flag_ap, flag_buf):
        assert self.block_dmas is None
        for e in ENGS:
            o = self._add(e, None, [flag_buf], (), False, kind="ifb")
            o.aux = flag_ap
        self.seg += 1
        self.cond_segs.add(self.seg)
        self.block_dmas = {e: [] for e in ENGS}

    def if_end(self):
        cseg = self.seg
        for e in ENGS:
            o = self._add(e, None, (), (), False, kind="ife")
            o.aux = list(self.block_dmas[e])
        self.block_dmas = None
        self.seg += 1
        self.markers[cseg] = {}
        for e in ENGS:
            m = self._add(e, lambda h: h.drain(), (), (), False, kind="marker")
            self.markers[cseg][e] = m

    def emit(self):
        nc = self.nc
        fin = Op("sp", None, False)
        fin.seg = self.seg
        fin.idx = len(self.streams["sp"])
        for o in self.out_dmas:
            fin.deps.append(self._redirect(o))
            fin.deps[-1].signal = True
        self.streams["sp"].append(fin)
        nsem = {}
        for e in ENGS:
            c = {}
            for o in self.streams[e]:
                if o.is_dma:
                    continue
                if o.signal:
                    c[o.seg] = c.get(o.seg, 0) + 1
                    o.count = c[o.seg]
            for sg, v in c.items():
                nsem[(e, sg)] = (v - 1) // SEM_EPOCH + 1
        with contextlib.ExitStack() as es:
            sems = {}
            for (e, sg), n in nsem.items():
                for k in range(n):
                    sems[(e, sg, k)] = es.enter_context(nc.semaphore(f"s_{e}_{sg}_{k}"))
            dsems = {}
            for key in self.dma_uses:
                dsems[key] = es.enter_context(nc.semaphore(f"d_{key[0]}_{key[1]}_{key[2]}"))
            block = es.enter_context(nc.Block())
            handles = {"pe": nc.tensor, "dve": nc.vector, "act": nc.scalar, "pool": nc.gpsimd, "sp": nc.sync}

            def run(e):
                h = handles[e]
                state = {"waited": {}}
                stack = []

                def wait(semkey, sem, val):
                    w = state["waited"]
                    if w.get(semkey, 0) >= val:
                        return
                    w[semkey] = val
                    h.wait_ge(sem, val)

                for o in self.streams[e]:
                    for d in o.deps:
                        if d.is_dma:
                            wait(("d",) + d.dsem, dsems[d.dsem], d.dval)
                        else:
                            ep = (d.count - 1) // SEM_EPOCH
                            wait((d.eng, d.seg, ep), sems[(d.eng, d.seg, ep)], d.count - ep * SEM_EPOCH)
                    if o.kind == "ifb":
                        v = h.value_load(o.aux, min_val=0, max_val=1)
                        g = h.If(v)
                        g.__enter__()
                        stack.append((g, dict(state["waited"])))
                    elif o.kind == "ife":
                        for d in o.aux:
                            wait(("d",) + d.dsem, dsems[d.dsem], d.dval)
                        g, snap = stack.pop()
                        g.__exit__(None, None, None)
                        state["waited"] = snap
                    elif o.is_dma:
                        if o.dprev > 0:
                            wait(("d",) + o.dsem, dsems[o.dsem], o.dprev)
                        ins = o.fn(h)
                        ins.then_inc(dsems[o.dsem], 16)
                    elif o.fn is not None:
                        ins = o.fn(h)
                        if o.signal:
                            ep = (o.count - 1) // SEM_EPOCH
                            ins.then_inc(sems[(e, o.seg, ep)], 1)

            @block.tensor
            def _(eng):
                run("pe")

            @block.vector
            def _(eng):
                run("dve")

            @block.scalar
            def _(eng):
                run("act")

            @block.gpsimd
            def _(eng):
                run("pool")

            @block.sync
            def _(eng):
                run("sp")


D = 1024
S = 2048
NB = 2
NT = S // 128
NMEM = 256
EPS = 1e-6
NE = 32
DE = 512
C_MLQ, C_MLK, C_MLV, C_MLO, C_MLI, C_MLF = 0, 512, 1024, 2048, 3072, 3076
C_FXQ, C_FXK, C_FXV, C_FXF, C_GML, C_GFX = 3080, 4104, 5128, 6152, 6160, 7184
D_IN = 8208
CG_MIX, CG_XQ, CG_XMEM, CG_MOE, CG_MLH, CB_GML, CB_GFX = 0, 8, 16, 24, 32, 40, 48
R_MLI, R_MLF, R_FXF, R_RG, R_RE = 0, 4, 8, 16, 20
NROWS = 52
DENSE_MOE = False
NTT = NB * NT
XW = 1048
NGT = 11
DEBUG = False


def build_program(n_experts_run=NE):
    nc = bass.Bass("TRN2", target_bir_lowering=False)
    dt = lambda name, shape, dtype=F32, kind="ExternalInput": nc.dram_tensor(name, shape, dtype, kind=kind).ap()
    x_d = dt("x", [NB, S, D])
    mem_d = dt("mem", [NB, NMEM, D])
    w_in_d = dt("w_in", [D, D_IN])
    w_pm_d = dt("w_proj_ml", [D, D])
    w_pf_d = dt("w_proj_fx", [D, D])
    w_out_d = dt("w_out", [D, D])
    w_xq_d = dt("w_xq", [D, D])
    w_xkv_d = dt("w_xkv", [D, 2 * D])
    w_xo_d = dt("w_xo", [D, D])
    w_r_d = dt("w_r", [D, 36])
    w_gate_d = dt("w_gate", [NE * 128, 8 * DE])
    w_up_d = dt("w_up", [NE * 128, 8 * DE])
    w_down_d = dt("w_down", [NE * 128, 4 * D])
    cols_d = dt("cols", [128, 56])
    rows_d = dt("rows", [1, NROWS])
    gfin_d = dt("g_final", [1, D])
    ident_d = dt("ident", [128, 128])
    tri_d = dt("tri", [128, 128])
    out_d = dt("out", [NB, S, D], F32, "ExternalOutput")
    ymlT_d = dt("ymlT_scr", [NB, NT, 128, 1024], BF16, "Internal")
    yfxT_d = dt("yfxT_scr", [NB, 8, 128, S], BF16, "Internal")
    x1_d = dt("x1_scr", [NB, NT, 128, 1024], F32, "Internal")
    x2_d = dt("x2_scr", [NB, NT, 128, 1024], F32, "Internal")
    gmoe_d = dt("g_moe_row", [1, D])
    cgu_d = dt("cgu", [128, 64])
    cd_d = dt("cd", [128, 32])
    NSL = NGT * 512
    dbg_d = dt("dbg", [NB, 128, 64], F32, "ExternalOutput") if DEBUG else None
    xn3tm_d = dt("xn3tm_scr", [NB, NT, 128, 1024], BF16, "Internal")
    Xs_d = dt("xs_scr", [NSL, XW], BF16, "Internal")
    C8s_d = dt("c8s_scr", [NSL, 128], F32, "Internal")
    Ys_d = dt("ys_scr", [NSL, D], F32, "Internal")
    wg_rows, wu_rows, wd_rows = w_gate_d, w_up_d, w_down_d

    P = Prog(nc)
    with contextlib.ExitStack() as es:
        sb = lambda n, s, d: es.enter_context(nc.sbuf_tensor(n, s, d))
        ps = lambda n, s, d: es.enter_context(nc.psum_tensor(n, s, d))
        ASZ = 35840
        ARENA = sb("ARENA", [128, ASZ], F32)
        ARB = ARENA[:, :].bitcast(BF16)
        LG_OFF = ASZ - 576 * NB
        A32 = sb("A32", [128, 8, S], BF16)
        idf = sb("idf", [128, 128], F32)
        idb = sb("idb", [128, 128], BF16)
        trif = sb("trif", [128, 128], F32)
        onesf = sb("onesf", [128, 128], F32)
        selb = sb("selb", [128, 128], BF16)
        strib = sb("strib", [128, 128], BF16)
        onesb = sb("onesb", [128, 128], BF16)
        gmoeB = sb("gmoeB", [128, D], F32)
        cguS = sb("cguS", [128, 64], F32)
        cdS = sb("cdS", [128, 32], F32)
        ARI = ARENA[:, :].bitcast(I32)
        colsS = sb("colsS", [128, 56], F32)
        rowsS = sb("rowsS", [128, NROWS], F32)
        gfinS = sb("gfin", [128, D], F32)
        epsc = sb("epsc", [128, 1], F32)
        onec = sb("onec", [128, 1], F32)
        wrS = sb("wr", [128, 8, 36], F32)
        xt = [sb(f"xt{i}", [128, D], F32) for i in range(2)]
        junk = sb("junk", [128, D], BF16)
        xs_b = [sb(f"xsb{i}", [128, D], BF16) for i in range(2)]
        xs_f = sb("xsf", [128, D], F32)
        st_ssq = [sb(f"ssq{i}", [128, 1], F32) for i in range(2)]
        st_ln = [sb(f"lnv{i}", [128, 1], F32) for i in range(2)]
        st_rs = [sb(f"rstd{i}", [128, 1], F32) for i in range(2)]
        PSF = ps("PSF", [128, 6, 512], F32)
        PSB = ps("PSB", [128, 2, 1024], BF16)

        bA32t = [Buf(f"A32_{i}") for i in range(NT)]
        bconst = Buf("const")
        bxt = [Buf("xt0"), Buf("xt1")]
        bjunk = Buf("junk")
        bxsb = [Buf("xsb0"), Buf("xsb1")]
        bxsf = Buf("xsf")
        bst = [Buf("st0"), Buf("st1")]
        bF = [Buf(f"PSF{i}") for i in range(6)]
        bB = [Buf(f"PSB{i}") for i in range(2)]
        bymlT_d = [[Buf(f"ymlTd{b}_{i}") for i in range(NT)] for b in range(NB)]
        byfxT_d = [[Buf(f"yfxTd{b}_{i}") for i in range(8)] for b in range(NB)]
        bx1_d = [[Buf(f"x1d{b}_{i}") for i in range(NT)] for b in range(NB)]
        bx2_d = [[Buf(f"x2d{b}_{i}") for i in range(NT)] for b in range(NB)]
        bxn3_d = [[Buf(f"xn3d{b}_{i}") for i in range(NT)] for b in range(NB)]
        bXs = [Buf(f"Xs{i}") for i in range(NGT)]
        bC8s = [Buf(f"C8s{i}") for i in range(7)]
        bYs = [Buf(f"Ys{i}") for i in range(NGT)]
        blg = Buf("lg")
        lg = ARENA[:, LG_OFF:LG_OFF + 576 * NB].rearrange("p (t n) -> p t n", n=36)

        def DMA(q, out, in_, r=(), w=(), is_out=False, group=None):
            return P.dma(q, lambda e: e.dma_start(out=out, in_=in_), r, w, is_out, group=group)

        def MM(out, lhsT, rhs, start=True, stop=True, r=(), w=()):
            return P.op("pe", lambda e: e.matmul(out, lhsT=lhsT, rhs=rhs, start=start, stop=stop), r, w)

        def TR(out, in_, r=(), w=()):
            return P.op("pe", lambda e: e.transpose(out=out, in_=in_, identity=idb[:]), list(r) + [bconst], w)

        def ACT(out, in_, func, bias=None, scale=1.0, accum=None, r=(), w=()):
            def f(e):
                kw = {}
                if bias is not None:
                    kw["bias"] = bias
                if accum is not None:
                    kw["accum_out"] = accum
                return e.activation(out=out, in_=in_, func=func, scale=scale, **kw)
            return P.op("act", f, r, w)

        def TS(eng, out, in0, s1, s2, op0, op1=None, r=(), w=()):
            def f(e):
                if op1 is None:
                    return e.tensor_scalar(out=out, in0=in0, scalar1=s1, scalar2=None, op0=op0)
                return e.tensor_scalar(out=out, in0=in0, scalar1=s1, scalar2=s2, op0=op0, op1=op1)
            return P.op(eng, f, r, w)

        def STT(eng, out, in0, scalar, in1, op0, op1, r=(), w=()):
            return P.op(eng, lambda e: e.scalar_tensor_tensor(out=out, in0=in0, scalar=scalar, in1=in1, op0=op0, op1=op1), r, w)

        def TT(eng, out, in0, in1, op, r=(), w=()):
            return P.op(eng, lambda e: e.tensor_tensor(out=out, in0=in0, in1=in1, op=op), r, w)

        def CP(eng, out, in_, r=(), w=()):
            if eng == "act":
                return P.op("act", lambda e: e.copy(out=out, in_=in_), r, w)
            return P.op(eng, lambda e: e.tensor_copy(out=out, in_=in_), r, w)

        def MSET(eng, ap, val, w=()):
            return P.op(eng, lambda e: e.memset(ap, val), (), w)

        def RED(eng, out, in_, op, r=(), w=()):
            return P.op(eng, lambda e: e.tensor_reduce(out=out, in_=in_, axis=AX.X, op=op), r, w)

        def RECIP(out, in_, r=(), w=()):
            return P.op("dve", lambda e: e.reciprocal(out=out, in_=in_), r, w)

        scr_off = [0]
        scr_lim = [LG_OFF]
        phase_bufs = [[]]

        def carve_f(n):
            o = scr_off[0]
            scr_off[0] += n
            assert scr_off[0] <= scr_lim[0], (scr_off[0], scr_lim[0])
            return ARENA[:, o:o + n]

        def carve_b(n):
            assert n % 2 == 0
            o = scr_off[0]
            scr_off[0] += n // 2
            assert scr_off[0] <= scr_lim[0], (scr_off[0], scr_lim[0])
            return ARB[:, 2 * o:2 * o + n]

        def new_phase(names, base, manual=None):
            manual = manual or {}
            bs = {n: Buf(n) for n in names}
            P.inherit([v for n, v in bs.items() if n not in manual], phase_bufs[0])
            for n, olds in manual.items():
                P.inherit([bs[n]], olds)
            phase_bufs[0] = list(bs.values())
            scr_off[0] = base
            return bs

        WBv = lambda off, nk, ncols: ARB[:, off:off + nk * ncols].rearrange("p (k n) -> p k n", n=ncols)
        SCR0 = 24640
        SCR5 = 16384
        last_p6 = [[]]
        keep = {}

        DMA("sp", idf[:], ident_d[:, :], w=[bconst])
        DMA("sp", trif[:], tri_d[:, :], w=[bconst])
        DMA("sp", colsS[:], cols_d[:, :], w=[bconst])
        DMA("sp", rowsS[:], rows_d.partition_broadcast(128), w=[bconst])
        DMA("sp", gfinS[:], gfin_d.partition_broadcast(128), w=[bconst])
        DMA("sp", wrS[:], w_r_d.rearrange("(k p) n -> p k n", p=128), w=[bconst])
        MSET("pool", onesf[:], 1.0, w=[bconst])
        MSET("pool", epsc[:], EPS, w=[bconst])
        MSET("pool", onec[:], 1.0, w=[bconst])
        CP("dve", idb[:], idf[:], r=[bconst], w=[bconst])
        DMA("sp", gmoeB[:], gmoe_d.partition_broadcast(128), w=[bconst])
        DMA("sp", cguS[:], cgu_d[:, :], w=[bconst])
        DMA("sp", cdS[:], cd_d[:, :], w=[bconst])
        TT("dve", strib[:], trif[:], idf[:], ALU.subtract, r=[bconst], w=[bconst])
        CP("dve", onesb[:], onesf[:], r=[bconst], w=[bconst])
        MSET("dve", selb[:], 0.0, w=[bconst])
        for p0 in (0, 32, 64):
            MSET("dve", selb[p0:p0 + 1, :], 1.0, w=[bconst])

        zt = xs_b[0]
        MSET("dve", zt[:], 0.0, w=[bxsb[0]])
        for r0 in range(0, NSL, 128):
            DMA("sp", Xs_d[r0:r0 + 128, 0:1024], zt[:], r=[bxsb[0]], w=bXs, group=("zero",))
            DMA("sp", Xs_d[r0:r0 + 128, 1024:XW], zt[:, 0:XW - 1024], r=[bxsb[0]], w=bXs, group=("zero",))
        nt_ctr = [0]

        def norm_transpose(src, src_bufs, gcol, dstT, dst_bufs, fp32=False, dstT_b=None, dstb_bufs=(), tm_store=None):
            i = nt_ctr[0] % 2
            nt_ctr[0] += 1
            ssq, lnv, rstd = st_ssq[i], st_ln[i], st_rs[i]
            ACT(junk[:], src, AF.Square, accum=ssq[:], r=list(src_bufs), w=[bjunk, bst[i]])
            ACT(lnv[:], ssq[:], AF.Ln, bias=epsc[:, 0:1], scale=1.0 / D, r=[bst[i], bconst], w=[bst[i]])
            ACT(rstd[:], lnv[:], AF.Exp, scale=-0.5, r=[bst[i]], w=[bst[i]])
            gb = gcol.unsqueeze(2).to_broadcast([128, 8, 128])
            if not fp32:
                xs = xs_b[i]
                TS("dve", xs[:], src, rstd[:, 0:1], None, ALU.mult, r=list(src_bufs) + [bst[i]], w=[bxsb[i]])
                for k in range(8):
                    TR(PSB[:, i, k * 128:(k + 1) * 128], xs[:, k * 128:(k + 1) * 128], r=[bxsb[i]], w=[bB[i]])
                pv = PSB[:, i, :].rearrange("p (k t) -> p k t", t=128)
                TT("dve", dstT, pv, gb, ALU.mult, r=[bB[i], bconst], w=list(dst_bufs))
            else:
                TS("dve", xs_f[:], src, rstd[:, 0:1], None, ALU.mult, r=list(src_bufs) + [bst[i]], w=[bxsf])
                for k in range(8):
                    MM(PSF[:, 4 + k // 4, (k % 4) * 128:(k % 4 + 1) * 128], xs_f[:, k * 128:(k + 1) * 128], idf[:],
                       r=[bxsf, bconst], w=[bF[4 + k // 4]])
                pv = PSF[:, 4:6, :].rearrange("p a (c t) -> p (a c) t", t=128)
                TT("dve", dstT, pv, gb, ALU.mult, r=[bF[4], bF[5], bconst], w=list(dst_bufs))
                if dstT_b is not None:
                    CP("act", dstT_b, dstT, r=list(dst_bufs), w=list(dstb_bufs))
                if tm_store is not None:
                    TT("pool", xs_b[i][:], xs_f[:], gmoeB[:], ALU.mult, r=[bxsf, bconst], w=[bxsb[i]])
                    DMA("sp", tm_store[0], xs_b[i][:], r=[bxsb[i]], w=[tm_store[1]])

        grp_ctr = [0]

        def load_w(dst_view, src, wbufs, nk=8):
            grp_ctr[0] += 1
            gid_ = ("lw", grp_ctr[0])
            for k in range(nk):
                P.dma("pool", (lambda o_, i_: (lambda e: e.dma_start(out=o_, in_=i_)))(dst_view[:, k, :], src[k * 128:(k + 1) * 128, :]),
                      (), list(wbufs), group=gid_)

        for b in range(NB):
            def p1_tile(bb, i):
                DMA("sp", xt[i % 2][:], x_d[bb, i * 128:(i + 1) * 128, :], w=[bxt[i % 2]])
                norm_transpose(xt[i % 2][:], [bxt[i % 2]], colsS[:, CG_MIX:CG_MIX + 8],
                               A32[:, :, i * 128:(i + 1) * 128], [bA32t[i]])

            if b == 0:
                for i in range(NT):
                    p1_tile(0, i)

            bS = new_phase("W CnT Cnb vext0 vext1 qT0 qT1 kT0 kT1 kw0 kw1 PT sig0 sig1 yb0 yb1 ymlT gsm gA".split(), SCR0)
            bW = [bS["W"]]
            keep["Wml"] = bS["W"]
            wml = WBv(0, 8, 3080)
            load_w(wml, w_in_d[:, 0:3080], bW)
            bWfx = Buf("Wfx")
            P.inherit([bWfx], last_p6[0])
            wfx = WBv(24640, 8, 3080)
            load_w(wfx, w_in_d[:, C_FXQ:C_FXQ + 3080], [bWfx])
            CnT = carve_f(4 * 258).rearrange("p (h n) -> p h n", n=258)
            Cnb = carve_b(4 * 258).rearrange("p (h n) -> p h n", n=258)
            vext2 = [carve_b(4 * 258).rearrange("p (h n) -> p h n", n=258) for _ in range(2)]
            qT2 = [carve_b(512).rearrange("p (h n) -> p h n", n=128) for _ in range(2)]
            kT2 = [carve_b(512).rearrange("p (h n) -> p h n", n=128) for _ in range(2)]
            kw2 = [carve_b(512).rearrange("p (h n) -> p h n", n=128) for _ in range(2)]
            PT = carve_b(128)
            sig2 = [carve_f(1024) for _ in range(2)]
            yb2 = [carve_b(1024) for _ in range(2)]
            ymlT_t = carve_b(1024).rearrange("p (k t) -> p k t", t=128)
            gsm = carve_f(64)
            d1, sc, sa, t2, tot = gsm[:, 28:32], gsm[:, 32:36], gsm[:, 36:40], gsm[:, 40:44], gsm[:, 44:48]
            gA = carve_f(64 * 7)
            gi_a, l1_a, tmp_a, eb_a, dec_a, wv_a, wd_a = [gA[:, 64 * q:64 * (q + 1)] for q in range(7)]
            G_ = [bS["gsm"]]
            bGA = [bS["gA"]]
            MSET("dve", CnT, 0.0, w=[bS["CnT"]])
            MSET("dve", Cnb, 0.0, w=[bS["Cnb"]])
            for q in range(2):
                MSET("dve", vext2[q], 1.0, w=[bS[f"vext{q}"]])
            for c in range(NT):
                for k in range(8):
                    MM(PSF[:, 5, c * 8:(c + 1) * 8], A32[:, k, c * 128:(c + 1) * 128], wml[:, k, C_MLI:C_MLI + 8], k == 0, k == 7,
                       r=[bA32t[c]] + bW, w=[bF[5]])
            pg3 = PSF[:, 5, 0:128].rearrange("p (t g) -> p t g", g=8)
            v3 = lambda ap: ap.rearrange("p (t h) -> p t h", h=4)
            TT("dve", v3(gi_a), pg3[:, :, 0:4], rowsS[:, R_MLI:R_MLI + 4].unsqueeze(1).to_broadcast([128, 16, 4]), ALU.add,
               r=[bF[5], bconst], w=bGA)
            TT("dve", v3(l1_a), pg3[:, :, 4:8], rowsS[:, R_MLF:R_MLF + 4].unsqueeze(1).to_broadcast([128, 16, 4]), ALU.add,
               r=[bF[5], bconst], w=bGA)
            ACT(l1_a, l1_a, AF.Exp, scale=-1.0, r=bGA, w=bGA)
            ACT(l1_a, l1_a, AF.Ln, bias=onec[:, 0:1], r=bGA + [bconst], w=bGA)
            MM(PSF[:, 4, 0:64], trif[:], l1_a, r=bGA + [bconst], w=[bF[4]])
            MM(PSF[:, 4, 64:128], onesf[:], l1_a, r=bGA + [bconst], w=[bF[4]])
            ACT(eb_a, PSF[:, 4, 0:64], AF.Exp, scale=-1.0, r=[bF[4]], w=bGA)
            ACT(dec_a, PSF[:, 4, 64:128], AF.Exp, scale=-1.0, r=[bF[4]], w=bGA)
            TT("dve", tmp_a, gi_a, PSF[:, 4, 0:64], ALU.add, r=bGA + [bF[4]], w=bGA)
            ACT(wv_a, tmp_a, AF.Exp, r=bGA, w=bGA)
            TT("dve", tmp_a, tmp_a, PSF[:, 4, 64:128], ALU.subtract, r=bGA + [bF[4]], w=bGA)
            ACT(wd_a, tmp_a, AF.Exp, r=bGA, w=bGA)

            def p2_proj(c):
                q = c % 2
                tsl = slice(c * 128, (c + 1) * 128)
                qT, kT, kw, vext, sig = qT2[q], kT2[q], kw2[q], vext2[q], sig2[q]
                wd = wd_a[:, 4 * c:4 * c + 4]
                rA = [bA32t[c]] + bW
                for h in range(4):
                    for k in range(8):
                        MM(PSF[:, 4, h * 128:(h + 1) * 128], wml[:, k, C_MLQ + h * 128:C_MLQ + (h + 1) * 128], A32[:, k, tsl],
                           k == 0, k == 7, r=rA, w=[bF[4]])
                ACT(qT, PSF[:, 4, :].rearrange("p (h n) -> p h n", n=128), AF.Identity, scale=128.0 ** -0.5, r=[bF[4]], w=[bS[f"qT{q}"]])
                for h in range(4):
                    for k in range(8):
                        MM(PSF[:, 5, h * 128:(h + 1) * 128], wml[:, k, C_MLK + h * 128:C_MLK + (h + 1) * 128], A32[:, k, tsl],
                           k == 0, k == 7, r=rA, w=[bF[5]])
                CP("dve", kT, PSF[:, 5, :].rearrange("p (h n) -> p h n", n=128), r=[bF[5]], w=[bS[f"kT{q}"]])
                for k in range(8):
                    MM(PSF[:, 4, :], A32[:, k, tsl], wml[:, k, C_MLK:C_MLK + 512], k == 0, k == 7, r=rA, w=[bF[4]])
                TT("dve", kw, PSF[:, 4, :].rearrange("p (h n) -> p h n", n=128), wd.unsqueeze(2).to_broadcast([128, 4, 128]),
                   ALU.mult, r=[bF[4]] + bGA, w=[bS[f"kw{q}"]])
                for hf in range(2):
                    pb_ = 5 - hf
                    for k in range(8):
                        MM(PSF[:, pb_, :], A32[:, k, tsl], wml[:, k, C_MLV + hf * 512:C_MLV + (hf + 1) * 512], k == 0, k == 7,
                           r=rA, w=[bF[pb_]])
                    CP("act", vext[:, 2 * hf:2 * hf + 2, 0:256], PSF[:, pb_, :].rearrange("p (h n) -> p h n", n=256),
                       r=[bF[pb_]], w=[bS[f"vext{q}"]])
                for hf in range(2):
                    pb_ = 5 - hf
                    for k in range(8):
                        MM(PSF[:, pb_, :], A32[:, k, tsl], wml[:, k, C_MLO + hf * 512:C_MLO + (hf + 1) * 512], k == 0, k == 7,
                           r=rA, w=[bF[pb_]])
                    ACT(sig[:, hf * 512:(hf + 1) * 512], PSF[:, pb_, :], AF.Sigmoid, r=[bF[pb_]], w=[bS[f"sig{q}"]])

            def p2_mix(c):
                q = c % 2
                qT, kT, kw, vext, sig, yb = qT2[q], kT2[q], kw2[q], vext2[q], sig2[q], yb2[q]
                eb, dec, wv_ = eb_a[:, 4 * c:4 * c + 4], dec_a[:, 4 * c:4 * c + 4], wv_a[:, 4 * c:4 * c + 4]
                for h in range(4):
                    MM(PSF[:, 4, 0:128], kT[:, h, :], qT[:, h, :], r=[bS[f"kT{q}"], bS[f"qT{q}"]], w=[bF[4]])
                    STT("dve", PT, PSF[:, 4, 0:128], wv_[:, h:h + 1], trif[:], ALU.mult, ALU.mult,
                        r=[bF[4], bconst] + bGA, w=[bS["PT"]])
                    MM(PSF[:, h, 0:257], PT, vext[:, h, 0:257], True, False, r=[bS["PT"], bS[f"vext{q}"]], w=[bF[h]])
                    MM(PSF[:, h, 0:257], qT[:, h, :], Cnb[:, h, 0:257], False, True, r=[bS[f"qT{q}"], bS["Cnb"]], w=[bF[h]])
                for h in range(4):
                    pb_ = 5 - (h % 2)
                    MM(PSF[:, pb_, 0:257], kw[:, h, :], vext[:, h, 0:257], r=[bS[f"kw{q}"], bS[f"vext{q}"]], w=[bF[pb_]])
                    STT("dve", CnT[:, h, 0:257], CnT[:, h, 0:257], dec[:, h:h + 1], PSF[:, pb_, 0:257], ALU.mult, ALU.add,
                        r=[bS["CnT"], bF[pb_]] + bGA, w=[bS["CnT"]])
                    CP("act", Cnb[:, h, 0:257], CnT[:, h, 0:257], r=[bS["CnT"]], w=[bS["Cnb"]])
                den = PSF[:, 0:4, 256]
                CP("dve", d1, den, r=bF[0:4], w=G_)
                STT("dve", d1, d1, -1.0, d1, ALU.mult, ALU.max, r=G_, w=G_)
                TT("dve", d1, d1, eb, ALU.mult, r=G_ + bGA, w=G_)
                TS("dve", d1, d1, 1.0, None, ALU.max, r=G_, w=G_)
                RECIP(d1, d1, r=G_, w=G_)
                TT("dve", sc, d1, eb, ALU.mult, r=G_ + bGA, w=G_)
                for h in range(4):
                    ACT(junk[:, 0:256], PSF[:, h, 0:256], AF.Square, accum=sa[:, h:h + 1], r=[bF[h]], w=[bjunk] + G_)
                TT("dve", t2, sc, sc, ALU.mult, r=G_, w=G_)
                TT("dve", t2, t2, sa, ALU.mult, r=G_, w=G_)
                ACT(t2, t2, AF.Ln, bias=epsc[:, 0:1], scale=1.0 / 256, r=G_ + [bconst], w=G_)
                ACT(t2, t2, AF.Exp, scale=-0.5, r=G_, w=G_)
                TT("dve", tot, t2, sc, ALU.mult, r=G_, w=G_)
                for h in range(4):
                    STT("dve", yb[:, h * 256:(h + 1) * 256], PSF[:, h, 0:256], tot[:, h:h + 1], sig[:, h * 256:(h + 1) * 256],
                        ALU.mult, ALU.mult, r=[bF[h], bS[f"sig{q}"]] + G_, w=[bS[f"yb{q}"]])

            def p2_store(c):
                q = c % 2
                yb = yb2[q]
                for k in range(8):
                    TR(PSB[:, 0, k * 128:(k + 1) * 128], yb[:, k * 128:(k + 1) * 128], r=[bS[f"yb{q}"]], w=[bB[0]])
                TT("dve", ymlT_t, PSB[:, 0, :].rearrange("p (k t) -> p k t", t=128),
                   colsS[:, CG_MLH:CG_MLH + 8].unsqueeze(2).to_broadcast([128, 8, 128]), ALU.mult,
                   r=[bB[0], bconst], w=[bS["ymlT"]])
                DMA("sp", ymlT_d[b, c, :, :], ymlT_t.rearrange("p k t -> p (k t)"), r=[bS["ymlT"]], w=[bymlT_d[b][c]])

            for c in range(NT + 1):
                if c < NT:
                    p2_proj(c)
                if c >= 1:
                    p2_store(c - 1)
                if c < NT:
                    p2_mix(c)

            bS = new_phase("kTh qTh vxh l1f af cend biasall PT0 PT1 rcol yft0 yft1 yo0 yo1 Rr0 Rr1".split(), SCR0)
            bS["W"] = bWfx
            phase_bufs[0] = phase_bufs[0] + [bWfx]
            bW = [bWfx]
            bW40, bW41 = Buf("W40"), Buf("W41")
            P.inherit([bW40, bW41], [keep["Wml"]])
            wg = WBv(0, 8, 2048)
            wpm = WBv(16384, 8, 1024)
            load_w(wg, w_in_d[:, C_GML:C_GML + 2048], [bW40])
            load_w(wpm, w_pm_d, [bW41])
            kTh = carve_b(2048)
            qTh = carve_b(2048)
            vxh = carve_b(16 * 130).rearrange("p (t n) -> p t n", n=130)
            l1f = carve_f(128)
            af = carve_f(128).rearrange("p (t h) -> p t h", h=8)
            cend = carve_f(128).rearrange("p (t h) -> p t h", h=8)
            ncf = carve_f(128)
            tmpf = carve_f(128)
            Rall = carve_b(128)
            tmpb = carve_b(128)
            Rrow = [carve_b(512).rearrange("p (j t) -> p j t", t=128) for _ in range(2)]
            PTf = [carve_b(512).rearrange("p (j t) -> p j t", t=128) for _ in range(2)]
            rcol = carve_f(4)
            yft = [carve_b(128) for _ in range(2)]
            yfo = [carve_b(2048) for _ in range(2)]
            for i in range(NT):
                for k in range(8):
                    MM(PSF[:, 0, i * 8:(i + 1) * 8], A32[:, k, i * 128:(i + 1) * 128], wfx[:, k, 3072:3080], k == 0, k == 7,
                       r=[bA32t[i]] + bW, w=[bF[0]])
            TT("dve", l1f.rearrange("p (t h) -> p t h", h=8), PSF[:, 0, 0:128].rearrange("p (t h) -> p t h", h=8),
               rowsS[:, R_FXF:R_FXF + 8].unsqueeze(1).to_broadcast([128, 16, 8]), ALU.add, r=[bF[0], bconst], w=[bS["l1f"]])
            ACT(l1f, l1f, AF.Exp, scale=-1.0, r=[bS["l1f"]], w=[bS["l1f"]])
            ACT(l1f, l1f, AF.Ln, bias=onec[:, 0:1], r=[bS["l1f"], bconst], w=[bS["l1f"]])
            MM(PSF[:, 1, 0:128], trif[:], l1f, r=[bS["l1f"], bconst], w=[bF[1]])
            MM(PSF[:, 2, 0:128], onesf[:], l1f, r=[bS["l1f"], bconst], w=[bF[2]])
            pTt = PSF[:, 2, 0:128].rearrange("p (t h) -> p t h", h=8)
            CP("dve", cend[:, 0, :], pTt[:, 0, :], r=[bF[2]], w=[bS["cend"]])
            for m in range(1, NT):
                TT("dve", cend[:, m, :], cend[:, m - 1, :], pTt[:, m, :], ALU.add, r=[bF[2], bS["cend"]], w=[bS["cend"]])
            TT("dve", af, cend, pTt, ALU.subtract, r=[bS["cend"], bF[2]], w=[bS["af"]])
            TT("dve", af, af, PSF[:, 1, 0:128].rearrange("p (t h) -> p t h", h=8), ALU.add, r=[bS["af"], bF[1]], w=[bS["af"]])
            cflat = cend.rearrange("p t h -> p (t h)")
            TS("dve", ncf, cflat, -1.0, None, ALU.mult, r=[bS["cend"]], w=[bS["biasall"]])
            CP("dve", Rall, ncf, r=[bS["biasall"]], w=[bS["biasall"]])
            CP("dve", tmpf, Rall, r=[bS["biasall"]], w=[bS["biasall"]])
            TT("dve", ncf, ncf, tmpf, ALU.subtract, r=[bS["biasall"]], w=[bS["biasall"]])
            CP("dve", Rall[32:64, :], ncf[32:64, :], r=[bS["biasall"]], w=[bS["biasall"]])
            CP("dve", tmpb, ncf, r=[bS["biasall"]], w=[bS["biasall"]])
            CP("dve", tmpf, tmpb, r=[bS["biasall"]], w=[bS["biasall"]])
            TT("dve", ncf, ncf, tmpf, ALU.subtract, r=[bS["biasall"]], w=[bS["biasall"]])
            CP("dve", Rall[64:96, :], ncf[64:96, :], r=[bS["biasall"]], w=[bS["biasall"]])
            Rall3 = Rall.rearrange("p (t h) -> p t h", h=8)
            MSET("dve", vxh, 1.0, w=[bS["vxh"]])
            pti = 0
            for h in range(8):
                yo = yfo[h % 2]
                byo = bS[f"yo{h % 2}"]
                for G in range(4):
                    gsl = slice(G * 512, (G + 1) * 512)
                    for k in range(8):
                        MM(PSF[:, 0, :], wfx[:, k, 1024 + h * 128:1024 + (h + 1) * 128], A32[:, k, gsl], k == 0, k == 7,
                           r=bA32t[4 * G:4 * G + 4] + bW, w=[bF[0]])
                    CP("dve", kTh[:, gsl], PSF[:, 0, :], r=[bF[0]], w=[bS["kTh"]])
                    for k in range(8):
                        MM(PSF[:, 1, :], wfx[:, k, h * 128:(h + 1) * 128], A32[:, k, gsl], k == 0, k == 7,
                           r=bA32t[4 * G:4 * G + 4] + bW, w=[bF[1]])
                    ACT(qTh[:, gsl], PSF[:, 1, :], AF.Identity, scale=128.0 ** -0.5, r=[bF[1]], w=[bS["qTh"]])
                    for ti in range(4):
                        i = G * 4 + ti
                        for k in range(8):
                            MM(PSF[:, 2, ti * 128:(ti + 1) * 128], A32[:, k, i * 128:(i + 1) * 128],
                               wfx[:, k, 2048 + h * 128:2048 + (h + 1) * 128], k == 0, k == 7, r=[bA32t[i]] + bW, w=[bF[2]])
                    CP("dve", vxh[:, G * 4:(G + 1) * 4, 0:128], PSF[:, 2, :].rearrange("p (t n) -> p t n", n=128),
                       r=[bF[2]], w=[bS["vxh"]])
                steps = [(J, kb) for J in range(4) for kb in range(4 * J + 4)]

                def emit_qk(si):
                    J, kb = steps[si]
                    jmin = max(0, kb - 4 * J)
                    nq = 4 - jmin
                    if kb == 0:
                        CP("dve", Rrow[J % 2], Rall3[:, 4 * J:4 * J + 4, h:h + 1].to_broadcast([128, 4, 128]),
                           r=[bS["biasall"]], w=[bS[f"Rr{J % 2}"]])
                    MM(PSF[:, si % 2, 0:nq * 128], kTh[:, kb * 128:(kb + 1) * 128],
                       qTh[:, (4 * J + jmin) * 128:(4 * J + 4) * 128], True, False, r=[bS["kTh"], bS["qTh"]], w=[bF[si % 2]])
                    MM(PSF[:, si % 2, 0:nq * 128], selb[:], Rrow[J % 2].rearrange("p j t -> p (j t)")[:, jmin * 128:512], False, True,
                       r=[bconst, bS[f"Rr{J % 2}"]], w=[bF[si % 2]])

                def emit_rest(si):
                    J, kb = steps[si]
                    jmin = max(0, kb - 4 * J)
                    sb_i = si % 2
                    ptb = PTf[si % 2]
                    ptflat = ptb.rearrange("p j t -> p (j t)")
                    bpt = bS[f"PT{si % 2}"]
                    ACT(ptflat[:, jmin * 128:512], PSF[:, sb_i, 0:(4 - jmin) * 128], AF.Exp,
                        bias=af[:, kb, h:h + 1], r=[bF[sb_i], bS["af"]], w=[bpt])
                    for jj in range(jmin, 4):
                        if kb == 4 * J + jj:
                            TT("pool", ptb[:, jj, :], ptb[:, jj, :], trif[:], ALU.mult, r=[bpt, bconst], w=[bpt])
                    for jj in range(jmin, 4):
                        j = 4 * J + jj
                        MM(PSF[:, 2 + jj, 0:129], ptb[:, jj, :], vxh[:, kb, 0:129], kb == 0, kb == j,
                           r=[bpt, bS["vxh"]], w=[bF[2 + jj]])
                    if kb == 4 * J + 3:
                        for jj in range(4):
                            j = 4 * J + jj
                            RECIP(rcol[:, jj:jj + 1], PSF[:, 2 + jj, 128:129], r=[bF[2 + jj]], w=[bS["rcol"]])
                            TS("dve", yft[jj % 2], PSF[:, 2 + jj, 0:128], rcol[:, jj:jj + 1], None, ALU.mult,
                               r=[bF[2 + jj], bS["rcol"]], w=[bS[f"yft{jj % 2}"]])
                            TR(PSB[:, 1, (jj % 2) * 128:(jj % 2 + 1) * 128], yft[jj % 2], r=[bS[f"yft{jj % 2}"]], w=[bB[1]])
                            CP("act", yo[:, j * 128:(j + 1) * 128], PSB[:, 1, (jj % 2) * 128:(jj % 2 + 1) * 128], r=[bB[1]], w=[byo])

                emit_qk(0)
                for si in range(len(steps)):
                    if si + 1 < len(steps):
                        emit_qk(si + 1)
                    emit_rest(si)
                DMA("sp", yfxT_d[b, h, :, :], yo, r=[byo], w=[byfxT_d[b][h]])

            bS = new_phase("W2 W3 yg0 yg1 yf0 yf1 mT sgA sgB tA x1t0 x1t1".split(), SCR0,
                           manual={"W2": [keep["Wml"], bWfx], "W3": [bWfx], "yg1": [bWfx], "yf1": [bWfx]})
            bS["W0"], bS["W1"] = bW40, bW41
            phase_bufs[0] = phase_bufs[0] + [bW40, bW41]
            wpf = WBv(24576, 8, 1024)
            wo = WBv(32768, 8, 1024)
            load_w(wpf, w_pf_d, [bS["W2"]])
            load_w(wo, w_out_d, [bS["W3"]])
            ymlTg = [carve_b(4096).rearrange("p (i k t) -> p i k t", k=8, t=128),
                     ARB[:, 40960:45056].rearrange("p (i k t) -> p i k t", k=8, t=128)]
            yfxTg = [carve_b(4096).rearrange("p (k t) -> p k t", t=512),
                     ARB[:, 45056:49152].rearrange("p (k t) -> p k t", t=512)]
            mT = carve_b(4096).rearrange("p (k t) -> p k t", t=512)
            sgA = carve_f(512)
            sgB = carve_f(512)
            tA = carve_f(512)
            x1tt = [carve_f(1024) for _ in range(2)]
            for G in range(4):
                gsl = slice(G * 512, (G + 1) * 512)
                yg = ymlTg[G % 2]
                byg = bS[f"yg{G % 2}"]
                yf = yfxTg[G % 2]
                byf = bS[f"yf{G % 2}"]
                for ti in range(4):
                    DMA("sp", yg[:, ti, :, :].rearrange("p k t -> p (k t)"), ymlT_d[b, G * 4 + ti, :, :],
                        r=[bymlT_d[b][G * 4 + ti]], w=[byg])
                DMA("sp", yf, yfxT_d[b, :, :, G * 512:(G + 1) * 512].rearrange("h p t -> p h t"), r=byfxT_d[b], w=[byf])
                for m in range(8):
                    msl = slice(m * 128, (m + 1) * 128)
                    for k in range(8):
                        MM(PSF[:, 0, :], wpm[:, k, msl], yg[:, :, k, :], k == 0, k == 7, r=[bS["W1"], byg], w=[bF[0]])
                    for k in range(8):
                        MM(PSF[:, 1, :], wg[:, k, msl], A32[:, k, gsl], k == 0, k == 7, r=[bS["W0"]] + bA32t[4 * G:4 * G + 4], w=[bF[1]])
                    for k in range(8):
                        MM(PSF[:, 2, :], wpf[:, k, msl], yf[:, k, :], k == 0, k == 7, r=[bS["W2"], byf], w=[bF[2]])
                    for k in range(8):
                        MM(PSF[:, 3, :], wg[:, k, 1024 + m * 128:1024 + (m + 1) * 128], A32[:, k, gsl], k == 0, k == 7,
                           r=[bS["W0"]] + bA32t[4 * G:4 * G + 4], w=[bF[3]])
                    ACT(sgA, PSF[:, 1, :], AF.Sigmoid, bias=colsS[:, CB_GML + m:CB_GML + m + 1], r=[bF[1], bconst], w=[bS["sgA"]])
                    ACT(sgB, PSF[:, 3, :], AF.Sigmoid, bias=colsS[:, CB_GFX + m:CB_GFX + m + 1], r=[bF[3], bconst], w=[bS["sgB"]])
                    TT("dve", tA, PSF[:, 0, :], sgA, ALU.mult, r=[bF[0], bS["sgA"]], w=[bS["tA"]])
                    TT("dve", sgB, PSF[:, 2, :], sgB, ALU.mult, r=[bF[2], bS["sgB"]], w=[bS["sgB"]])
                    TT("pool", mT[:, m, :], tA, sgB, ALU.add, r=[bS["tA"], bS["sgB"]], w=[bS["mT"]])
                for ti in range(4):
                    i = G * 4 + ti
                    DMA("sp", xt[i % 2][:], x_d[b, i * 128:(i + 1) * 128, :], w=[bxt[i % 2]])
                    for hf in range(2):
                        for k in range(8):
                            MM(PSF[:, 4 + hf, :], mT[:, k, ti * 128:(ti + 1) * 128], wo[:, k, hf * 512:(hf + 1) * 512], k == 0, k == 7,
                               r=[bS["mT"], bS["W3"]], w=[bF[4 + hf]])
                        TT("dve", x1tt[i % 2][:, hf * 512:(hf + 1) * 512], xt[i % 2][:, hf * 512:(hf + 1) * 512], PSF[:, 4 + hf, :], ALU.add,
                           r=[bxt[i % 2], bF[4 + hf]], w=[bS[f"x1t{i % 2}"]])
                    DMA("sp", x1_d[b, i, :, :], x1tt[i % 2], r=[bS[f"x1t{i % 2}"]], w=[bx1_d[b][i]])

            bS = new_phase("W0 W1 W3 mnT KT Vx xn2T xqT PT0 PT1 otm x1a0 x1a1 x1b0 x1b1 x2t0 x2t1 xn3f0 xn3f1 rc5".split(), SCR5)
            wxq = WBv(0, 8, 1024)
            wxkv = WBv(8192, 8, 2048)
            wxo = WBv(24576, 8, 1024)
            load_w(wxkv, w_xkv_d, [bS["W1"]])
            load_w(wxq, w_xq_d, [bS["W0"]])
            load_w(wxo, w_xo_d, [bS["W3"]])
            mn_o = scr_off[0]
            mnT = carve_b(2048).rearrange("p (k n) -> p k n", n=256)
            KT = carve_b(2048).rearrange("p (c n) -> p c n", n=256)
            Vx = carve_b(2 * 4 * 258).rearrange("p (t h n) -> p t h n", h=4, n=258)
            xn2T = carve_b(4096).rearrange("p (k t) -> p k t", t=512)
            oT = xn2T
            xqT = carve_b(4096).rearrange("p (k t) -> p k t", t=512)
            PTx = [carve_b(1024).rearrange("p (n t) -> p n t", t=512) for _ in range(2)]
            otm = carve_b(4096).rearrange("p (t f) -> p t f", f=1024)
            x1a_ = [carve_f(1024), ARENA[:, mn_o:mn_o + 1024]]
            bS["x1a1"] = bS["mnT"]
            x1b_ = [carve_f(1024) for _ in range(2)]
            x2t_ = [carve_f(1024) for _ in range(2)]
            xn3f_ = [carve_f(1024).rearrange("p (k t) -> p k t", t=128) for _ in range(2)]
            rc5 = carve_f(4)
            if b == 0:
                P.inherit([blg], phase_bufs[0])
            MSET("dve", Vx, 1.0, w=[bS["Vx"]])
            for mt in range(2):
                DMA("sp", xt[mt][:], mem_d[b, mt * 128:(mt + 1) * 128, :], w=[bxt[mt]])
                norm_transpose(xt[mt][:], [bxt[mt]], colsS[:, CG_XMEM:CG_XMEM + 8], mnT[:, :, mt * 128:(mt + 1) * 128], [bS["mnT"]])
            for c8 in range(8):
                for k in range(8):
                    MM(PSF[:, c8 % 2, 0:256], wxkv[:, k, c8 * 128:(c8 + 1) * 128], mnT[:, k, :], k == 0, k == 7,
                       r=[bS["W1"], bS["mnT"]], w=[bF[c8 % 2]])
                CP("dve", KT[:, c8, :], PSF[:, c8 % 2, 0:256], r=[bF[c8 % 2]], w=[bS["KT"]])
            for mt in range(2):
                for hf in range(2):
                    for k in range(8):
                        MM(PSF[:, 2 + hf, :], mnT[:, k, mt * 128:(mt + 1) * 128], wxkv[:, k, 1024 + hf * 512:1024 + (hf + 1) * 512],
                           k == 0, k == 7, r=[bS["W1"], bS["mnT"]], w=[bF[2 + hf]])
                    CP("act", Vx[:, mt, 2 * hf:2 * hf + 2, 0:256], PSF[:, 2 + hf, :].rearrange("p (h n) -> p h n", n=256),
                       r=[bF[2 + hf]], w=[bS["Vx"]])
            pti = 0
            for G in range(4):
                for ti in range(4):
                    i = G * 4 + ti
                    DMA("sp", x1a_[ti % 2], x1_d[b, i, :, :], r=[bx1_d[b][i]], w=[bS[f"x1a{ti % 2}"]])
                    norm_transpose(x1a_[ti % 2], [bS[f"x1a{ti % 2}"]], colsS[:, CG_XQ:CG_XQ + 8], xn2T[:, :, ti * 128:(ti + 1) * 128], [bS["xn2T"]])
                for c8 in range(8):
                    for k in range(8):
                        MM(PSF[:, c8 % 2, :], wxq[:, k, c8 * 128:(c8 + 1) * 128], xn2T[:, k, :], k == 0, k == 7,
                           r=[bS["W0"], bS["xn2T"]], w=[bF[c8 % 2]])
                    ACT(xqT[:, c8, :], PSF[:, c8 % 2, :], AF.Identity, scale=1.0 / 16.0, r=[bF[c8 % 2]], w=[bS["xqT"]])
                def x_qk(h):
                    for nt_ in range(2):
                        pbk = nt_ if h % 2 == 0 else 4 + nt_
                        for c2 in range(2):
                            MM(PSF[:, pbk, :], KT[:, 2 * h + c2, nt_ * 128:(nt_ + 1) * 128], xqT[:, 2 * h + c2, :], c2 == 0, c2 == 1,
                               r=[bS["KT"], bS["xqT"]], w=[bF[pbk]])

                def x_rest(h):
                    ptb = PTx[h % 2]
                    bpt = bS[f"PT{h % 2}"]
                    for nt_ in range(2):
                        pbk = nt_ if h % 2 == 0 else 4 + nt_
                        ACT(ptb[:, nt_, :], PSF[:, pbk, :], AF.Exp, r=[bF[pbk]], w=[bpt])
                    for ti in range(4):
                        pb = 2 + (ti % 2)
                        for nt_ in range(2):
                            MM(PSF[:, pb, 0:257], ptb[:, nt_, ti * 128:(ti + 1) * 128], Vx[:, nt_, h, 0:257], nt_ == 0, nt_ == 1,
                               r=[bpt, bS["Vx"]], w=[bF[pb]])
                        RECIP(rc5[:, ti:ti + 1], PSF[:, pb, 256:257], r=[bF[pb]], w=[bS["rc5"]])
                        TS("dve", otm[:, ti, h * 256:(h + 1) * 256], PSF[:, pb, 0:256], rc5[:, ti:ti + 1], None, ALU.mult,
                           r=[bF[pb], bS["rc5"]], w=[bS["otm"]])

                x_qk(0)
                for h in range(4):
                    if h + 1 < 4:
                        x_qk(h + 1)
                    x_rest(h)
                if b + 1 < NB:
                    for ti in range(4):
                        p1_tile(b + 1, G * 4 + ti)
                for ti in range(4):
                    for k in range(8):
                        TR(PSB[:, ti % 2, k * 128:(k + 1) * 128], otm[:, ti, k * 128:(k + 1) * 128], r=[bS["otm"]], w=[bB[ti % 2]])
                    CP("act", oT[:, :, ti * 128:(ti + 1) * 128], PSB[:, ti % 2, :].rearrange("p (k t) -> p k t", t=128),
                       r=[bB[ti % 2]], w=[bS["xn2T"]])
                def e_mm(ti):
                    i = G * 4 + ti
                    x1b, x2t = x1b_[ti % 2], x2t_[ti % 2]
                    bx1b, bx2t = bS[f"x1b{ti % 2}"], bS[f"x2t{ti % 2}"]
                    DMA("sp", x1b, x1_d[b, i, :, :], r=[bx1_d[b][i]], w=[bx1b])
                    for hf in range(2):
                        for k in range(8):
                            MM(PSF[:, 2 + hf, :], oT[:, k, ti * 128:(ti + 1) * 128], wxo[:, k, hf * 512:(hf + 1) * 512], k == 0, k == 7,
                               r=[bS["xn2T"], bS["W3"]], w=[bF[2 + hf]])
                        TT("dve", x2t[:, hf * 512:(hf + 1) * 512], x1b[:, hf * 512:(hf + 1) * 512], PSF[:, 2 + hf, :], ALU.add,
                           r=[bx1b, bF[2 + hf]], w=[bx2t])
                    DMA("sp", x2_d[b, i, :, :], x2t, r=[bx2t], w=[bx2_d[b][i]])

                def e_tail(ti):
                    i = G * 4 + ti
                    x2t, xn3f = x2t_[ti % 2], xn3f_[ti % 2]
                    bx2t, bxn3f = bS[f"x2t{ti % 2}"], bS[f"xn3f{ti % 2}"]
                    norm_transpose(x2t, [bx2t], colsS[:, CG_MOE:CG_MOE + 8], xn3f, [bxn3f], fp32=True,
                                   tm_store=(xn3tm_d[b, i, :, :], bxn3_d[b][i]))
                    for k in range(8):
                        MM(PSF[:, 0, 0:36], xn3f[:, k, :], wrS[:, k, :], k == 0, k == 7, r=[bxn3f, bconst], w=[bF[0]])
                    TT("dve", lg[:, b * NT + i, :], PSF[:, 0, 0:36], rowsS[:, R_RG:R_RG + 36], ALU.add, r=[bF[0], bconst], w=[blg])

                e_mm(0)
                for ti in range(4):
                    if ti + 1 < 4:
                        e_mm(ti + 1)
                    e_tail(ti)

            phase_bufs[0] = phase_bufs[0] + [blg]
            last_p6[0] = list(phase_bufs[0])
        b = None
        names6 = ("GU0 GU1 GU2 DN0 DN1 route srt idx xst0 xst1 xst2 xg0 xg1 c8d0 c8d1 xT he0 he1 sg0 sg1 ys").split()
        bS = new_phase(names6, 16384)
        gmax = carve_f(NTT)
        goh = carve_f(NTT * 4).rearrange("p (t g) -> p t g", g=4)
        gex = carve_f(NTT * 4).rearrange("p (t g) -> p t g", g=4)
        gp = carve_f(NTT)
        tmp32_o = scr_off[0]
        tmp32 = carve_f(NTT * 32).rearrange("p (t g e) -> p t g e", g=4, e=8)
        esel = carve_f(NTT * 8).rearrange("p (t e) -> p t e", e=8)
        m1 = carve_f(NTT)
        m2 = carve_f(NTT)
        oh1 = carve_f(NTT * 8).rearrange("p (t e) -> p t e", e=8)
        oh2 = carve_f(NTT * 8).rearrange("p (t e) -> p t e", e=8)
        msk = carve_f(NTT * 8).rearrange("p (t e) -> p t e", e=8)
        w1 = carve_f(NTT)
        w2 = carve_f(NTT)
        c8t = carve_f(NTT * 8).rearrange("p (t e) -> p t e", e=8)
        bR = [bS["route"]]
        bc16 = lambda ap, n: ap.unsqueeze(2).to_broadcast([128, NTT, n])
        gl = lg[:, :, 0:4]
        RED("dve", gmax, gl, ALU.max, r=[blg], w=bR)
        TT("dve", goh, gl, bc16(gmax, 4), ALU.is_equal, r=[blg] + bR, w=bR)
        TT("dve", gex, gl, bc16(gmax, 4), ALU.subtract, r=[blg] + bR, w=bR)
        ACT(gex, gex, AF.Exp, r=bR, w=bR)
        RED("dve", gp, gex, ALU.add, r=bR, w=bR)
        RECIP(gp, gp, r=bR, w=bR)
        el = lg[:, :, 4:36].rearrange("p t (g e) -> p t g e", e=8)
        TT("dve", tmp32, el, goh.unsqueeze(3).to_broadcast([128, NTT, 4, 8]), ALU.mult, r=[blg] + bR, w=bR)
        RED("dve", esel, tmp32.rearrange("p t g e -> p t e g"), ALU.add, r=bR, w=bR)
        RED("dve", m1, esel, ALU.max, r=bR, w=bR)
        TT("dve", oh1, esel, bc16(m1, 8), ALU.is_equal, r=bR, w=bR)
        STT("dve", msk, oh1, -1e30, esel, ALU.mult, ALU.add, r=bR, w=bR)
        RED("dve", m2, msk, ALU.max, r=bR, w=bR)
        TT("dve", oh2, msk, bc16(m2, 8), ALU.is_equal, r=bR, w=bR)
        TT("dve", w2, m2, m1, ALU.subtract, r=bR, w=bR)
        ACT(w2, w2, AF.Exp, r=bR, w=bR)
        TS("dve", w2, w2, 1.0, None, ALU.add, r=bR, w=bR)
        RECIP(w1, w2, r=bR, w=bR)
        TT("dve", w1, w1, gp, ALU.mult, r=bR, w=bR)
        TT("dve", w2, gp, w1, ALU.subtract, r=bR, w=bR)
        TT("dve", c8t, oh1, bc16(w1, 8), ALU.mult, r=bR, w=bR)
        TT("dve", oh2, oh2, bc16(w2, 8), ALU.mult, r=bR, w=bR)
        TT("dve", c8t, c8t, oh2, ALU.add, r=bR, w=bR)
        bT = [bS["srt"]]
        gohb = carve_b(NTT * 4)
        pre = carve_f(NTT * 4).rearrange("p (t g) -> p t g", g=4)
        posall = carve_f(NTT * 4).rearrange("p (t g) -> p t g", g=4)
        ntot = carve_f(4)
        cnt = carve_f(4)
        tmp4 = carve_f(4)
        endg = carve_f(4)
        baseg = carve_f(4)
        pos_f = carve_f(NTT)
        pos_o = scr_off[0]
        pos_i = ARI[:, pos_o:pos_o + NTT]
        carve_f(NTT)
        gid = carve_f(NGT)
        g8k = carve_f(NGT)
        g4k = carve_f(NGT)
        idxf = carve_f(8)
        io = scr_off[0]
        idx_e = ARI[:, io:io + NGT * 8].rearrange("p (s n) -> p s n", n=8)
        carve_f(NGT * 8)
        CP("dve", gohb, goh.rearrange("p t g -> p (t g)"), r=bR, w=bT)
        MM(PSF[:, 0, 0:NTT * 4], strib[:], gohb, r=bT + [bconst], w=[bF[0]])
        MM(PSF[:, 1, 0:NTT * 4], onesb[:], gohb, r=bT + [bconst], w=[bF[1]])
        Lp = PSF[:, 0, 0:NTT * 4].rearrange("p (t g) -> p t g", g=4)
        Tp = PSF[:, 1, 0:NTT * 4].rearrange("p (t g) -> p t g", g=4)
        MSET("dve", pre[:, 0, :], 0.0, w=bT)
        for i in range(1, NTT):
            TT("dve", pre[:, i, :], pre[:, i - 1, :], Tp[:, i - 1, :], ALU.add, r=bT + [bF[1]], w=bT)
        TT("dve", ntot, pre[:, NTT - 1, :], Tp[:, NTT - 1, :], ALU.add, r=bT + [bF[1]], w=bT)
        TS("dve", cnt, ntot, 0.0, None, ALU.is_gt, r=bT, w=bT)
        for q in range(1, 8):
            TS("dve", tmp4, ntot, 512.0 * q, None, ALU.is_gt, r=bT, w=bT)
            TT("dve", cnt, cnt, tmp4, ALU.add, r=bT, w=bT)
        TS("dve", cnt, cnt, 512.0, None, ALU.mult, r=bT, w=bT)
        CP("dve", endg[:, 0:1], cnt[:, 0:1], r=bT, w=bT)
        for g in (1, 2, 3):
            TT("dve", endg[:, g:g + 1], endg[:, g - 1:g], cnt[:, g:g + 1], ALU.add, r=bT, w=bT)
        TT("dve", baseg, endg, cnt, ALU.subtract, r=bT, w=bT)
        TT("dve", posall, pre, Lp, ALU.add, r=bT + [bF[0]], w=bT)
        TT("dve", posall, posall, baseg.unsqueeze(1).to_broadcast([128, NTT, 4]), ALU.add, r=bT, w=bT)
        TT("dve", posall, posall, goh, ALU.mult, r=bT + bR, w=bT)
        RED("dve", pos_f, posall, ALU.add, r=bT, w=bT)
        CP("dve", pos_i, pos_f, r=bT, w=bT)
        for sidx in range(NGT):
            TS("dve", tmp4, endg, 512.0 * sidx, None, ALU.is_le, r=bT, w=bT)
            RED("dve", gid[:, sidx:sidx + 1], tmp4, ALU.add, r=bT, w=bT)
        TS("dve", gid, gid, 3.0, None, ALU.min, r=bT, w=bT)
        TS("dve", g8k, gid, 1024.0, None, ALU.mult, r=bT, w=bT)
        bI = [bS["idx"]]
        for sidx in range(NGT):
            TS("dve", idxf, cguS[:, 0:8], g8k[:, sidx:sidx + 1], None, ALU.add, r=bT + [bconst], w=bT)
            CP("dve", idx_e[:, sidx, :], idxf, r=bT, w=bI)
        if DEBUG:
            dbt = carve_f(64)
            MSET("dve", dbt, 0.0, w=bT)
            CP("dve", dbt[:, 0:16], pos_f, r=bT, w=bT)
            CP("dve", dbt[:, 16:24], gid, r=bT, w=bT)
            CP("dve", dbt[:, 24:28], endg, r=bT, w=bT)
            CP("dve", dbt[:, 28:32], ntot, r=bT, w=bT)
            CP("dve", dbt[:, 32:48], pos_i, r=bT, w=bT)
            DMA("sp", dbg_d[0, :, :], dbt, r=bT, is_out=True)
        c8h = ARENA[:, tmp32_o:tmp32_o + NTT * 8]
        c8r = ARENA[:, tmp32_o + NTT * 8:tmp32_o + NTT * 16]
        c8p = ARB[:, 2 * (tmp32_o + NTT * 16):2 * (tmp32_o + NTT * 16) + NTT * 24].rearrange("p (t q e) -> p t q e", q=3, e=8)
        c8flat = c8t.rearrange("p t e -> p (t e)")
        c8rv = c8r.rearrange("p (t e) -> p t e", e=8)
        c8hv = c8h.rearrange("p (t e) -> p t e", e=8)
        CP("dve", c8p[:, :, 0, :], c8t, r=bR, w=bT)
        CP("dve", c8hv, c8p[:, :, 0, :], r=bT, w=bT)
        TT("dve", c8r, c8flat, c8h, ALU.subtract, r=bR + bT, w=bT)
        CP("dve", c8p[:, :, 1, :], c8rv, r=bT, w=bT)
        CP("dve", c8hv, c8p[:, :, 1, :], r=bT, w=bT)
        TT("dve", c8r, c8r, c8h, ALU.subtract, r=bT, w=bT)
        CP("dve", c8p[:, :, 2, :], c8rv, r=bT, w=bT)
        xst = [carve_b(XW) for _ in range(2)]
        for i in range(NTT):
            bx = bS[f"xst{i % 2}"]
            DMA("sp", xst[i % 2][:, 0:1024], xn3tm_d[i // NT, i % NT, :, :], r=[bxn3_d[i // NT][i % NT]], w=[bx])
            CP("dve", xst[i % 2][:, 1024:1048], c8p[:, i, :, :].rearrange("p q e -> p (q e)"), r=bT, w=[bx])
            P.dma("pool", (lambda xs_, col: (lambda e: e.indirect_dma_start(
                out=Xs_d[:, :], out_offset=bass.IndirectOffsetOnAxis(ap=pos_i[:, col:col + 1], axis=0),
                in_=xs_, in_offset=None, bounds_check=NSL - 1, oob_is_err=False)))(xst[i % 2], i),
                [bx] + bT, bXs, group=("scatter",))
        fin_base = scr_off[0]
        xg2 = [carve_b(4 * XW).rearrange("p (c f) -> p c f", f=XW) for _ in range(2)]
        c8s2 = [carve_f(32).rearrange("p (c e) -> p c e", e=8) for _ in range(2)]
        xTs = carve_b(4096).rearrange("p (k t) -> p k t", t=512)
        heT = [carve_b(2048).rearrange("p (f t) -> p f t", t=512) for _ in range(2)]
        sgm = [carve_f(512) for _ in range(2)]
        ys = carve_f(4096).rearrange("p (c f) -> p c f", f=D)
        bhe = [bS["he0"], bS["he1"]]
        bsg = [bS["sg0"], bS["sg1"]]
        bGU = [bS["GU0"], bS["GU1"], bS["GU2"]]
        bDN = [bS["DN0"], bS["DN1"]]
        bxg = [bS["xg0"], bS["xg1"]]
        units = [(sidx, j) for sidx in range(NGT) for j in range(8)]
        NU = len(units)

        def gather_gu(u):
            sidx, j = units[u]
            g_ = u % 3
            grp_ctr[0] += 1
            for (dst_, rows_) in ((WBv(g_ * 8192, 8, 512), wg_rows), (WBv(g_ * 8192 + 4096, 8, 512), wu_rows)):
                P.dma("pool", (lambda o_, r_, ix_: (lambda e: e.indirect_dma_start(
                    out=o_, out_offset=None, in_=r_, in_offset=bass.IndirectOffsetOnAxis(ap=ix_, axis=0))))(
                        dst_.rearrange("p k n -> p (k n)"), rows_[:, :], idx_e[:, sidx, j:j + 1]),
                    bI, [bGU[g_]], group=("gu", grp_ctr[0]))

        def gather_dn(u):
            sidx, j = units[u]
            d_ = u % 2
            grp_ctr[0] += 1
            P.dma("pool", (lambda o_, r_, ix_: (lambda e: e.indirect_dma_start(
                out=o_, out_offset=None, in_=r_, in_offset=bass.IndirectOffsetOnAxis(ap=ix_, axis=0))))(
                    WBv(24576 + d_ * 4096, 4, 1024).rearrange("p k n -> p (k n)"), wd_rows[:, :], idx_e[:, sidx, j:j + 1]),
                bI, [bDN[d_]], group=("dn", grp_ctr[0]))

        def prologue(sidx):
            xg = xg2[sidx % 2]
            DMA("sp", xg, Xs_d[sidx * 512:(sidx + 1) * 512, :].rearrange("(c p) f -> p c f", p=128), r=[bXs[sidx]], w=[bxg[sidx % 2]])
            for c in range(4):
                for k in range(8):
                    TR(PSB[:, c % 2, k * 128:(k + 1) * 128], xg[:, c, k * 128:(k + 1) * 128], r=[bxg[sidx % 2]], w=[bB[c % 2]])
                CP("act" if c % 2 == 0 else "dve", xTs[:, :, c * 128:(c + 1) * 128],
                   PSB[:, c % 2, :].rearrange("p (k t) -> p k t", t=128), r=[bB[c % 2]], w=[bS["xT"]])
            pcs = xg[:, :, 1024:1048].rearrange("p c (q e) -> p c q e", e=8)
            c8d = c8s2[sidx % 2]
            TT("dve", c8d, pcs[:, :, 0, :], pcs[:, :, 1, :], ALU.add, r=[bxg[sidx % 2]], w=[bS[f"c8d{sidx % 2}"]])
            TT("dve", c8d, c8d, pcs[:, :, 2, :], ALU.add, r=[bxg[sidx % 2], bS[f"c8d{sidx % 2}"]], w=[bS[f"c8d{sidx % 2}"]])

        def emit_gu(u):
            g_ = u % 3
            wge = WBv(g_ * 8192, 8, 512)
            wue = WBv(g_ * 8192 + 4096, 8, 512)
            he, bh = heT[u % 2], bhe[u % 2]
            for fc in range(4):
                fsl = slice(fc * 128, (fc + 1) * 128)
                pg, pu = (fc % 2) * 2, (fc % 2) * 2 + 1
                for k in range(8):
                    MM(PSF[:, pg, :], wge[:, k, fsl], xTs[:, k, :], k == 0, k == 7, r=[bGU[g_], bS["xT"]], w=[bF[pg]])
                for k in range(8):
                    MM(PSF[:, pu, :], wue[:, k, fsl], xTs[:, k, :], k == 0, k == 7, r=[bGU[g_], bS["xT"]], w=[bF[pu]])
                ACT(sgm[fc % 2], PSF[:, pg, :], AF.Silu, r=[bF[pg]], w=[bsg[fc % 2]])
                TT("dve", he[:, fc, :], sgm[fc % 2], PSF[:, pu, :], ALU.mult, r=[bsg[fc % 2], bF[pu]], w=[bh])

        def emit_down(u):
            sidx, j = units[u]
            d_ = u % 2
            wde = WBv(24576 + d_ * 4096, 4, 1024)
            he, bh = heT[u % 2], bhe[u % 2]
            c8s = c8s2[sidx % 2]
            for c in range(4):
                for hf in range(2):
                    for fc in range(4):
                        MM(PSF[:, 4 + hf, :], he[:, fc, c * 128:(c + 1) * 128], wde[:, fc, hf * 512:(hf + 1) * 512],
                           fc == 0, fc == 3, r=[bh, bDN[d_]], w=[bF[4 + hf]])
                    ysl = ys[:, c, hf * 512:(hf + 1) * 512]
                    if j == 0:
                        TS("dve", ysl, PSF[:, 4 + hf, :], c8s[:, c, 0:1], None, ALU.mult, r=[bF[4 + hf], bS[f"c8d{sidx % 2}"]], w=[bS["ys"]])
                    else:
                        STT("dve", ysl, PSF[:, 4 + hf, :], c8s[:, c, j:j + 1], ysl, ALU.mult, ALU.add,
                            r=[bF[4 + hf], bS[f"c8d{sidx % 2}"], bS["ys"]], w=[bS["ys"]])
            if j == 7:
                DMA("sp", Ys_d[sidx * 512:(sidx + 1) * 512, :].rearrange("(c p) f -> p c f", p=128), ys, r=[bS["ys"]], w=[bYs[sidx]])

        gather_gu(0)
        gather_dn(0)
        gather_gu(1)
        gather_dn(1)
        prologue(0)
        emit_gu(0)
        for u in range(NU):
            if u + 2 < NU:
                gather_gu(u + 2)
            if u + 1 < NU:
                if units[u + 1][0] != units[u][0]:
                    prologue(units[u + 1][0])
                emit_gu(u + 1)
            emit_down(u)
            if u + 2 < NU:
                gather_dn(u + 2)
        fin_b = [Buf(f"yg{q}") for q in range(4)] + [Buf(f"x2f{q}") for q in range(4)]
        P.inherit(fin_b, [bxg[0], bxg[1], bS["xT"], bhe[0], bhe[1], bsg[0], bsg[1]])
        phase_bufs[0] = phase_bufs[0] + fin_b
        scr_off[0] = fin_base
        ygt = [carve_f(1024) for _ in range(4)]
        x2f = [carve_f(1024) for _ in range(4)]
        fst = [carve_f(4) for _ in range(4)]
        def fin_load(i):
            q = i % 4
            byg, bx2 = fin_b[q], fin_b[4 + q]
            P.dma("pool", (lambda o_, col: (lambda e: e.indirect_dma_start(
                out=o_, out_offset=None, in_=Ys_d[:, :],
                in_offset=bass.IndirectOffsetOnAxis(ap=pos_i[:, col:col + 1], axis=0))))(ygt[q], i),
                bYs + bT, [byg])
            DMA("sp", x2f[q], x2_d[i // NT, i % NT, :, :], r=[bx2_d[i // NT][i % NT]], w=[bx2])

        def fin_compute(i):
            q = i % 4
            byg, bx2 = fin_b[q], fin_b[4 + q]
            TT("dve", x2f[q], x2f[q], ygt[q], ALU.add, r=[bx2, byg], w=[bx2])
            ACT(junk[:], x2f[q], AF.Square, accum=fst[q][:, 0:1], r=[bx2], w=[bjunk, bfst[q]])
            ACT(fst[q][:, 1:2], fst[q][:, 0:1], AF.Ln, bias=epsc[:, 0:1], scale=1.0 / D, r=[bfst[q], bconst], w=[bfst[q]])
            ACT(fst[q][:, 2:3], fst[q][:, 1:2], AF.Exp, scale=-0.5, r=[bfst[q]], w=[bfst[q]])
            STT("dve", x2f[q], x2f[q], fst[q][:, 2:3], gfinS[:], ALU.mult, ALU.mult, r=[bx2, bfst[q], bconst], w=[bx2])
            DMA("sp", out_d[i // NT, (i % NT) * 128:(i % NT + 1) * 128, :], x2f[q], r=[bx2], is_out=True)

        bfst = [Buf(f"fst{q}") for q in range(4)]
        P.inherit(bfst, [bxg[0], bxg[1], bS["xT"], bhe[0], bhe[1], bsg[0], bsg[1]])
        phase_bufs[0] = phase_bufs[0] + bfst
        for i in range(NTT + 3):
            if i < NTT:
                fin_load(i)
            if i >= 3:
                fin_compute(i - 3)
        P.emit()
    return nc


_CACHE = {}
_p = np.arange(128, dtype=np.float32)[:, None]
_CGU = np.zeros((128, 64), np.float32)
_CGU[:, 0:8] = np.arange(8, dtype=np.float32)[None, :] * 128 + _p
_CD = np.ascontiguousarray((np.arange(8, dtype=np.float32)[None, :, None] * 512 + np.arange(4, dtype=np.float32)[None, None, :] * 128
                            + _p[:, :, None]).reshape(128, 32).astype(np.float32))


def _host_consts():
    ident = np.eye(128, dtype=np.float32)
    tri = np.triu(np.ones((128, 128), np.float32))
    return ident, tri


def kernel(x, mem, g_mix, w_in, b_ml_i, b_ml_f, b_fx_f, b_gate_ml, b_gate_fx, g_ml_head,
           w_proj_ml, w_proj_fx, w_out, g_xq, g_xmem, w_xq, w_xkv, w_xo, g_moe,
           w_rg, b_rg, w_re, b_re, w_gate, w_up, w_down, g_final):
    f = lambda a: np.ascontiguousarray(np.asarray(a, dtype=np.float32))
    x = f(x)
    mem = f(mem)
    col = lambda v: f(v).reshape(8, 128).T
    cols = np.ascontiguousarray(np.concatenate(
        [col(g_mix[0]), col(g_xq[0]), col(g_xmem[0]), col(g_moe[0]), col(g_ml_head[0]),
         col(b_gate_ml[0]), col(b_gate_fx[0])], axis=1))
    rows = np.ascontiguousarray(np.concatenate(
        [f(b_ml_i[0]), f(b_ml_f[0]), f(b_fx_f[0]), f(b_rg[0]), f(b_re[0])])[None, :])
    w_r = np.ascontiguousarray(np.concatenate([f(w_rg[0]), f(w_re[0])], axis=1))
    ident, tri = _host_consts()
    shared = dict(
        w_in=f(w_in[0]), w_proj_ml=f(w_proj_ml[0]), w_proj_fx=f(w_proj_fx[0]), w_out=f(w_out[0]),
        w_xq=f(w_xq[0]), w_xkv=f(w_xkv[0]), w_xo=f(w_xo[0]), w_r=w_r,
        w_gate=np.ascontiguousarray(f(w_gate[0]).reshape(NE, 8, 128, DE).transpose(0, 2, 1, 3)).reshape(NE * 128, 8 * DE),
        w_up=np.ascontiguousarray(f(w_up[0]).reshape(NE, 8, 128, DE).transpose(0, 2, 1, 3)).reshape(NE * 128, 8 * DE),
        w_down=np.ascontiguousarray(f(w_down[0]).reshape(NE, 4, 128, D).transpose(0, 2, 1, 3)).reshape(NE * 128, 4 * D),
        cols=cols, rows=rows, g_final=f(g_final)[None, :], ident=ident, tri=tri,
        g_moe_row=f(g_moe[0])[None, :], cgu=_CGU, cd=_CD)
    if "nc" not in _CACHE:
        _CACHE["nc"] = build_program()
    nc = _CACHE["nc"]
    in_maps = []
    for c in range(8):
        m = dict(shared)
        m["x"] = np.ascontiguousarray(x[2 * c:2 * c + 2])
        m["mem"] = np.ascontiguousarray(mem[2 * c:2 * c + 2])
        in_maps.append(m)
    res = run_bass_kernel_spmd(nc, in_maps, core_ids=list(range(8)))
    out = np.concatenate([np.asarray(r["out"]) for r in res.results], axis=0)
    return out.astype(np.float32)
```

```python
import contextlib
import numpy as np
import concourse.bass as bass
import concourse.mybir as mybir
from concourse.bass_utils import run_bass_kernel_spmd

F32 = mybir.dt.float32
BF16 = mybir.dt.bfloat16
I32 = mybir.dt.int32
AF = mybir.ActivationFunctionType
ALU = mybir.AluOpType
AX = mybir.AxisListType

ENGS = ("pe", "dve", "act", "pool", "sp")
SEM_EPOCH = 20000
NDMA_SEMS = 12
STRICT_SAME_ENGINE = True


class Buf:
    __slots__ = ("name", "last_w", "readers", "wgroup")

    def __init__(self, name):
        self.name = name
        self.last_w = []
        self.readers = []
        self.wgroup = None


class Op:
    __slots__ = ("eng", "fn", "deps", "is_dma", "idx", "signal", "count", "dsem", "dval", "dprev", "seg", "kind", "aux")

    def __init__(self, eng, fn, is_dma):
        self.eng = eng
        self.fn = fn
        self.deps = []
        self.is_dma = is_dma
        self.idx = -1
        self.signal = False
        self.count = 0
        self.dsem = -1
        self.dval = 0
        self.dprev = 0
        self.seg = 0
        self.kind = "op"
        self.aux = None


class Prog:
    def __init__(self, nc):
        self.nc = nc
        self.streams = {e: [] for e in ENGS}
        self.ndma = {}
        self.dma_uses = {}
        self.out_dmas = []
        self.seg = 0
        self.cond_segs = set()
        self.markers = {}
        self.block_dmas = None

    def _redirect(self, d):
        if d.seg in self.cond_segs and d.seg != self.seg:
            return self.markers[d.seg][d.eng]
        return d

    def _add(self, eng, fn, reads, writes, is_dma, kind="op", group=None):
        op = Op(eng, fn, is_dma)
        op.seg = self.seg
        op.kind = kind
        st = self.streams[eng]
        op.idx = len(st)
        deps = []
        for b in reads:
            if b is not None:
                for w_ in b.last_w:
                    deps.append((w_, "raw"))
        for b in writes:
            if b is None:
                continue
            if not (group is not None and b.wgroup == group):
                for w_ in b.last_w:
                    deps.append((w_, "waw"))
            for r in b.readers:
                deps.append((r, "war"))
        latest = {}
        dma_deps = {}
        for (d, kind_) in deps:
            d = self._redirect(d)
            if d is op:
                continue
            if d.is_dma:
                dma_deps[id(d)] = d
                continue
            if d.eng == eng and not is_dma:
                if eng == "pe":
                    continue
                if not STRICT_SAME_ENGINE and (kind_ != "raw" or (op.idx - d.idx) > 2):
                    continue
            cur = latest.get(d.eng)
            if cur is None or d.idx > cur.idx:
                latest[d.eng] = d
        for d in list(latest.values()) + list(dma_deps.values()):
            op.deps.append(d)
            d.signal = True
        for b in reads:
            if b is not None:
                b.readers.append(op)
        for b in writes:
            if b is not None:
                if group is not None and b.wgroup == group:
                    b.last_w.append(op)
                else:
                    b.last_w = [op]
                    b.wgroup = group
                b.readers = []
        if is_dma:
            n = self.ndma.get((eng, self.seg), 0)
            k = n % NDMA_SEMS
            self.ndma[(eng, self.seg)] = n + 1
            key = (eng, self.seg, k)
            u = self.dma_uses.get(key, 0)
            op.dsem = key
            op.dprev = 16 * u
            op.dval = 16 * (u + 1)
            self.dma_uses[key] = u + 1
            if self.block_dmas is not None:
                self.block_dmas[eng].append(op)
        st.append(op)
        return op

    def op(self, eng, fn, reads=(), writes=()):
        return self._add(eng, fn, reads, writes, False)

    def dma(self, eng, fn, reads=(), writes=(), is_out=False, group=None):
        o = self._add(eng, fn, reads, writes, True, group=group)
        if is_out:
            self.out_dmas.append(o)
        return o

    def inherit(self, new_bufs, old_bufs):
        latest = {}
        dmas = {}
        for b in old_bufs:
            cand = list(b.readers) + list(b.last_w)
            for d in cand:
                d = self._redirect(d)
                if d.is_dma:
                    dmas[id(d)] = d
                else:
                    cur = latest.get(d.eng)
                    if cur is None or d.idx > cur.idx:
                        latest[d.eng] = d
        S = list(latest.values()) + list(dmas.values())
        for nb in new_bufs:
            nb.last_w = []
            nb.wgroup = None
            nb.readers = list(S)

    def if_begin(self, flag_ap, flag_buf):
        assert self.block_dmas is None
        for e in ENGS:
            o = self._add(e, None, [flag_buf], (), False, kind="ifb")
            o.aux = flag_ap
        self.seg += 1
        self.cond_segs.add(self.seg)
        self.block_dmas = {e: [] for e in ENGS}

    def if_end(self):
        cseg = self.seg
        for e in ENGS:
            o = self._add(e, None, (), (), False, kind="ife")
            o.aux = list(self.block_dmas[e])
        self.block_dmas = None
        self.seg += 1
        self.markers[cseg] = {}
        for e in ENGS:
            m = self._add(e, lambda h: h.drain(), (), (), False, kind="marker")
            self.markers[cseg][e] = m

    def emit(self):
        nc = self.nc
        fin = Op("sp", None, False)
        fin.seg = self.seg
        fin.idx = len(self.streams["sp"])
        for o in self.out_dmas:
            fin.deps.append(self._redirect(o))
            fin.deps[-1].signal = True
        self.streams["sp"].append(fin)
        nsem = {}
        for e in ENGS:
            c = {}
            for o in self.streams[e]:
                if o.is_dma:
                    continue
                if o.signal:
                    c[o.seg] = c.get(o.seg, 0) + 1
                    o.count = c[o.seg]
            for sg, v in c.items():
                nsem[(e, sg)] = (v - 1) // SEM_EPOCH + 1
        with contextlib.ExitStack() as es:
            sems = {}
            for (e, sg), n in nsem.items():
                for k in range(n):
                    sems[(e, sg, k)] = es.enter_context(nc.semaphore(f"s_{e}_{sg}_{k}"))
            dsems = {}
            for key in self.dma_uses:
                dsems[key] = es.enter_context(nc.semaphore(f"d_{key[0]}_{key[1]}_{key[2]}"))
            block = es.enter_context(nc.Block())
            handles = {"pe": nc.tensor, "dve": nc.vector, "act": nc.scalar, "pool": nc.gpsimd, "sp": nc.sync}

            def run(e):
                h = handles[e]
                state = {"waited": {}}
                stack = []

                def wait(semkey, sem, val):
                    w = state["waited"]
                    if w.get(semkey, 0) >= val:
                        return
                    w[semkey] = val
                    h.wait_ge(sem, val)

                for o in self.streams[e]:
                    for d in o.deps:
                        if d.is_dma:
                            wait(("d",) + d.dsem, dsems[d.dsem], d.dval)
                        else:
                            ep = (d.count - 1) // SEM_EPOCH
                            wait((d.eng, d.seg, ep), sems[(d.eng, d.seg, ep)], d.count - ep * SEM_EPOCH)
                    if o.kind == "ifb":
                        v = h.value_load(o.aux, min_val=0, max_val=1)
                        g = h.If(v)
                        g.__enter__()
                        stack.append((g, dict(state["waited"])))
                    elif o.kind == "ife":
                        for d in o.aux:
                            wait(("d",) + d.dsem, dsems[d.dsem], d.dval)
                        g, snap = stack.pop()
                        g.__exit__(None, None, None)
                        state["waited"] = snap
                    elif o.is_dma:
                        if o.dprev > 0:
                            wait(("d",) + o.dsem, dsems[o.dsem], o.dprev)
                        ins = o.fn(h)
                        ins.then_inc(dsems[o.dsem], 16)
                    elif o.fn is not None:
                        ins = o.fn(h)
                        if o.signal:
                            ep = (o.count - 1) // SEM_EPOCH
                            ins.then_inc(sems[(e, o.seg, ep)], 1)

            @block.tensor
            def _(eng):
                run("pe")

            @block.vector
            def _(eng):
                run("dve")

            @block.scalar
            def _(eng):
                run("act")

            @block.gpsimd
            def _(eng):
                run("pool")

            @block.sync
            def _(eng):
                run("sp")


D = 1024
S = 2048
NB = 2
NT = S // 128
NMEM = 256
EPS = 1e-6
NE = 32
DE = 512
C_MLQ, C_MLK, C_MLV, C_MLO, C_MLI, C_MLF = 0, 512, 1024, 2048, 3072, 3076
C_FXQ, C_FXK, C_FXV, C_FXF, C_GML, C_GFX = 3080, 4104, 5128, 6152, 6160, 7184
D_IN = 8208
CG_MIX, CG_XQ, CG_XMEM, CG_MOE, CG_MLH, CB_GML, CB_GFX = 0, 8, 16, 24, 32, 40, 48
R_MLI, R_MLF, R_FXF, R_RG, R_RE = 0, 4, 8, 16, 20
NROWS = 52
DENSE_MOE = False
NTT = NB * NT
XW = 1048
NGT = 11
DEBUG = False


def build_program(n_experts_run=NE):
    nc = bass.Bass("TRN2", target_bir_lowering=False)
    dt = lambda name, shape, dtype=F32, kind="ExternalInput": nc.dram_tensor(name, shape, dtype, kind=kind).ap()
    x_d = dt("x", [NB, S, D])
    mem_d = dt("mem", [NB, NMEM, D])
    w_in_d = dt("w_in", [D, D_IN])
    w_pm_d = dt("w_proj_ml", [D, D])
    w_pf_d = dt("w_proj_fx", [D, D])
    w_out_d = dt("w_out", [D, D])
    w_xq_d = dt("w_xq", [D, D])
    w_xkv_d = dt("w_xkv", [D, 2 * D])
    w_xo_d = dt("w_xo", [D, D])
    w_r_d = dt("w_r", [D, 36])
    w_gate_d = dt("w_gate", [NE * 128, 8 * DE])
    w_up_d = dt("w_up", [NE * 128, 8 * DE])
    w_down_d = dt("w_down", [NE * 128, 4 * D])
    cols_d = dt("cols", [128, 56])
    rows_d = dt("rows", [1, NROWS])
    gfin_d = dt("g_final", [1, D])
    ident_d = dt("ident", [128, 128])
    tri_d = dt("tri", [128, 128])
    out_d = dt("out", [NB, S, D], F32, "ExternalOutput")
    ymlT_d = dt("ymlT_scr", [NB, NT, 128, 1024], BF16, "Internal")
    yfxT_d = dt("yfxT_scr", [NB, 8, 128, S], BF16, "Internal")
    x1_d = dt("x1_scr", [NB, NT, 128, 1024], F32, "Internal")
    x2_d = dt("x2_scr", [NB, NT, 128, 1024], F32, "Internal")
    gmoe_d = dt("g_moe_row", [1, D])
    cgu_d = dt("cgu", [128, 64])
    cd_d = dt("cd", [128, 32])
    NSL = NGT * 512
    dbg_d = dt("dbg", [NB, 128, 64], F32, "ExternalOutput") if DEBUG else None
    xn3tm_d = dt("xn3tm_scr", [NB, NT, 128, 1024], BF16, "Internal")
    Xs_d = dt("xs_scr", [NSL, XW], BF16, "Internal")
    C8s_d = dt("c8s_scr", [NSL, 128], F32, "Internal")
    Ys_d = dt("ys_scr", [NSL, D], F32, "Internal")
    wg_rows, wu_rows, wd_rows = w_gate_d, w_up_d, w_down_d

    P = Prog(nc)
    with contextlib.ExitStack() as es:
        sb = lambda n, s, d: es.enter_context(nc.sbuf_tensor(n, s, d))
        ps = lambda n, s, d: es.enter_context(nc.psum_tensor(n, s, d))
        ASZ = 35840
        ARENA = sb("ARENA", [128, ASZ], F32)
        ARB = ARENA[:, :].bitcast(BF16)
        LG_OFF = ASZ - 576 * NB
        A32 = sb("A32", [128, 8, S], BF16)
        idf = sb("idf", [128, 128], F32)
        idb = sb("idb", [128, 128], BF16)
        trif = sb("trif", [128, 128], F32)
        onesf = sb("onesf", [128, 128], F32)
        selb = sb("selb", [128, 128], BF16)
        strib = sb("strib", [128, 128], BF16)
        onesb = sb("onesb", [128, 128], BF16)
        gmoeB = sb("gmoeB", [128, D], F32)
        cguS = sb("cguS", [128, 64], F32)
        cdS = sb("cdS", [128, 32], F32)
        ARI = ARENA[:, :].bitcast(I32)
        colsS = sb("colsS", [128, 56], F32)
        rowsS = sb("rowsS", [128, NROWS], F32)
        gfinS = sb("gfin", [128, D], F32)
        epsc = sb("epsc", [128, 1], F32)
        onec = sb("onec", [128, 1], F32)
        wrS = sb("wr", [128, 8, 36], F32)
        xt = [sb(f"xt{i}", [128, D], F32) for i in range(2)]
        junk = sb("junk", [128, D], BF16)
        xs_b = [sb(f"xsb{i}", [128, D], BF16) for i in range(2)]
        xs_f = sb("xsf", [128, D], F32)
        st_ssq = [sb(f"ssq{i}", [128, 1], F32) for i in range(2)]
        st_ln = [sb(f"lnv{i}", [128, 1], F32) for i in range(2)]
        st_rs = [sb(f"rstd{i}", [128, 1], F32) for i in range(2)]
        PSF = ps("PSF", [128, 6, 512], F32)
        PSB = ps("PSB", [128, 2, 1024], BF16)

        bA32t = [Buf(f"A32_{i}") for i in range(NT)]
        bconst = Buf("const")
        bxt = [Buf("xt0"), Buf("xt1")]
        bjunk = Buf("junk")
        bxsb = [Buf("xsb0"), Buf("xsb1")]
        bxsf = Buf("xsf")
        bst = [Buf("st0"), Buf("st1")]
        bF = [Buf(f"PSF{i}") for i in range(6)]
        bB = [Buf(f"PSB{i}") for i in range(2)]
        bymlT_d = [[Buf(f"ymlTd{b}_{i}") for i in range(NT)] for b in range(NB)]
        byfxT_d = [[Buf(f"yfxTd{b}_{i}") for i in range(8)] for b in range(NB)]
        bx1_d = [[Buf(f"x1d{b}_{i}") for i in range(NT)] for b in range(NB)]
        bx2_d = [[Buf(f"x2d{b}_{i}") for i in range(NT)] for b in range(NB)]
        bxn3_d = [[Buf(f"xn3d{b}_{i}") for i in range(NT)] for b in range(NB)]
        bXs = [Buf(f"Xs{i}") for i in range(NGT)]
        bC8s = [Buf(f"C8s{i}") for i in range(7)]
        bYs = [Buf(f"Ys{i}") for i in range(NGT)]
        blg = Buf("lg")
        lg = ARENA[:, LG_OFF:LG_OFF + 576 * NB].rearrange("p (t n) -> p t n", n=36)

        def DMA(q, out, in_, r=(), w=(), is_out=False, group=None):
            return P.dma(q, lambda e: e.dma_start(out=out, in_=in_), r, w, is_out, group=group)

        def MM(out, lhsT, rhs, start=True, stop=True, r=(), w=()):
            return P.op("pe", lambda e: e.matmul(out, lhsT=lhsT, rhs=rhs, start=start, stop=stop), r, w)

        def TR(out, in_, r=(), w=()):
            return P.op("pe", lambda e: e.transpose(out=out, in_=in_, identity=idb[:]), list(r) + [bconst], w)

        def ACT(out, in_, func, bias=None, scale=1.0, accum=None, r=(), w=()):
            def f(e):
                kw = {}
                if bias is not None:
                    kw["bias"] = bias
                if accum is not None:
                    kw["accum_out"] = accum
                return e.activation(out=out, in_=in_, func=func, scale=scale, **kw)
            return P.op("act", f, r, w)

        def TS(eng, out, in0, s1, s2, op0, op1=None, r=(), w=()):
            def f(e):
                if op1 is None:
                    return e.tensor_scalar(out=out, in0=in0, scalar1=s1, scalar2=None, op0=op0)
                return e.tensor_scalar(out=out, in0=in0, scalar1=s1, scalar2=s2, op0=op0, op1=op1)
            return P.op(eng, f, r, w)

        def STT(eng, out, in0, scalar, in1, op0, op1, r=(), w=()):
            return P.op(eng, lambda e: e.scalar_tensor_tensor(out=out, in0=in0, scalar=scalar, in1=in1, op0=op0, op1=op1), r, w)

        def TT(eng, out, in0, in1, op, r=(), w=()):
            return P.op(eng, lambda e: e.tensor_tensor(out=out, in0=in0, in1=in1, op=op), r, w)

        def CP(eng, out, in_, r=(), w=()):
            if eng == "act":
                return P.op("act", lambda e: e.copy(out=out, in_=in_), r, w)
            return P.op(eng, lambda e: e.tensor_copy(out=out, in_=in_), r, w)

        def MSET(eng, ap, val, w=()):
            return P.op(eng, lambda e: e.memset(ap, val), (), w)

        def RED(eng, out, in_, op, r=(), w=()):
            return P.op(eng, lambda e: e.tensor_reduce(out=out, in_=in_, axis=AX.X, op=op), r, w)

        def RECIP(out, in_, r=(), w=()):
            return P.op("dve", lambda e: e.reciprocal(out=out, in_=in_), r, w)

        scr_off = [0]
        scr_lim = [LG_OFF]
        phase_bufs = [[]]

        def carve_f(n):
            o = scr_off[0]
            scr_off[0] += n
            assert scr_off[0] <= scr_lim[0], (scr_off[0], scr_lim[0])
            return ARENA[:, o:o + n]

        def carve_b(n):
            assert n % 2 == 0
            o = scr_off[0]
            scr_off[0] += n // 2
            assert scr_off[0] <= scr_lim[0], (scr_off[0], scr_lim[0])
            return ARB[:, 2 * o:2 * o + n]

        def new_phase(names, base, manual=None):
            manual = manual or {}
            bs = {n: Buf(n) for n in names}
            P.inherit([v for n, v in bs.items() if n not in manual], phase_bufs[0])
            for n, olds in manual.items():
                P.inherit([bs[n]], olds)
            phase_bufs[0] = list(bs.values())
            scr_off[0] = base
            return bs

        WBv = lambda off, nk, ncols: ARB[:, off:off + nk * ncols].rearrange("p (k n) -> p k n", n=ncols)
        SCR0 = 24640
        SCR5 = 16384
        last_p6 = [[]]
        keep = {}

        DMA("sp", idf[:], ident_d[:, :], w=[bconst])
        DMA("sp", trif[:], tri_d[:, :], w=[bconst])
        DMA("sp", colsS[:], cols_d[:, :], w=[bconst])
        DMA("sp", rowsS[:], rows_d.partition_broadcast(128), w=[bconst])
        DMA("sp", gfinS[:], gfin_d.partition_broadcast(128), w=[bconst])
        DMA("sp", wrS[:], w_r_d.rearrange("(k p) n -> p k n", p=128), w=[bconst])
        MSET("pool", onesf[:], 1.0, w=[bconst])
        MSET("pool", epsc[:], EPS, w=[bconst])
        MSET("pool", onec[:], 1.0, w=[bconst])
        CP("dve", idb[:], idf[:], r=[bconst], w=[bconst])
        DMA("sp", gmoeB[:], gmoe_d.partition_broadcast(128), w=[bconst])
        DMA("sp", cguS[:], cgu_d[:, :], w=[bconst])
        DMA("sp", cdS[:], cd_d[:, :], w=[bconst])
        TT("dve", strib[:], trif[:], idf[:], ALU.subtract, r=[bconst], w=[bconst])
        CP("dve", onesb[:], onesf[:], r=[bconst], w=[bconst])
        MSET("dve", selb[:], 0.0, w=[bconst])
        for p0 in (0, 32, 64):
            MSET("dve", selb[p0:p0 + 1, :], 1.0, w=[bconst])

        nt_ctr = [0]

        def norm_transpose(src, src_bufs, gcol, dstT, dst_bufs, fp32=False, dstT_b=None, dstb_bufs=(), tm_store=None):
            i = nt_ctr[0] % 2
            nt_ctr[0] += 1
            ssq, lnv, rstd = st_ssq[i], st_ln[i], st_rs[i]
            ACT(junk[:], src, AF.Square, accum=ssq[:], r=list(src_bufs), w=[bjunk, bst[i]])
            ACT(lnv[:], ssq[:], AF.Ln, bias=epsc[:, 0:1], scale=1.0 / D, r=[bst[i], bconst], w=[bst[i]])
            ACT(rstd[:], lnv[:], AF.Exp, scale=-0.5, r=[bst[i]], w=[bst[i]])
            gb = gcol.unsqueeze(2).to_broadcast([128, 8, 128])
            if not fp32:
                xs = xs_b[i]
                TS("dve", xs[:], src, rstd[:, 0:1], None, ALU.mult, r=list(src_bufs) + [bst[i]], w=[bxsb[i]])
                for k in range(8):
                    TR(PSB[:, i, k * 128:(k + 1) * 128], xs[:, k * 128:(k + 1) * 128], r=[bxsb[i]], w=[bB[i]])
                pv = PSB[:, i, :].rearrange("p (k t) -> p k t", t=128)
                TT("dve", dstT, pv, gb, ALU.mult, r=[bB[i], bconst], w=list(dst_bufs))
            else:
                TS("dve", xs_f[:], src, rstd[:, 0:1], None, ALU.mult, r=list(src_bufs) + [bst[i]], w=[bxsf])
                for k in range(8):
                    MM(PSF[:, 4 + k // 4, (k % 4) * 128:(k % 4 + 1) * 128], xs_f[:, k * 128:(k + 1) * 128], idf[:],
                       r=[bxsf, bconst], w=[bF[4 + k // 4]])
                pv = PSF[:, 4:6, :].rearrange("p a (c t) -> p (a c) t", t=128)
                TT("dve", dstT, pv, gb, ALU.mult, r=[bF[4], bF[5], bconst], w=list(dst_bufs))
                if dstT_b is not None:
                    CP("act", dstT_b, dstT, r=list(dst_bufs), w=list(dstb_bufs))
                if tm_store is not None:
                    TT("pool", xs_b[i][:], xs_f[:], gmoeB[:], ALU.mult, r=[bxsf, bconst], w=[bxsb[i]])
                    DMA("sp", tm_store[0], xs_b[i][:], r=[bxsb[i]], w=[tm_store[1]])

        grp_ctr = [0]

        def load_w(dst_view, src, wbufs, nk=8):
            grp_ctr[0] += 1
            gid_ = ("lw", grp_ctr[0])
            for k in range(nk):
                P.dma("pool", (lambda o_, i_: (lambda e: e.dma_start(out=o_, in_=i_)))(dst_view[:, k, :], src[k * 128:(k + 1) * 128, :]),
                      (), list(wbufs), group=gid_)

        for b in range(NB):
            def p1_tile(bb, i):
                DMA("sp", xt[i % 2][:], x_d[bb, i * 128:(i + 1) * 128, :], w=[bxt[i % 2]])
                norm_transpose(xt[i % 2][:], [bxt[i % 2]], colsS[:, CG_MIX:CG_MIX + 8],
                               A32[:, :, i * 128:(i + 1) * 128], [bA32t[i]])

            if b == 0:
                for i in range(NT):
                    p1_tile(0, i)
                zt = xs_b[0]
                MSET("dve", zt[:], 0.0, w=[bxsb[0]])
                for r0 in range(0, NSL, 128):
                    DMA("sp", Xs_d[r0:r0 + 128, 0:1024], zt[:], r=[bxsb[0]], w=bXs, group=("zero",))
                    DMA("sp", Xs_d[r0:r0 + 128, 1024:XW], zt[:, 0:XW - 1024], r=[bxsb[0]], w=bXs, group=("zero",))

            bS = new_phase("W CnT Cnb vext0 vext1 qT0 qT1 kT0 kT1 kw0 kw1 PT sig0 sig1 yb0 yb1 ymlT gsm gA".split(), SCR0)
            bW = [bS["W"]]
            keep["Wml"] = bS["W"]
            wml = WBv(0, 8, 3080)
            load_w(wml, w_in_d[:, 0:3080], bW)
            bWfx = Buf("Wfx")
            P.inherit([bWfx], last_p6[0])
            wfx = WBv(24640, 8, 3080)
            load_w(wfx, w_in_d[:, C_FXQ:C_FXQ + 3080], [bWfx])
            CnT = carve_f(4 * 258).rearrange("p (h n) -> p h n", n=258)
            Cnb = carve_b(4 * 258).rearrange("p (h n) -> p h n", n=258)
            vext2 = [carve_b(4 * 258).rearrange("p (h n) -> p h n", n=258) for _ in range(2)]
            qT2 = [carve_b(512).rearrange("p (h n) -> p h n", n=128) for _ in range(2)]
            kT2 = [carve_b(512).rearrange("p (h n) -> p h n", n=128) for _ in range(2)]
            kw2 = [carve_b(512).rearrange("p (h n) -> p h n", n=128) for _ in range(2)]
            PT = carve_b(128)
            sig2 = [carve_f(1024) for _ in range(2)]
            yb2 = [carve_b(1024) for _ in range(2)]
            ymlT_t = carve_b(1024).rearrange("p (k t) -> p k t", t=128)
            gsm = carve_f(64)
            d1, sc, sa, t2, tot = gsm[:, 28:32], gsm[:, 32:36], gsm[:, 36:40], gsm[:, 40:44], gsm[:, 44:48]
            gA = carve_f(64 * 7)
            gi_a, l1_a, tmp_a, eb_a, dec_a, wv_a, wd_a = [gA[:, 64 * q:64 * (q + 1)] for q in range(7)]
            G_ = [bS["gsm"]]
            bGA = [bS["gA"]]
            MSET("dve", CnT, 0.0, w=[bS["CnT"]])
            MSET("dve", Cnb, 0.0, w=[bS["Cnb"]])
            for q in range(2):
                MSET("dve", vext2[q], 1.0, w=[bS[f"vext{q}"]])
            for c in range(NT):
                for k in range(8):
                    MM(PSF[:, 5, c * 8:(c + 1) * 8], A32[:, k, c * 128:(c + 1) * 128], wml[:, k, C_MLI:C_MLI + 8], k == 0, k == 7,
                       r=[bA32t[c]] + bW, w=[bF[5]])
            pg3 = PSF[:, 5, 0:128].rearrange("p (t g) -> p t g", g=8)
            v3 = lambda ap: ap.rearrange("p (t h) -> p t h", h=4)
            TT("dve", v3(gi_a), pg3[:, :, 0:4], rowsS[:, R_MLI:R_MLI + 4].unsqueeze(1).to_broadcast([128, 16, 4]), ALU.add,
               r=[bF[5], bconst], w=bGA)
            TT("dve", v3(l1_a), pg3[:, :, 4:8], rowsS[:, R_MLF:R_MLF + 4].unsqueeze(1).to_broadcast([128, 16, 4]), ALU.add,
               r=[bF[5], bconst], w=bGA)
            ACT(l1_a, l1_a, AF.Exp, scale=-1.0, r=bGA, w=bGA)
            ACT(l1_a, l1_a, AF.Ln, bias=onec[:, 0:1], r=bGA + [bconst], w=bGA)
            MM(PSF[:, 4, 0:64], trif[:], l1_a, r=bGA + [bconst], w=[bF[4]])
            MM(PSF[:, 4, 64:128], onesf[:], l1_a, r=bGA + [bconst], w=[bF[4]])
            ACT(eb_a, PSF[:, 4, 0:64], AF.Exp, scale=-1.0, r=[bF[4]], w=bGA)
            ACT(dec_a, PSF[:, 4, 64:128], AF.Exp, scale=-1.0, r=[bF[4]], w=bGA)
            TT("dve", tmp_a, gi_a, PSF[:, 4, 0:64], ALU.add, r=bGA + [bF[4]], w=bGA)
            ACT(wv_a, tmp_a, AF.Exp, r=bGA, w=bGA)
            TT("dve", tmp_a, tmp_a, PSF[:, 4, 64:128], ALU.subtract, r=bGA + [bF[4]], w=bGA)
            ACT(wd_a, tmp_a, AF.Exp, r=bGA, w=bGA)

            def p2_proj(c):
                q = c % 2
                tsl = slice(c * 128, (c + 1) * 128)
                qT, kT, kw, vext, sig = qT2[q], kT2[q], kw2[q], vext2[q], sig2[q]
                wd = wd_a[:, 4 * c:4 * c + 4]
                rA = [bA32t[c]] + bW
                for h in range(4):
                    for k in range(8):
                        MM(PSF[:, 4, h * 128:(h + 1) * 128], wml[:, k, C_MLQ + h * 128:C_MLQ + (h + 1) * 128], A32[:, k, tsl],
                           k == 0, k == 7, r=rA, w=[bF[4]])
                ACT(qT, PSF[:, 4, :].rearrange("p (h n) -> p h n", n=128), AF.Identity, scale=128.0 ** -0.5, r=[bF[4]], w=[bS[f"qT{q}"]])
                for h in range(4):
                    for k in range(8):
                        MM(PSF[:, 5, h * 128:(h + 1) * 128], wml[:, k, C_MLK + h * 128:C_MLK + (h + 1) * 128], A32[:, k, tsl],
                           k == 0, k == 7, r=rA, w=[bF[5]])
                CP("dve", kT, PSF[:, 5, :].rearrange("p (h n) -> p h n", n=128), r=[bF[5]], w=[bS[f"kT{q}"]])
                for k in range(8):
                    MM(PSF[:, 4, :], A32[:, k, tsl], wml[:, k, C_MLK:C_MLK + 512], k == 0, k == 7, r=rA, w=[bF[4]])
                TT("dve", kw, PSF[:, 4, :].rearrange("p (h n) -> p h n", n=128), wd.unsqueeze(2).to_broadcast([128, 4, 128]),
                   ALU.mult, r=[bF[4]] + bGA, w=[bS[f"kw{q}"]])
                for hf in range(2):
                    pb_ = 5 - hf
                    for k in range(8):
                        MM(PSF[:, pb_, :], A32[:, k, tsl], wml[:, k, C_MLV + hf * 512:C_MLV + (hf + 1) * 512], k == 0, k == 7,
                           r=rA, w=[bF[pb_]])
                    CP("act", vext[:, 2 * hf:2 * hf + 2, 0:256], PSF[:, pb_, :].rearrange("p (h n) -> p h n", n=256),
                       r=[bF[pb_]], w=[bS[f"vext{q}"]])
                for hf in range(2):
                    pb_ = 5 - hf
                    for k in range(8):
                        MM(PSF[:, pb_, :], A32[:, k, tsl], wml[:, k, C_MLO + hf * 512:C_MLO + (hf + 1) * 512], k == 0, k == 7,
                           r=rA, w=[bF[pb_]])
                    ACT(sig[:, hf * 512:(hf + 1) * 512], PSF[:, pb_, :], AF.Sigmoid, r=[bF[pb_]], w=[bS[f"sig{q}"]])

            def p2_mix(c):
                q = c % 2
                qT, kT, kw, vext, sig, yb = qT2[q], kT2[q], kw2[q], vext2[q], sig2[q], yb2[q]
                eb, dec, wv_ = eb_a[:, 4 * c:4 * c + 4], dec_a[:, 4 * c:4 * c + 4], wv_a[:, 4 * c:4 * c + 4]
                for h in range(4):
                    MM(PSF[:, 4, 0:128], kT[:, h, :], qT[:, h, :], r=[bS[f"kT{q}"], bS[f"qT{q}"]], w=[bF[4]])
                    STT("dve", PT, PSF[:, 4, 0:128], wv_[:, h:h + 1], trif[:], ALU.mult, ALU.mult,
                        r=[bF[4], bconst] + bGA, w=[bS["PT"]])
                    MM(PSF[:, h, 0:257], PT, vext[:, h, 0:257], True, False, r=[bS["PT"], bS[f"vext{q}"]], w=[bF[h]])
                    MM(PSF[:, h, 0:257], qT[:, h, :], Cnb[:, h, 0:257], False, True, r=[bS[f"qT{q}"], bS["Cnb"]], w=[bF[h]])
                for h in range(4):
                    pb_ = 5 - (h % 2)
                    MM(PSF[:, pb_, 0:257], kw[:, h, :], vext[:, h, 0:257], r=[bS[f"kw{q}"], bS[f"vext{q}"]], w=[bF[pb_]])
                    STT("dve", CnT[:, h, 0:257], CnT[:, h, 0:257], dec[:, h:h + 1], PSF[:, pb_, 0:257], ALU.mult, ALU.add,
                        r=[bS["CnT"], bF[pb_]] + bGA, w=[bS["CnT"]])
                    CP("act", Cnb[:, h, 0:257], CnT[:, h, 0:257], r=[bS["CnT"]], w=[bS["Cnb"]])
                den = PSF[:, 0:4, 256]
                CP("dve", d1, den, r=bF[0:4], w=G_)
                STT("dve", d1, d1, -1.0, d1, ALU.mult, ALU.max, r=G_, w=G_)
                TT("dve", d1, d1, eb, ALU.mult, r=G_ + bGA, w=G_)
                TS("dve", d1, d1, 1.0, None, ALU.max, r=G_, w=G_)
                RECIP(d1, d1, r=G_, w=G_)
                TT("dve", sc, d1, eb, ALU.mult, r=G_ + bGA, w=G_)
                for h in range(4):
                    ACT(junk[:, 0:256], PSF[:, h, 0:256], AF.Square, accum=sa[:, h:h + 1], r=[bF[h]], w=[bjunk] + G_)
                TT("dve", t2, sc, sc, ALU.mult, r=G_, w=G_)
                TT("dve", t2, t2, sa, ALU.mult, r=G_, w=G_)
                ACT(t2, t2, AF.Ln, bias=epsc[:, 0:1], scale=1.0 / 256, r=G_ + [bconst], w=G_)
                ACT(t2, t2, AF.Exp, scale=-0.5, r=G_, w=G_)
                TT("dve", tot, t2, sc, ALU.mult, r=G_, w=G_)
                for h in range(4):
                    STT("dve", yb[:, h * 256:(h + 1) * 256], PSF[:, h, 0:256], tot[:, h:h + 1], sig[:, h * 256:(h + 1) * 256],
                        ALU.mult, ALU.mult, r=[bF[h], bS[f"sig{q}"]] + G_, w=[bS[f"yb{q}"]])

            def p2_store(c):
                q = c % 2
                yb = yb2[q]
                for k in range(8):
                    TR(PSB[:, 0, k * 128:(k + 1) * 128], yb[:, k * 128:(k + 1) * 128], r=[bS[f"yb{q}"]], w=[bB[0]])
                TT("dve", ymlT_t, PSB[:, 0, :].rearrange("p (k t) -> p k t", t=128),
                   colsS[:, CG_MLH:CG_MLH + 8].unsqueeze(2).to_broadcast([128, 8, 128]), ALU.mult,
                   r=[bB[0], bconst], w=[bS["ymlT"]])
                DMA("sp", ymlT_d[b, c, :, :], ymlT_t.rearrange("p k t -> p (k t)"), r=[bS["ymlT"]], w=[bymlT_d[b][c]])

            for c in range(NT + 1):
                if c < NT:
                    p2_proj(c)
                if c >= 1:
                    p2_store(c - 1)
                if c < NT:
                    p2_mix(c)

            bS = new_phase("kTh qTh vxh l1f af cend biasall PT0 PT1 rcol yft0 yft1 yo0 yo1 Rr0 Rr1".split(), SCR0)
            bS["W"] = bWfx
            phase_bufs[0] = phase_bufs[0] + [bWfx]
            bW = [bWfx]
            bW40, bW41 = Buf("W40"), Buf("W41")
            P.inherit([bW40, bW41], [keep["Wml"]])
            wg = WBv(0, 8, 2048)
            wpm = WBv(16384, 8, 1024)
            load_w(wg, w_in_d[:, C_GML:C_GML + 2048], [bW40])
            load_w(wpm, w_pm_d, [bW41])
            kTh = carve_b(2048)
            qTh = carve_b(2048)
            vxh = carve_b(16 * 130).rearrange("p (t n) -> p t n", n=130)
            l1f = carve_f(128)
            af = carve_f(128).rearrange("p (t h) -> p t h", h=8)
            cend = carve_f(128).rearrange("p (t h) -> p t h", h=8)
            ncf = carve_f(128)
            tmpf = carve_f(128)
            Rall = carve_b(128)
            tmpb = carve_b(128)
            Rrow = [carve_b(512).rearrange("p (j t) -> p j t", t=128) for _ in range(2)]
            PTf = [carve_b(512).rearrange("p (j t) -> p j t", t=128) for _ in range(2)]
            rcol = carve_f(4)
            yft = [carve_b(128) for _ in range(2)]
            yfo = [carve_b(2048) for _ in range(2)]
            for i in range(NT):
                for k in range(8):
                    MM(PSF[:, 0, i * 8:(i + 1) * 8], A32[:, k, i * 128:(i + 1) * 128], wfx[:, k, 3072:3080], k == 0, k == 7,
                       r=[bA32t[i]] + bW, w=[bF[0]])
            TT("dve", l1f.rearrange("p (t h) -> p t h", h=8), PSF[:, 0, 0:128].rearrange("p (t h) -> p t h", h=8),
               rowsS[:, R_FXF:R_FXF + 8].unsqueeze(1).to_broadcast([128, 16, 8]), ALU.add, r=[bF[0], bconst], w=[bS["l1f"]])
            ACT(l1f, l1f, AF.Exp, scale=-1.0, r=[bS["l1f"]], w=[bS["l1f"]])
            ACT(l1f, l1f, AF.Ln, bias=onec[:, 0:1], r=[bS["l1f"], bconst], w=[bS["l1f"]])
            MM(PSF[:, 1, 0:128], trif[:], l1f, r=[bS["l1f"], bconst], w=[bF[1]])
            MM(PSF[:, 2, 0:128], onesf[:], l1f, r=[bS["l1f"], bconst], w=[bF[2]])
            pTt = PSF[:, 2, 0:128].rearrange("p (t h) -> p t h", h=8)
            CP("dve", cend[:, 0, :], pTt[:, 0, :], r=[bF[2]], w=[bS["cend"]])
            for m in range(1, NT):
                TT("dve", cend[:, m, :], cend[:, m - 1, :], pTt[:, m, :], ALU.add, r=[bF[2], bS["cend"]], w=[bS["cend"]])
            TT("dve", af, cend, pTt, ALU.subtract, r=[bS["cend"], bF[2]], w=[bS["af"]])
            TT("dve", af, af, PSF[:, 1, 0:128].rearrange("p (t h) -> p t h", h=8), ALU.add, r=[bS["af"], bF[1]], w=[bS["af"]])
            cflat = cend.rearrange("p t h -> p (t h)")
            TS("dve", ncf, cflat, -1.0, None, ALU.mult, r=[bS["cend"]], w=[bS["biasall"]])
            CP("dve", Rall, ncf, r=[bS["biasall"]], w=[bS["biasall"]])
            CP("dve", tmpf, Rall, r=[bS["biasall"]], w=[bS["biasall"]])
            TT("dve", ncf, ncf, tmpf, ALU.subtract, r=[bS["biasall"]], w=[bS["biasall"]])
            CP("dve", Rall[32:64, :], ncf[32:64, :], r=[bS["biasall"]], w=[bS["biasall"]])
            CP("dve", tmpb, ncf, r=[bS["biasall"]], w=[bS["biasall"]])
            CP("dve", tmpf, tmpb, r=[bS["biasall"]], w=[bS["biasall"]])
            TT("dve", ncf, ncf, tmpf, ALU.subtract, r=[bS["biasall"]], w=[bS["biasall"]])
            CP("dve", Rall[64:96, :], ncf[64:96, :], r=[bS["biasall"]], w=[bS["biasall"]])
            Rall3 = Rall.rearrange("p (t h) -> p t h", h=8)
            MSET("dve", vxh, 1.0, w=[bS["vxh"]])
            pti = 0
            for h in range(8):
                yo = yfo[h % 2]
                byo = bS[f"yo{h % 2}"]
                for G in range(4):
                    gsl = slice(G * 512, (G + 1) * 512)
                    for k in range(8):
                        MM(PSF[:, 0, :], wfx[:, k, 1024 + h * 128:1024 + (h + 1) * 128], A32[:, k, gsl], k == 0, k == 7,
                           r=bA32t[4 * G:4 * G + 4] + bW, w=[bF[0]])
                    CP("dve", kTh[:, gsl], PSF[:, 0, :], r=[bF[0]], w=[bS["kTh"]])
                    for k in range(8):
                        MM(PSF[:, 1, :], wfx[:, k, h * 128:(h + 1) * 128], A32[:, k, gsl], k == 0, k == 7,
                           r=bA32t[4 * G:4 * G + 4] + bW, w=[bF[1]])
                    ACT(qTh[:, gsl], PSF[:, 1, :], AF.Identity, scale=128.0 ** -0.5, r=[bF[1]], w=[bS["qTh"]])
                    for ti in range(4):
                        i = G * 4 + ti
                        for k in range(8):
                            MM(PSF[:, 2, ti * 128:(ti + 1) * 128], A32[:, k, i * 128:(i + 1) * 128],
                               wfx[:, k, 2048 + h * 128:2048 + (h + 1) * 128], k == 0, k == 7, r=[bA32t[i]] + bW, w=[bF[2]])
                    CP("dve", vxh[:, G * 4:(G + 1) * 4, 0:128], PSF[:, 2, :].rearrange("p (t n) -> p t n", n=128),
                       r=[bF[2]], w=[bS["vxh"]])
                steps = [(J, kb) for J in range(4) for kb in range(4 * J + 4)]

                def emit_qk(si):
                    J, kb = steps[si]
                    jmin = max(0, kb - 4 * J)
                    nq = 4 - jmin
                    if kb == 0:
                        CP("dve", Rrow[J % 2], Rall3[:, 4 * J:4 * J + 4, h:h + 1].to_broadcast([128, 4, 128]),
                           r=[bS["biasall"]], w=[bS[f"Rr{J % 2}"]])
                    MM(PSF[:, si % 2, 0:nq * 128], kTh[:, kb * 128:(kb + 1) * 128],
                       qTh[:, (4 * J + jmin) * 128:(4 * J + 4) * 128], True, False, r=[bS["kTh"], bS["qTh"]], w=[bF[si % 2]])
                    MM(PSF[:, si % 2, 0:nq * 128], selb[:], Rrow[J % 2].rearrange("p j t -> p (j t)")[:, jmin * 128:512], False, True,
                       r=[bconst, bS[f"Rr{J % 2}"]], w=[bF[si % 2]])

                def emit_rest(si):
                    J, kb = steps[si]
                    jmin = max(0, kb - 4 * J)
                    sb_i = si % 2
                    ptb = PTf[si % 2]
                    ptflat = ptb.rearrange("p j t -> p (j t)")
                    bpt = bS[f"PT{si % 2}"]
                    ACT(ptflat[:, jmin * 128:512], PSF[:, sb_i, 0:(4 - jmin) * 128], AF.Exp,
                        bias=af[:, kb, h:h + 1], r=[bF[sb_i], bS["af"]], w=[bpt])
                    for jj in range(jmin, 4):
                        if kb == 4 * J + jj:
                            TT("pool", ptb[:, jj, :], ptb[:, jj, :], trif[:], ALU.mult, r=[bpt, bconst], w=[bpt])
                    for jj in range(jmin, 4):
                        j = 4 * J + jj
                        MM(PSF[:, 2 + jj, 0:129], ptb[:, jj, :], vxh[:, kb, 0:129], kb == 0, kb == j,
                           r=[bpt, bS["vxh"]], w=[bF[2 + jj]])
                    if kb == 4 * J + 3:
                        for jj in range(4):
                            j = 4 * J + jj
                            RECIP(rcol[:, jj:jj + 1], PSF[:, 2 + jj, 128:129], r=[bF[2 + jj]], w=[bS["rcol"]])
                            TS("dve", yft[jj % 2], PSF[:, 2 + jj, 0:128], rcol[:, jj:jj + 1], None, ALU.mult,
                               r=[bF[2 + jj], bS["rcol"]], w=[bS[f"yft{jj % 2}"]])
                            TR(PSB[:, 1, (jj % 2) * 128:(jj % 2 + 1) * 128], yft[jj % 2], r=[bS[f"yft{jj % 2}"]], w=[bB[1]])
                            CP("act", yo[:, j * 128:(j + 1) * 128], PSB[:, 1, (jj % 2) * 128:(jj % 2 + 1) * 128], r=[bB[1]], w=[byo])

                emit_qk(0)
                for si in range(len(steps)):
                    if si + 1 < len(steps):
                        emit_qk(si + 1)
                    emit_rest(si)
                DMA("sp", yfxT_d[b, h, :, :], yo, r=[byo], w=[byfxT_d[b][h]])

            bS = new_phase("W2 W3 yg0 yg1 yf0 yf1 mT sgA sgB tA x1t0 x1t1".split(), SCR0,
                           manual={"W2": [keep["Wml"], bWfx], "W3": [bWfx], "yg1": [bWfx], "yf1": [bWfx]})
            bS["W0"], bS["W1"] = bW40, bW41
            phase_bufs[0] = phase_bufs[0] + [bW40, bW41]
            wpf = WBv(24576, 8, 1024)
            wo = WBv(32768, 8, 1024)
            load_w(wpf, w_pf_d, [bS["W2"]])
            load_w(wo, w_out_d, [bS["W3"]])
            ymlTg = [carve_b(4096).rearrange("p (i k t) -> p i k t", k=8, t=128),
                     ARB[:, 40960:45056].rearrange("p (i k t) -> p i k t", k=8, t=128)]
            yfxTg = [carve_b(4096).rearrange("p (k t) -> p k t", t=512),
                     ARB[:, 45056:49152].rearrange("p (k t) -> p k t", t=512)]
            mT = carve_b(4096).rearrange("p (k t) -> p k t", t=512)
            sgA = carve_f(512)
            sgB = carve_f(512)
            tA = carve_f(512)
            x1tt = [carve_f(1024) for _ in range(2)]
            def p4_loads(G):
                yg = ymlTg[G % 2]
                byg = bS[f"yg{G % 2}"]
                yf = yfxTg[G % 2]
                byf = bS[f"yf{G % 2}"]
                for ti in range(4):
                    DMA("sp", yg[:, ti, :, :].rearrange("p k t -> p (k t)"), ymlT_d[b, G * 4 + ti, :, :],
                        r=[bymlT_d[b][G * 4 + ti]], w=[byg])
                DMA("sp", yf, yfxT_d[b, :, :, G * 512:(G + 1) * 512].rearrange("h p t -> p h t"), r=byfxT_d[b], w=[byf])

            p4_loads(0)
            for G in range(4):
                gsl = slice(G * 512, (G + 1) * 512)
                yg = ymlTg[G % 2]
                byg = bS[f"yg{G % 2}"]
                yf = yfxTg[G % 2]
                byf = bS[f"yf{G % 2}"]
                for m in range(8):
                    msl = slice(m * 128, (m + 1) * 128)
                    for k in range(8):
                        MM(PSF[:, 0, :], wpm[:, k, msl], yg[:, :, k, :], k == 0, k == 7, r=[bS["W1"], byg], w=[bF[0]])
                    for k in range(8):
                        MM(PSF[:, 1, :], wg[:, k, msl], A32[:, k, gsl], k == 0, k == 7, r=[bS["W0"]] + bA32t[4 * G:4 * G + 4], w=[bF[1]])
                    for k in range(8):
                        MM(PSF[:, 2, :], wpf[:, k, msl], yf[:, k, :], k == 0, k == 7, r=[bS["W2"], byf], w=[bF[2]])
                    for k in range(8):
                        MM(PSF[:, 3, :], wg[:, k, 1024 + m * 128:1024 + (m + 1) * 128], A32[:, k, gsl], k == 0, k == 7,
                           r=[bS["W0"]] + bA32t[4 * G:4 * G + 4], w=[bF[3]])
                    ACT(sgA, PSF[:, 1, :], AF.Sigmoid, bias=colsS[:, CB_GML + m:CB_GML + m + 1], r=[bF[1], bconst], w=[bS["sgA"]])
                    ACT(sgB, PSF[:, 3, :], AF.Sigmoid, bias=colsS[:, CB_GFX + m:CB_GFX + m + 1], r=[bF[3], bconst], w=[bS["sgB"]])
                    TT("dve", tA, PSF[:, 0, :], sgA, ALU.mult, r=[bF[0], bS["sgA"]], w=[bS["tA"]])
                    TT("dve", sgB, PSF[:, 2, :], sgB, ALU.mult, r=[bF[2], bS["sgB"]], w=[bS["sgB"]])
                    TT("pool", mT[:, m, :], tA, sgB, ALU.add, r=[bS["tA"], bS["sgB"]], w=[bS["mT"]])
                if G + 1 < 4:
                    p4_loads(G + 1)
                for ti in range(4):
                    i = G * 4 + ti
                    DMA("sp", xt[i % 2][:], x_d[b, i * 128:(i + 1) * 128, :], w=[bxt[i % 2]])
                    for hf in range(2):
                        for k in range(8):
                            MM(PSF[:, 4 + hf, :], mT[:, k, ti * 128:(ti + 1) * 128], wo[:, k, hf * 512:(hf + 1) * 512], k == 0, k == 7,
                               r=[bS["mT"], bS["W3"]], w=[bF[4 + hf]])
                        TT("dve", x1tt[i % 2][:, hf * 512:(hf + 1) * 512], xt[i % 2][:, hf * 512:(hf + 1) * 512], PSF[:, 4 + hf, :], ALU.add,
                           r=[bxt[i % 2], bF[4 + hf]], w=[bS[f"x1t{i % 2}"]])
                    DMA("sp", x1_d[b, i, :, :], x1tt[i % 2], r=[bS[f"x1t{i % 2}"]], w=[bx1_d[b][i]])

            bS = new_phase("W0 W1 W3 mnT KT Vx xn2T xqT PT0 PT1 otm x1a0 x1a1 x1b0 x1b1 x2t0 x2t1 xn3f0 xn3f1 rc5".split(), SCR5)
            wxq = WBv(0, 8, 1024)
            wxkv = WBv(8192, 8, 2048)
            wxo = WBv(24576, 8, 1024)
            load_w(wxkv, w_xkv_d, [bS["W1"]])
            load_w(wxq, w_xq_d, [bS["W0"]])
            load_w(wxo, w_xo_d, [bS["W3"]])
            mn_o = scr_off[0]
            mnT = carve_b(2048).rearrange("p (k n) -> p k n", n=256)
            KT = carve_b(2048).rearrange("p (c n) -> p c n", n=256)
            Vx = carve_b(2 * 4 * 258).rearrange("p (t h n) -> p t h n", h=4, n=258)
            xn2T = carve_b(4096).rearrange("p (k t) -> p k t", t=512)
            oT = xn2T
            xqT = carve_b(4096).rearrange("p (k t) -> p k t", t=512)
            PTx = [carve_b(1024).rearrange("p (n t) -> p n t", t=512) for _ in range(2)]
            otm = carve_b(4096).rearrange("p (t f) -> p t f", f=1024)
            x1a_ = [carve_f(1024), ARENA[:, mn_o:mn_o + 1024]]
            bS["x1a1"] = bS["mnT"]
            x1b_ = [carve_f(1024) for _ in range(2)]
            x2t_ = [carve_f(1024) for _ in range(2)]
            xn3f_ = [carve_f(1024).rearrange("p (k t) -> p k t", t=128) for _ in range(2)]
            rc5 = carve_f(4)
            if b == 0:
                P.inherit([blg], phase_bufs[0])
            MSET("dve", Vx, 1.0, w=[bS["Vx"]])
            for mt in range(2):
                DMA("sp", xt[mt][:], mem_d[b, mt * 128:(mt + 1) * 128, :], w=[bxt[mt]])
                norm_transpose(xt[mt][:], [bxt[mt]], colsS[:, CG_XMEM:CG_XMEM + 8], mnT[:, :, mt * 128:(mt + 1) * 128], [bS["mnT"]])
            for c8 in range(8):
                for k in range(8):
                    MM(PSF[:, c8 % 2, 0:256], wxkv[:, k, c8 * 128:(c8 + 1) * 128], mnT[:, k, :], k == 0, k == 7,
                       r=[bS["W1"], bS["mnT"]], w=[bF[c8 % 2]])
                CP("dve", KT[:, c8, :], PSF[:, c8 % 2, 0:256], r=[bF[c8 % 2]], w=[bS["KT"]])
            for mt in range(2):
                for hf in range(2):
                    for k in range(8):
                        MM(PSF[:, 2 + hf, :], mnT[:, k, mt * 128:(mt + 1) * 128], wxkv[:, k, 1024 + hf * 512:1024 + (hf + 1) * 512],
                           k == 0, k == 7, r=[bS["W1"], bS["mnT"]], w=[bF[2 + hf]])
                    CP("act", Vx[:, mt, 2 * hf:2 * hf + 2, 0:256], PSF[:, 2 + hf, :].rearrange("p (h n) -> p h n", n=256),
                       r=[bF[2 + hf]], w=[bS["Vx"]])
            pti = 0
            for G in range(4):
                for ti in range(4):
                    i = G * 4 + ti
                    DMA("sp", x1a_[ti % 2], x1_d[b, i, :, :], r=[bx1_d[b][i]], w=[bS[f"x1a{ti % 2}"]])
                    norm_transpose(x1a_[ti % 2], [bS[f"x1a{ti % 2}"]], colsS[:, CG_XQ:CG_XQ + 8], xn2T[:, :, ti * 128:(ti + 1) * 128], [bS["xn2T"]])
                for c8 in range(8):
                    for k in range(8):
                        MM(PSF[:, c8 % 2, :], wxq[:, k, c8 * 128:(c8 + 1) * 128], xn2T[:, k, :], k == 0, k == 7,
                           r=[bS["W0"], bS["xn2T"]], w=[bF[c8 % 2]])
                    ACT(xqT[:, c8, :], PSF[:, c8 % 2, :], AF.Identity, scale=1.0 / 16.0, r=[bF[c8 % 2]], w=[bS["xqT"]])
                def x_qk(h):
                    for nt_ in range(2):
                        pbk = nt_ if h % 2 == 0 else 4 + nt_
                        for c2 in range(2):
                            MM(PSF[:, pbk, :], KT[:, 2 * h + c2, nt_ * 128:(nt_ + 1) * 128], xqT[:, 2 * h + c2, :], c2 == 0, c2 == 1,
                               r=[bS["KT"], bS["xqT"]], w=[bF[pbk]])

                def x_rest(h):
                    ptb = PTx[h % 2]
                    bpt = bS[f"PT{h % 2}"]
                    for nt_ in range(2):
                        pbk = nt_ if h % 2 == 0 else 4 + nt_
                        ACT(ptb[:, nt_, :], PSF[:, pbk, :], AF.Exp, r=[bF[pbk]], w=[bpt])
                    for ti in range(4):
                        pb = 2 + (ti % 2)
                        for nt_ in range(2):
                            MM(PSF[:, pb, 0:257], ptb[:, nt_, ti * 128:(ti + 1) * 128], Vx[:, nt_, h, 0:257], nt_ == 0, nt_ == 1,
                               r=[bpt, bS["Vx"]], w=[bF[pb]])
                        RECIP(rc5[:, ti:ti + 1], PSF[:, pb, 256:257], r=[bF[pb]], w=[bS["rc5"]])
                        TS("dve", otm[:, ti, h * 256:(h + 1) * 256], PSF[:, pb, 0:256], rc5[:, ti:ti + 1], None, ALU.mult,
                           r=[bF[pb], bS["rc5"]], w=[bS["otm"]])

                x_qk(0)
                for h in range(4):
                    if h + 1 < 4:
                        x_qk(h + 1)
                    x_rest(h)
                if b + 1 < NB:
                    for ti in range(4):
                        p1_tile(b + 1, G * 4 + ti)
                for ti in range(4):
                    for k in range(8):
                        TR(PSB[:, ti % 2, k * 128:(k + 1) * 128], otm[:, ti, k * 128:(k + 1) * 128], r=[bS["otm"]], w=[bB[ti % 2]])
                    CP("act", oT[:, :, ti * 128:(ti + 1) * 128], PSB[:, ti % 2, :].rearrange("p (k t) -> p k t", t=128),
                       r=[bB[ti % 2]], w=[bS["xn2T"]])
                def e_mm(ti):
                    i = G * 4 + ti
                    x1b, x2t = x1b_[ti % 2], x2t_[ti % 2]
                    bx1b, bx2t = bS[f"x1b{ti % 2}"], bS[f"x2t{ti % 2}"]
                    DMA("sp", x1b, x1_d[b, i, :, :], r=[bx1_d[b][i]], w=[bx1b])
                    for hf in range(2):
                        for k in range(8):
                            MM(PSF[:, 2 + hf, :], oT[:, k, ti * 128:(ti + 1) * 128], wxo[:, k, hf * 512:(hf + 1) * 512], k == 0, k == 7,
                               r=[bS["xn2T"], bS["W3"]], w=[bF[2 + hf]])
                        TT("dve", x2t[:, hf * 512:(hf + 1) * 512], x1b[:, hf * 512:(hf + 1) * 512], PSF[:, 2 + hf, :], ALU.add,
                           r=[bx1b, bF[2 + hf]], w=[bx2t])
                    DMA("sp", x2_d[b, i, :, :], x2t, r=[bx2t], w=[bx2_d[b][i]])

                def e_tail(ti):
                    i = G * 4 + ti
                    x2t, xn3f = x2t_[ti % 2], xn3f_[ti % 2]
                    bx2t, bxn3f = bS[f"x2t{ti % 2}"], bS[f"xn3f{ti % 2}"]
                    norm_transpose(x2t, [bx2t], colsS[:, CG_MOE:CG_MOE + 8], xn3f, [bxn3f], fp32=True,
                                   tm_store=(xn3tm_d[b, i, :, :], bxn3_d[b][i]))
                    for k in range(8):
                        MM(PSF[:, 0, 0:36], xn3f[:, k, :], wrS[:, k, :], k == 0, k == 7, r=[bxn3f, bconst], w=[bF[0]])
                    TT("dve", lg[:, b * NT + i, :], PSF[:, 0, 0:36], rowsS[:, R_RG:R_RG + 36], ALU.add, r=[bF[0], bconst], w=[blg])

                e_mm(0)
                for ti in range(4):
                    if ti + 1 < 4:
                        e_mm(ti + 1)
                    e_tail(ti)

            phase_bufs[0] = phase_bufs[0] + [blg]
            last_p6[0] = list(phase_bufs[0])
        b = None
        names6 = ("GU0 GU1 GU2 DN0 DN1 route srt idx xst0 xst1 xst2 xg0 xg1 c8d0 c8d1 xT he0 he1 sg0 sg1 ys").split()
        bS = new_phase(names6, 16384)
        gmax = carve_f(NTT)
        goh = carve_f(NTT * 4).rearrange("p (t g) -> p t g", g=4)
        gex = carve_f(NTT * 4).rearrange("p (t g) -> p t g", g=4)
        gp = carve_f(NTT)
        tmp32_o = scr_off[0]
        tmp32 = carve_f(NTT * 32).rearrange("p (t g e) -> p t g e", g=4, e=8)
        esel = carve_f(NTT * 8).rearrange("p (t e) -> p t e", e=8)
        m1 = carve_f(NTT)
        m2 = carve_f(NTT)
        oh1 = carve_f(NTT * 8).rearrange("p (t e) -> p t e", e=8)
        oh2 = carve_f(NTT * 8).rearrange("p (t e) -> p t e", e=8)
        msk = carve_f(NTT * 8).rearrange("p (t e) -> p t e", e=8)
        w1 = carve_f(NTT)
        w2 = carve_f(NTT)
        c8t = carve_f(NTT * 8).rearrange("p (t e) -> p t e", e=8)
        bR = [bS["route"]]
        bc16 = lambda ap, n: ap.unsqueeze(2).to_broadcast([128, NTT, n])
        gl = lg[:, :, 0:4]
        RED("dve", gmax, gl, ALU.max, r=[blg], w=bR)
        TT("dve", goh, gl, bc16(gmax, 4), ALU.is_equal, r=[blg] + bR, w=bR)
        TT("dve", gex, gl, bc16(gmax, 4), ALU.subtract, r=[blg] + bR, w=bR)
        ACT(gex, gex, AF.Exp, r=bR, w=bR)
        RED("dve", gp, gex, ALU.add, r=bR, w=bR)
        RECIP(gp, gp, r=bR, w=bR)
        el = lg[:, :, 4:36].rearrange("p t (g e) -> p t g e", e=8)
        TT("dve", tmp32, el, goh.unsqueeze(3).to_broadcast([128, NTT, 4, 8]), ALU.mult, r=[blg] + bR, w=bR)
        RED("dve", esel, tmp32.rearrange("p t g e -> p t e g"), ALU.add, r=bR, w=bR)
        RED("dve", m1, esel, ALU.max, r=bR, w=bR)
        TT("dve", oh1, esel, bc16(m1, 8), ALU.is_equal, r=bR, w=bR)
        STT("dve", msk, oh1, -1e30, esel, ALU.mult, ALU.add, r=bR, w=bR)
        RED("dve", m2, msk, ALU.max, r=bR, w=bR)
        TT("dve", oh2, msk, bc16(m2, 8), ALU.is_equal, r=bR, w=bR)
        TT("dve", w2, m2, m1, ALU.subtract, r=bR, w=bR)
        ACT(w2, w2, AF.Exp, r=bR, w=bR)
        TS("dve", w2, w2, 1.0, None, ALU.add, r=bR, w=bR)
        RECIP(w1, w2, r=bR, w=bR)
        TT("dve", w1, w1, gp, ALU.mult, r=bR, w=bR)
        TT("dve", w2, gp, w1, ALU.subtract, r=bR, w=bR)
        TT("dve", c8t, oh1, bc16(w1, 8), ALU.mult, r=bR, w=bR)
        TT("dve", oh2, oh2, bc16(w2, 8), ALU.mult, r=bR, w=bR)
        TT("dve", c8t, c8t, oh2, ALU.add, r=bR, w=bR)
        bT = [bS["srt"]]
        gohb = carve_b(NTT * 4)
        pre = carve_f(NTT * 4).rearrange("p (t g) -> p t g", g=4)
        posall = carve_f(NTT * 4).rearrange("p (t g) -> p t g", g=4)
        ntot = carve_f(4)
        cnt = carve_f(4)
        tmp4 = carve_f(4)
        endg = carve_f(4)
        baseg = carve_f(4)
        pos_f = carve_f(NTT)
        pos_o = scr_off[0]
        pos_i = ARI[:, pos_o:pos_o + NTT]
        carve_f(NTT)
        gid = carve_f(NGT)
        g8k = carve_f(NGT)
        g4k = carve_f(NGT)
        idxf = carve_f(8)
        io = scr_off[0]
        idx_e = ARI[:, io:io + NGT * 8].rearrange("p (s n) -> p s n", n=8)
        carve_f(NGT * 8)
        CP("dve", gohb, goh.rearrange("p t g -> p (t g)"), r=bR, w=bT)
        MM(PSF[:, 0, 0:NTT * 4], strib[:], gohb, r=bT + [bconst], w=[bF[0]])
        MM(PSF[:, 1, 0:NTT * 4], onesb[:], gohb, r=bT + [bconst], w=[bF[1]])
        Lp = PSF[:, 0, 0:NTT * 4].rearrange("p (t g) -> p t g", g=4)
        Tp = PSF[:, 1, 0:NTT * 4].rearrange("p (t g) -> p t g", g=4)
        MSET("dve", pre[:, 0, :], 0.0, w=bT)
        for i in range(1, NTT):
            TT("dve", pre[:, i, :], pre[:, i - 1, :], Tp[:, i - 1, :], ALU.add, r=bT + [bF[1]], w=bT)
        TT("dve", ntot, pre[:, NTT - 1, :], Tp[:, NTT - 1, :], ALU.add, r=bT + [bF[1]], w=bT)
        TS("dve", cnt, ntot, 0.0, None, ALU.is_gt, r=bT, w=bT)
        for q in range(1, 8):
            TS("dve", tmp4, ntot, 512.0 * q, None, ALU.is_gt, r=bT, w=bT)
            TT("dve", cnt, cnt, tmp4, ALU.add, r=bT, w=bT)
        TS("dve", cnt, cnt, 512.0, None, ALU.mult, r=bT, w=bT)
        CP("dve", endg[:, 0:1], cnt[:, 0:1], r=bT, w=bT)
        for g in (1, 2, 3):
            TT("dve", endg[:, g:g + 1], endg[:, g - 1:g], cnt[:, g:g + 1], ALU.add, r=bT, w=bT)
        TT("dve", baseg, endg, cnt, ALU.subtract, r=bT, w=bT)
        TT("dve", posall, pre, Lp, ALU.add, r=bT + [bF[0]], w=bT)
        TT("dve", posall, posall, baseg.unsqueeze(1).to_broadcast([128, NTT, 4]), ALU.add, r=bT, w=bT)
        TT("dve", posall, posall, goh, ALU.mult, r=bT + bR, w=bT)
        RED("dve", pos_f, posall, ALU.add, r=bT, w=bT)
        CP("dve", pos_i, pos_f, r=bT, w=bT)
        for sidx in range(NGT):
            TS("dve", tmp4, endg, 512.0 * sidx, None, ALU.is_le, r=bT, w=bT)
            RED("dve", gid[:, sidx:sidx + 1], tmp4, ALU.add, r=bT, w=bT)
        TS("dve", gid, gid, 3.0, None, ALU.min, r=bT, w=bT)
        TS("dve", g8k, gid, 1024.0, None, ALU.mult, r=bT, w=bT)
        bI = [bS["idx"]]
        for sidx in range(NGT):
            TS("dve", idxf, cguS[:, 0:8], g8k[:, sidx:sidx + 1], None, ALU.add, r=bT + [bconst], w=bT)
            CP("dve", idx_e[:, sidx, :], idxf, r=bT, w=bI)
        if DEBUG:
            dbt = carve_f(64)
            MSET("dve", dbt, 0.0, w=bT)
            CP("dve", dbt[:, 0:16], pos_f, r=bT, w=bT)
            CP("dve", dbt[:, 16:24], gid, r=bT, w=bT)
            CP("dve", dbt[:, 24:28], endg, r=bT, w=bT)
            CP("dve", dbt[:, 28:32], ntot, r=bT, w=bT)
            CP("dve", dbt[:, 32:48], pos_i, r=bT, w=bT)
            DMA("sp", dbg_d[0, :, :], dbt, r=bT, is_out=True)
        c8h = ARENA[:, tmp32_o:tmp32_o + NTT * 8]
        c8r = ARENA[:, tmp32_o + NTT * 8:tmp32_o + NTT * 16]
        c8p = ARB[:, 2 * (tmp32_o + NTT * 16):2 * (tmp32_o + NTT * 16) + NTT * 24].rearrange("p (t q e) -> p t q e", q=3, e=8)
        c8flat = c8t.rearrange("p t e -> p (t e)")
        c8rv = c8r.rearrange("p (t e) -> p t e", e=8)
        c8hv = c8h.rearrange("p (t e) -> p t e", e=8)
        CP("dve", c8p[:, :, 0, :], c8t, r=bR, w=bT)
        CP("dve", c8hv, c8p[:, :, 0, :], r=bT, w=bT)
        TT("dve", c8r, c8flat, c8h, ALU.subtract, r=bR + bT, w=bT)
        CP("dve", c8p[:, :, 1, :], c8rv, r=bT, w=bT)
        CP("dve", c8hv, c8p[:, :, 1, :], r=bT, w=bT)
        TT("dve", c8r, c8r, c8h, ALU.subtract, r=bT, w=bT)
        CP("dve", c8p[:, :, 2, :], c8rv, r=bT, w=bT)
        xst = [carve_b(XW) for _ in range(2)]
        for i in range(NTT):
            bx = bS[f"xst{i % 2}"]
            DMA("sp", xst[i % 2][:, 0:1024], xn3tm_d[i // NT, i % NT, :, :], r=[bxn3_d[i // NT][i % NT]], w=[bx])
            CP("dve", xst[i % 2][:, 1024:1048], c8p[:, i, :, :].rearrange("p q e -> p (q e)"), r=bT, w=[bx])
            P.dma("pool", (lambda xs_, col: (lambda e: e.indirect_dma_start(
                out=Xs_d[:, :], out_offset=bass.IndirectOffsetOnAxis(ap=pos_i[:, col:col + 1], axis=0),
                in_=xs_, in_offset=None, bounds_check=NSL - 1, oob_is_err=False)))(xst[i % 2], i),
                [bx] + bT, bXs, group=("scatter",))
        fin_base = scr_off[0]
        xg2 = [carve_b(4 * XW).rearrange("p (c f) -> p c f", f=XW) for _ in range(2)]
        c8s2 = [carve_f(32).rearrange("p (c e) -> p c e", e=8) for _ in range(2)]
        xTs = carve_b(4096).rearrange("p (k t) -> p k t", t=512)
        heT = [carve_b(2048).rearrange("p (f t) -> p f t", t=512) for _ in range(2)]
        sgm = [carve_f(512) for _ in range(2)]
        ys = carve_f(4096).rearrange("p (c f) -> p c f", f=D)
        bhe = [bS["he0"], bS["he1"]]
        bsg = [bS["sg0"], bS["sg1"]]
        bGU = [bS["GU0"], bS["GU1"], bS["GU2"]]
        bDN = [bS["DN0"], bS["DN1"]]
        bxg = [bS["xg0"], bS["xg1"]]
        units = [(sidx, j) for sidx in range(NGT) for j in range(8)]
        NU = len(units)

        def gather_gu(u):
            sidx, j = units[u]
            g_ = u % 3
            grp_ctr[0] += 1
            for (dst_, rows_) in ((WBv(g_ * 8192, 8, 512), wg_rows), (WBv(g_ * 8192 + 4096, 8, 512), wu_rows)):
                P.dma("pool", (lambda o_, r_, ix_: (lambda e: e.indirect_dma_start(
                    out=o_, out_offset=None, in_=r_, in_offset=bass.IndirectOffsetOnAxis(ap=ix_, axis=0))))(
                        dst_.rearrange("p k n -> p (k n)"), rows_[:, :], idx_e[:, sidx, j:j + 1]),
                    bI, [bGU[g_]], group=("gu", grp_ctr[0]))

        def gather_dn(u):
            sidx, j = units[u]
            d_ = u % 2
            grp_ctr[0] += 1
            P.dma("pool", (lambda o_, r_, ix_: (lambda e: e.indirect_dma_start(
                out=o_, out_offset=None, in_=r_, in_offset=bass.IndirectOffsetOnAxis(ap=ix_, axis=0))))(
                    WBv(24576 + d_ * 4096, 4, 1024).rearrange("p k n -> p (k n)"), wd_rows[:, :], idx_e[:, sidx, j:j + 1]),
                bI, [bDN[d_]], group=("dn", grp_ctr[0]))

        def prologue(sidx):
            xg = xg2[sidx % 2]
            DMA("sp", xg, Xs_d[sidx * 512:(sidx + 1) * 512, :].rearrange("(c p) f -> p c f", p=128), r=[bXs[sidx]], w=[bxg[sidx % 2]])
            for c in range(4):
                for k in range(8):
                    TR(PSB[:, c % 2, k * 128:(k + 1) * 128], xg[:, c, k * 128:(k + 1) * 128], r=[bxg[sidx % 2]], w=[bB[c % 2]])
                CP("act" if c % 2 == 0 else "dve", xTs[:, :, c * 128:(c + 1) * 128],
                   PSB[:, c % 2, :].rearrange("p (k t) -> p k t", t=128), r=[bB[c % 2]], w=[bS["xT"]])
            pcs = xg[:, :, 1024:1048].rearrange("p c (q e) -> p c q e", e=8)
            c8d = c8s2[sidx % 2]
            TT("dve", c8d, pcs[:, :, 0, :], pcs[:, :, 1, :], ALU.add, r=[bxg[sidx % 2]], w=[bS[f"c8d{sidx % 2}"]])
            TT("dve", c8d, c8d, pcs[:, :, 2, :], ALU.add, r=[bxg[sidx % 2], bS[f"c8d{sidx % 2}"]], w=[bS[f"c8d{sidx % 2}"]])

        def emit_gu(u):
            g_ = u % 3
            wge = WBv(g_ * 8192, 8, 512)
            wue = WBv(g_ * 8192 + 4096, 8, 512)
            he, bh = heT[u % 2], bhe[u % 2]
            for fc in range(4):
                fsl = slice(fc * 128, (fc + 1) * 128)
                pg, pu = (fc % 2) * 2, (fc % 2) * 2 + 1
                for k in range(8):
                    MM(PSF[:, pg, :], wge[:, k, fsl], xTs[:, k, :], k == 0, k == 7, r=[bGU[g_], bS["xT"]], w=[bF[pg]])
                for k in range(8):
                    MM(PSF[:, pu, :], wue[:, k, fsl], xTs[:, k, :], k == 0, k == 7, r=[bGU[g_], bS["xT"]], w=[bF[pu]])
                ACT(sgm[fc % 2], PSF[:, pg, :], AF.Silu, r=[bF[pg]], w=[bsg[fc % 2]])
                TT("dve", he[:, fc, :], sgm[fc % 2], PSF[:, pu, :], ALU.mult, r=[bsg[fc % 2], bF[pu]], w=[bh])

        def emit_down(u):
            sidx, j = units[u]
            d_ = u % 2
            wde = WBv(24576 + d_ * 4096, 4, 1024)
            he, bh = heT[u % 2], bhe[u % 2]
            c8s = c8s2[sidx % 2]
            for c in range(4):
                for hf in range(2):
                    for fc in range(4):
                        MM(PSF[:, 4 + hf, :], he[:, fc, c * 128:(c + 1) * 128], wde[:, fc, hf * 512:(hf + 1) * 512],
                           fc == 0, fc == 3, r=[bh, bDN[d_]], w=[bF[4 + hf]])
                    ysl = ys[:, c, hf * 512:(hf + 1) * 512]
                    if j == 0:
                        TS("dve", ysl, PSF[:, 4 + hf, :], c8s[:, c, 0:1], None, ALU.mult, r=[bF[4 + hf], bS[f"c8d{sidx % 2}"]], w=[bS["ys"]])
                    else:
                        STT("dve", ysl, PSF[:, 4 + hf, :], c8s[:, c, j:j + 1], ysl, ALU.mult, ALU.add,
                            r=[bF[4 + hf], bS[f"c8d{sidx % 2}"], bS["ys"]], w=[bS["ys"]])
            if j == 7:
                DMA("sp", Ys_d[sidx * 512:(sidx + 1) * 512, :].rearrange("(c p) f -> p c f", p=128), ys, r=[bS["ys"]], w=[bYs[sidx]])

        gather_gu(0)
        gather_dn(0)
        gather_gu(1)
        gather_dn(1)
        prologue(0)
        emit_gu(0)
        for u in range(NU):
            if u + 2 < NU:
                gather_gu(u + 2)
            if u + 1 < NU:
                if units[u + 1][0] != units[u][0]:
                    prologue(units[u + 1][0])
                emit_gu(u + 1)
            emit_down(u)
            if u + 2 < NU:
                gather_dn(u + 2)
        fin_b = [Buf(f"yg{q}") for q in range(4)] + [Buf(f"x2f{q}") for q in range(4)]
        P.inherit(fin_b, [bxg[0], bxg[1], bS["xT"], bhe[0], bhe[1], bsg[0], bsg[1]])
        phase_bufs[0] = phase_bufs[0] + fin_b
        scr_off[0] = fin_base
        ygt = [carve_f(1024) for _ in range(4)]
        x2f = [carve_f(1024) for _ in range(4)]
        fst = [carve_f(4) for _ in range(4)]
        def fin_load(i):
            q = i % 4
            byg, bx2 = fin_b[q], fin_b[4 + q]
            P.dma("pool", (lambda o_, col: (lambda e: e.indirect_dma_start(
                out=o_, out_offset=None, in_=Ys_d[:, :],
                in_offset=bass.IndirectOffsetOnAxis(ap=pos_i[:, col:col + 1], axis=0))))(ygt[q], i),
                bYs + bT, [byg])
            DMA("sp", x2f[q], x2_d[i // NT, i % NT, :, :], r=[bx2_d[i // NT][i % NT]], w=[bx2])

        def fin_compute(i):
            q = i % 4
            byg, bx2 = fin_b[q], fin_b[4 + q]
            TT("dve", x2f[q], x2f[q], ygt[q], ALU.add, r=[bx2, byg], w=[bx2])
            ACT(junk[:], x2f[q], AF.Square, accum=fst[q][:, 0:1], r=[bx2], w=[bjunk, bfst[q]])
            ACT(fst[q][:, 1:2], fst[q][:, 0:1], AF.Ln, bias=epsc[:, 0:1], scale=1.0 / D, r=[bfst[q], bconst], w=[bfst[q]])
            ACT(fst[q][:, 2:3], fst[q][:, 1:2], AF.Exp, scale=-0.5, r=[bfst[q]], w=[bfst[q]])
            STT("dve", x2f[q], x2f[q], fst[q][:, 2:3], gfinS[:], ALU.mult, ALU.mult, r=[bx2, bfst[q], bconst], w=[bx2])
            DMA("sp", out_d[i // NT, (i % NT) * 128:(i % NT + 1) * 128, :], x2f[q], r=[bx2], is_out=True)

        bfst = [Buf(f"fst{q}") for q in range(4)]
        P.inherit(bfst, [bxg[0], bxg[1], bS["xT"], bhe[0], bhe[1], bsg[0], bsg[1]])
        phase_bufs[0] = phase_bufs[0] + bfst
        for i in range(NTT + 3):
            if i < NTT:
                fin_load(i)
            if i >= 3:
                fin_compute(i - 3)
        P.emit()
    return nc


_CACHE = {}
_p = np.arange(128, dtype=np.float32)[:, None]
_CGU = np.zeros((128, 64), np.float32)
_CGU[:, 0:8] = np.arange(8, dtype=np.float32)[None, :] * 128 + _p
_CD = np.ascontiguousarray((np.arange(8, dtype=np.float32)[None, :, None] * 512 + np.arange(4, dtype=np.float32)[None, None, :] * 128
                            + _p[:, :, None]).reshape(128, 32).astype(np.float32))


def _host_consts():
    ident = np.eye(128, dtype=np.float32)
    tri = np.triu(np.ones((128, 128), np.float32))
    return ident, tri


def kernel(x, mem, g_mix, w_in, b_ml_i, b_ml_f, b_fx_f, b_gate_ml, b_gate_fx, g_ml_head,
           w_proj_ml, w_proj_fx, w_out, g_xq, g_xmem, w_xq, w_xkv, w_xo, g_moe,
           w_rg, b_rg, w_re, b_re, w_gate, w_up, w_down, g_final):
    f = lambda a: np.ascontiguousarray(np.asarray(a, dtype=np.float32))
    x = f(x)
    mem = f(mem)
    col = lambda v: f(v).reshape(8, 128).T
    cols = np.ascontiguousarray(np.concatenate(
        [col(g_mix[0]), col(g_xq[0]), col(g_xmem[0]), col(g_moe[0]), col(g_ml_head[0]),
         col(b_gate_ml[0]), col(b_gate_fx[0])], axis=1))
    rows = np.ascontiguousarray(np.concatenate(
        [f(b_ml_i[0]), f(b_ml_f[0]), f(b_fx_f[0]), f(b_rg[0]), f(b_re[0])])[None, :])
    w_r = np.ascontiguousarray(np.concatenate([f(w_rg[0]), f(w_re[0])], axis=1))
    ident, tri = _host_consts()
    shared = dict(
        w_in=f(w_in[0]), w_proj_ml=f(w_proj_ml[0]), w_proj_fx=f(w_proj_fx[0]), w_out=f(w_out[0]),
        w_xq=f(w_xq[0]), w_xkv=f(w_xkv[0]), w_xo=f(w_xo[0]), w_r=w_r,
        w_gate=np.ascontiguousarray(f(w_gate[0]).reshape(NE, 8, 128, DE).transpose(0, 2, 1, 3)).reshape(NE * 128, 8 * DE),
        w_up=np.ascontiguousarray(f(w_up[0]).reshape(NE, 8, 128, DE).transpose(0, 2, 1, 3)).reshape(NE * 128, 8 * DE),
        w_down=np.ascontiguousarray(f(w_down[0]).reshape(NE, 4, 128, D).transpose(0, 2, 1, 3)).reshape(NE * 128, 4 * D),
        cols=cols, rows=rows, g_final=f(g_final)[None, :], ident=ident, tri=tri,
        g_moe_row=f(g_moe[0])[None, :], cgu=_CGU, cd=_CD)
    if "nc" not in _CACHE:
        _CACHE["nc"] = build_program()
    nc = _CACHE["nc"]
    in_maps = []
    for c in range(8):
        m = dict(shared)
        m["x"] = np.ascontiguousarray(x[2 * c:2 * c + 2])
        m["mem"] = np.ascontiguousarray(mem[2 * c:2 * c + 2])
        in_maps.append(m)
    res = run_bass_kernel_spmd(nc, in_maps, core_ids=list(range(8)))
    out = np.concatenate([np.asarray(r["out"]) for r in res.results], axis=0)
    return out.astype(np.float32)
```
